# Optimizing a Trainium2 kernel written in Bass

```python
import math
import jax
import jax.numpy as jnp
from jax import lax
import numpy as np


D_MODEL = 1024
BATCH = 2
SEQ = 16384
DEPTH = 2

N_META = 16
A_HEAD_DIM = 64
A_W = D_MODEL // 2
A_HEADS = A_W // A_HEAD_DIM
IDX_HEADS = 8
IDX_DIM = 64
TOPK_MAX = 256
B_HEAD_DIM = 64
B_HALF = B_HEAD_DIM // 2
B_W = D_MODEL // 4
B_HEADS = B_W // B_HEAD_DIM
C_HEAD_DIM = 64
C_W = D_MODEL // 4
C_HEADS = C_W // C_HEAD_DIM
RET_CHUNK = 128
ROPE_BASE = 10000.0
REL_BUCKETS = 32
REL_MAX_DIST = 128
D_FF = 4 * D_MODEL
Q_BLOCK = 128
EPS = 1e-6
IN_SIZES = (A_W, A_W, A_W, IDX_HEADS * IDX_DIM, IDX_DIM, IDX_HEADS, B_W, B_W, B_W, C_W, C_W, C_W, C_W)
IN_COLS = sum(IN_SIZES)

kernel_name = 'hybrid_dsa_diff_retention_block'


def _split_points():
    pts, acc = [], 0
    for s in IN_SIZES[:-1]:
        acc += s
        pts.append(acc)
    return pts


def rmsnorm(x, g):
    xf = x.astype(jnp.float32)
    y = xf * lax.rsqrt(jnp.mean(xf * xf, axis=-1, keepdims=True) + EPS)
    return (y * g.astype(jnp.float32)).astype(x.dtype)


def rel_bucket(dist):
    n = jnp.maximum(dist, 0)
    max_exact = REL_BUCKETS // 2
    nf = jnp.maximum(n, 1).astype(jnp.float32)
    large = max_exact + (jnp.log(nf / max_exact) / math.log(REL_MAX_DIST / max_exact)
                         * (REL_BUCKETS - max_exact)).astype(jnp.int32)
    large = jnp.minimum(large, REL_BUCKETS - 1)
    return jnp.where(n < max_exact, n, large)


def rotary(x, pos):
    d = x.shape[-1]
    inv = ROPE_BASE ** (-jnp.arange(0, d, 2, dtype=jnp.float32) / d)
    ang = pos.astype(jnp.float32)[:, None] * inv[None, :]
    cos = jnp.cos(ang)[None, :, None, :]
    sin = jnp.sin(ang)[None, :, None, :]
    xf = x.astype(jnp.float32)
    x1, x2 = xf[..., : d // 2], xf[..., d // 2:]
    return jnp.concatenate([x1 * cos - x2 * sin, x1 * sin + x2 * cos], axis=-1).astype(x.dtype)


def to_blocks(a, nblk):
    pad = nblk * Q_BLOCK - a.shape[1]
    a = jnp.pad(a, [(0, 0), (0, pad)] + [(0, 0)] * (a.ndim - 2))
    a = a.reshape((a.shape[0], nblk, Q_BLOCK) + a.shape[2:])
    return jnp.moveaxis(a, 1, 0)


def from_blocks(a, t_len):
    a = jnp.moveaxis(a, 0, 1)
    a = a.reshape((a.shape[0], -1) + a.shape[3:])
    return a[:, :t_len]


def sparse_indexer_attention(q, k, v, qi, ki, wi, bias_table, topk):
    bsz, t_len = q.shape[0], q.shape[1]
    nblk = -(-t_len // Q_BLOCK)
    key_pos = jnp.arange(t_len)
    scale = A_HEAD_DIM ** -0.5
    wi = wi.astype(jnp.float32) * (IDX_HEADS ** -0.5 * IDX_DIM ** -0.5)
    bidx = jnp.arange(bsz)[:, None]

    def body(xs):
        qb, qib, wib, start = xs
        qpos = start + jnp.arange(Q_BLOCK)
        causal = key_pos[None, :] <= qpos[:, None]
        dots = jax.nn.relu(jnp.einsum('bqhd,bsd->bqhs', qib, ki)).astype(jnp.float32)
        score = jnp.einsum('bqh,bqhs->bqs', wib, dots)
        score = jnp.where(causal[None], score, -jnp.inf)
        _, idx = lax.top_k(score, topk)
        flat = idx.reshape(bsz, Q_BLOCK * topk)
        kg = k[bidx, flat].reshape(bsz, Q_BLOCK, topk, A_HEADS, A_HEAD_DIM)
        vg = v[bidx, flat].reshape(bsz, Q_BLOCK, topk, A_HEADS, A_HEAD_DIM)
        dist = qpos[None, :, None] - idx
        bias = jnp.moveaxis(bias_table[rel_bucket(dist)], -1, 1).astype(jnp.float32)
        logits = jnp.einsum('bqhd,bqkhd->bhqk', qb, kg).astype(jnp.float32) * scale + bias
        logits = jnp.where((dist >= 0)[:, None], logits, -jnp.inf)
        p = jax.nn.softmax(logits, axis=-1).astype(v.dtype)
        return jnp.einsum('bhqk,bqkhd->bqhd', p, vg)

    starts = jnp.arange(nblk) * Q_BLOCK
    out = lax.map(body, (to_blocks(q, nblk), to_blocks(qi, nblk), to_blocks(wi, nblk), starts))
    return from_blocks(out, t_len)


def diff_attention(q, k, v, bias_table, lam):
    t_len = q.shape[1]
    nblk = -(-t_len // Q_BLOCK)
    key_pos = jnp.arange(t_len)
    scale = B_HALF ** -0.5

    def body(xs):
        qb, start = xs
        qpos = start + jnp.arange(Q_BLOCK)
        dist = qpos[:, None] - key_pos[None, :]
        bias = jnp.moveaxis(bias_table[rel_bucket(dist)], -1, 0).astype(jnp.float32)
        logits = jnp.einsum('bqhcd,bshcd->bchqs', qb, k).astype(jnp.float32) * scale + bias[None, None]
        logits = jnp.where(dist >= 0, logits, -jnp.inf)
        p = jax.nn.softmax(logits, axis=-1)
        w = (p[:, 0] - lam * p[:, 1]).astype(v.dtype)
        return jnp.einsum('bhqs,bshd->bqhd', w, v)

    starts = jnp.arange(nblk) * Q_BLOCK
    out = lax.map(body, (to_blocks(q, nblk), starts))
    return from_blocks(out, t_len)


def _retention_chunk(qc, kc, vc, state, log_gamma):
    L = qc.shape[1]
    i = jnp.arange(L, dtype=jnp.float32)
    diff = i[:, None] - i[None, :]
    dmat = jnp.where(diff >= 0, jnp.exp(log_gamma[:, None, None] * jnp.maximum(diff, 0.0)), 0.0)
    inner = jnp.einsum('bihd,bjhd->bhij', qc, kc) * dmat
    out = jnp.einsum('bhij,bjhe->bihe', inner, vc)
    q_decay = jnp.exp(log_gamma[None, :] * (i[:, None] + 1.0))
    out = out + jnp.einsum('bihd,bhde->bihe', qc, state) * q_decay[None, :, :, None]
    k_decay = jnp.exp(log_gamma[None, :] * (L - 1.0 - i[:, None]))
    state = state * jnp.exp(log_gamma * L)[None, :, None, None] + jnp.einsum('bjhd,jh,bjhe->bhde', kc, k_decay, vc)
    return out, state


def retention(q, k, v):
    bsz, t_len, nh, dk = q.shape
    dv = v.shape[-1]
    log_gamma = jnp.log1p(-jnp.exp2(-5.0 - jnp.arange(nh, dtype=jnp.float32)))
    qf = q.astype(jnp.float32)
    kf = k.astype(jnp.float32) * (dk ** -0.5)
    vf = v.astype(jnp.float32)
    state0 = jnp.zeros((bsz, nh, dk, dv), jnp.float32)
    out_meta, state = _retention_chunk(qf[:, :N_META], kf[:, :N_META], vf[:, :N_META], state0, log_gamma)
    nc = (t_len - N_META) // RET_CHUNK

    def to_chunks(a):
        return jnp.moveaxis(a[:, N_META:].reshape(bsz, nc, RET_CHUNK, nh, a.shape[-1]), 1, 0)

    def step(s, xs):
        qc, kc, vc = xs
        o, s = _retention_chunk(qc, kc, vc, s, log_gamma)
        return s, o

    _, outs = lax.scan(step, state, (to_chunks(qf), to_chunks(kf), to_chunks(vf)))
    out_real = jnp.moveaxis(outs, 0, 1).reshape(bsz, nc * RET_CHUNK, nh, dv)
    return jnp.concatenate([out_meta, out_real], axis=1)


def setup_inputs(seed: int = 0) -> dict:
    key = jax.random.key(seed)
    ks = jax.random.split(key, 16)

    def nrm(k, shape, scale):
        return jax.random.normal(k, shape, jnp.float32) * scale

    return {
        'x': nrm(ks[0], (BATCH, SEQ, D_MODEL), 1.0),
        'meta': nrm(ks[1], (N_META, D_MODEL), 1.0),
        'rel_bias': nrm(ks[2], (REL_BUCKETS, A_HEADS + B_HEADS), 0.5),
        'w_in': nrm(ks[3], (DEPTH, D_MODEL, IN_COLS), D_MODEL ** -0.5),
        'norm_mix': 1.0 + nrm(ks[4], (DEPTH, D_MODEL), 0.01),
        'diff_lambda': nrm(ks[5], (DEPTH, 4, B_HALF), 0.1),
        'diff_norm': 1.0 + nrm(ks[6], (DEPTH, B_HEAD_DIM), 0.01),
        'ret_norm': 1.0 + nrm(ks[7], (DEPTH, C_W), 0.01),
        'w_out': nrm(ks[8], (DEPTH, D_MODEL, D_MODEL), 0.5 * D_MODEL ** -0.5),
        'norm_ff': 1.0 + nrm(ks[9], (DEPTH, D_MODEL), 0.01),
        'w_ff1': nrm(ks[10], (DEPTH, D_MODEL, D_FF), D_MODEL ** -0.5),
        'w_ff2': nrm(ks[11], (DEPTH, D_FF, D_MODEL), 0.5 * D_FF ** -0.5),
        'final_norm': 1.0 + nrm(ks[12], (D_MODEL,), 0.01),
    }


def reference(x, meta, rel_bias, w_in, norm_mix, diff_lambda, diff_norm, ret_norm, w_out, norm_ff, w_ff1, w_ff2, final_norm):
    bsz, s_len, _ = x.shape
    t_len = s_len + N_META
    topk = min(TOPK_MAX, s_len // 4)
    h = jnp.concatenate([jnp.broadcast_to(meta.astype(x.dtype)[None], (bsz, N_META, D_MODEL)), x], axis=1)
    pos = jnp.arange(t_len)
    split_pts = _split_points()
    bias_a = rel_bias[:, :A_HEADS]
    bias_b = rel_bias[:, A_HEADS:]
    for l in range(DEPTH):
        u = rmsnorm(h, norm_mix[l])
        proj = u @ w_in[l]
        (qa, ka, va, qi, ki, wi, qb, kb, vb, qc, kc, vc, gc) = jnp.split(proj, split_pts, axis=-1)

        a_out = sparse_indexer_attention(
            qa.reshape(bsz, t_len, A_HEADS, A_HEAD_DIM),
            ka.reshape(bsz, t_len, A_HEADS, A_HEAD_DIM),
            va.reshape(bsz, t_len, A_HEADS, A_HEAD_DIM),
            qi.reshape(bsz, t_len, IDX_HEADS, IDX_DIM), ki, wi, bias_a, topk,
        ).reshape(bsz, t_len, A_W)

        lambda_init = 0.8 - 0.6 * math.exp(-0.3 * l)
        lp = diff_lambda[l].astype(jnp.float32)
        lam = jnp.exp(jnp.sum(lp[0] * lp[1])) - jnp.exp(jnp.sum(lp[2] * lp[3])) + lambda_init
        b_heads = diff_attention(
            qb.reshape(bsz, t_len, B_HEADS, 2, B_HALF),
            kb.reshape(bsz, t_len, B_HEADS, 2, B_HALF),
            vb.reshape(bsz, t_len, B_HEADS, B_HEAD_DIM), bias_b, lam,
        )
        b_out = (rmsnorm(b_heads, diff_norm[l]) * (1.0 - lambda_init)).reshape(bsz, t_len, B_W)

        qcr = rotary(qc.reshape(bsz, t_len, C_HEADS, C_HEAD_DIM), pos)
        kcr = rotary(kc.reshape(bsz, t_len, C_HEADS, C_HEAD_DIM), pos)
        ret = retention(qcr, kcr, vc.reshape(bsz, t_len, C_HEADS, C_HEAD_DIM)).astype(h.dtype)
        ret = rmsnorm(ret, ret_norm[l].reshape(C_HEADS, C_HEAD_DIM)).reshape(bsz, t_len, C_W)
        c_out = jax.nn.silu(gc) * ret

        h = h + jnp.concatenate([a_out, b_out, c_out], axis=-1) @ w_out[l]
        u = rmsnorm(h, norm_ff[l])
        h = h + jnp.square(jax.nn.relu(u @ w_ff1[l])) @ w_ff2[l]
    return rmsnorm(h, final_norm)[:, N_META:]
```

```python
import contextlib
import os
import numpy as np
import concourse.bass as bass
import concourse.mybir as mybir
from concourse.bass_utils import run_bass_kernel_spmd

F32 = mybir.dt.float32
BF16 = mybir.dt.bfloat16
U32 = mybir.dt.uint32
AF = mybir.ActivationFunctionType
ALU = mybir.AluOpType
AX = mybir.AxisListType

NSLOT = 24


class Prog:
    def __init__(self):
        self.nc = bass.Bass("TRN2", target_bir_lowering=False)
        self.st = contextlib.ExitStack()
        self.ops = []
        self.engs = {"pe": self.nc.tensor, "act": self.nc.scalar, "dve": self.nc.vector,
                     "pool": self.nc.gpsimd, "sp": self.nc.sync}
        self.nps = 0

    def dram(self, name, shape, dt, kind):
        return self.nc.dram_tensor(name, list(shape), dt, kind=kind).ap()

    def sb(self, name, shape, dt):
        return self.st.enter_context(self.nc.sbuf_tensor("s_" + name, list(shape), dt))

    def ps(self, name, shape=(128, 512), dt=F32):
        return self.st.enter_context(self.nc.psum_tensor("p_" + name, list(shape), dt))

    def op(self, eng, name, kw, r=(), w=()):
        def fn(e, name=name, kw=kw):
            return getattr(e, name)(**kw)
        self.ops.append((eng, fn, tuple(r), tuple(w), None))

    def dma(self, out, in_, r=(), w=(), q="sp", **kw):
        def fn(e, out=out, in_=in_, kw=kw):
            return e.dma_start(out=out, in_=in_, **kw)
        self.ops.append((q, fn, tuple(r), tuple(w), "dma"))

    def finish(self):
        nc = self.nc
        ops = self.ops
        n = len(ops)
        lastw = {}
        readers = {}
        deps = [None] * n
        hasdep = [False] * n
        def excl(k):
            return isinstance(k, tuple) and isinstance(k[0], str) and (k[0].startswith("ps") or k[0] in ("OA", "OB"))
        for i, (eng, fn, r, w, kind) in enumerate(ops):
            w = tuple(w) + tuple(k for k in r if excl(k) and k not in w)
            d = set()
            for k in r:
                if k in lastw:
                    d.add(lastw[k])
            for k in w:
                if k in lastw:
                    d.add(lastw[k])
                for x in readers.get(k, ()):
                    d.add(x)
            d.discard(i)
            dd = []
            for x in d:
                if kind is None and eng == "pe" and ops[x][0] == "pe" and ops[x][4] is None:
                    continue
                dd.append(x)
                hasdep[x] = True
            deps[i] = dd
            for k in w:
                lastw[k] = i
                readers[k] = []
            for k in r:
                readers.setdefault(k, []).append(i)
        sems = {}
        for e in ("pe", "act", "dve", "pool"):
            sems[e] = self.st.enter_context(nc.semaphore("s_" + e))
        slots = {}
        for q in ("sp", "act", "pool"):
            slots[q] = [self.st.enter_context(nc.semaphore("d_%s_%d" % (q, s))) for s in range(NSLOT)]
        slotval = {q: [0] * NSLOT for q in slots}
        slotnext = {q: 0 for q in slots}
        cnt = {e: 0 for e in sems}
        tok = [None] * n
        waited = {e: {} for e in self.engs}
        for i, (eng, fn, r, w, kind) in enumerate(ops):
            E = self.engs[eng]
            need = {}
            semobj = {}
            for x in deps[i]:
                s, v = tok[x]
                if need.get(id(s), 0) < v:
                    need[id(s)] = v
                    semobj[id(s)] = s
            if kind == "dma":
                s_i = slotnext[eng]
                slotnext[eng] = (s_i + 1) % NSLOT
                s = slots[eng][s_i]
                pv = slotval[eng][s_i]
                if pv > 0 and need.get(id(s), 0) < pv:
                    need[id(s)] = pv
                    semobj[id(s)] = s
            for sid, v in need.items():
                if waited[eng].get(sid, 0) >= v:
                    continue
                E.wait_ge(semobj[sid], v)
                waited[eng][sid] = v
            ins = fn(E)
            if kind == "dma":
                slotval[eng][s_i] += 16
                ins.then_inc(s, 16)
                tok[i] = (s, slotval[eng][s_i])
            elif hasdep[i]:
                cnt[eng] += 1
                ins.then_inc(sems[eng], 1)
                tok[i] = (sems[eng], cnt[eng])
        for q in slots:
            for s_i in range(NSLOT):
                if slotval[q][s_i] > 0:
                    nc.sync.wait_ge(slots[q][s_i], slotval[q][s_i])
        for e in sems:
            if cnt[e] > 0:
                nc.sync.wait_ge(sems[e], cnt[e])
        self.st.close()
        return nc


def run(prog_nc, in_maps):
    res = run_bass_kernel_spmd(prog_nc, in_maps, core_ids=list(range(len(in_maps))))
    return res.results


NT = 4112
NJ = 32
TALL = 16400
EPS = 1e-6
_OFF = dict(qa=0, ka=512, va=1024, qi=1536, ki=2048, wi=2112, qb=2120, kb=2376, vb=2632,
            qc=2888, kc=3144, vc=3400, gc=3656)
_SZ = dict(qa=512, ka=512, va=512, qi=512, ki=64, wi=8, qb=256, kb=256, vb=256, qc=256,
           kc=256, vc=256, gc=256)
FM_ORDER = ["qa", "ka", "qi", "qb", "kb", "ki"]
TM_ORDER = ["va", "vb", "vc", "qc", "kc", "gc", "wi"]
FM_OFF = {}
_o = 0
for _n in FM_ORDER:
    FM_OFF[_n] = _o
    _o += _SZ[_n]
NFM = _o
TM_OFF = {}
_o = 0
for _n in TM_ORDER:
    TM_OFF[_n] = _o
    _o += _SZ[_n]
NTM = _o
W_PERM = np.concatenate([np.arange(_OFF[n], _OFF[n] + _SZ[n]) for n in FM_ORDER + TM_ORDER])


def tiles_of(ntiles):
    t = [(0, 16)]
    for i in range(8):
        t.append((16 + 512 * i, 512))
    return t[:ntiles]


def emit_rmsnorm_T(P, tag, b, ht, uT, ntok, g_sb, sq, ps_ss, rstd, sd, ones_bf, eps_t, htkey="ht"):
    P.op("act", "activation", dict(out=sq[:, :, :ntok], in_=ht[:, :, :ntok], func=AF.Square),
         r=[(htkey, b)], w=[("sq",)])
    for k in range(8):
        P.op("pe", "matmul", dict(out=ps_ss[:, :ntok], lhsT=ones_bf[:, :], rhs=sq[:, k, :ntok],
                                  start=(k == 0), stop=(k == 7)),
             r=[("sq",), ("ones",)], w=[("ps_ss",)])
    P.op("act", "activation", dict(out=sd[:, :ntok], in_=ps_ss[:, :ntok], func=AF.Sqrt,
                                   bias=eps_t[:, 0:1], scale=1.0 / 1024.0),
         r=[("ps_ss",), ("eps",)], w=[("sd",)])
    P.op("dve", "reciprocal", dict(out=rstd[:, :ntok], in_=sd[:, :ntok]), r=[("sd",)], w=[("rstd",)])
    for k in range(8):
        P.op("dve", "scalar_tensor_tensor", dict(out=uT[:, k, :ntok], in0=ht[:, k, :ntok],
                                                 scalar=g_sb[:, k:k + 1], in1=rstd[:, :ntok],
                                                 op0=ALU.mult, op1=ALU.mult),
             r=[(htkey, b), ("rstd",), ("g",)], w=[(tag, b)])


def build_p1(ntiles=9):
    P = Prog()
    hT = P.dram("hT", [1024, NT], F32, "ExternalInput")
    w = P.dram("w", [1024, 3912], F32, "ExternalInput")
    g = P.dram("g", [128, 8], F32, "ExternalInput")
    cs = P.dram("cs", [NT, 512], F32, "ExternalInput")
    fmo = P.dram("fmo", [NFM, NT], BF16, "ExternalOutput")
    tmo = P.dram("tmo", [NT, NTM], F32, "ExternalOutput")
    tmb = P.dram("tmb", [NT, 780], BF16, "ExternalOutput")
    tbs = [P.sb("tb%d" % i, [128, 12, 65], BF16) for i in range(2)]
    w_sb = P.sb("w_sb", [128, 8, 3912], BF16)
    g_sb = P.sb("g_sb", [128, 8], F32)
    ones_bf = P.sb("ones_bf", [128, 128], BF16)
    eps_t = P.sb("eps_t", [128, 1], F32)
    hts = [P.sb("ht%d" % i, [128, 8, 512], F32) for i in range(2)]
    uTs = [P.sb("uT%d" % i, [128, 8, 512], BF16) for i in range(2)]
    sq = P.sb("sq", [128, 8, 512], BF16)
    sd = P.sb("sd", [128, 512], F32)
    rstd = P.sb("rstd", [128, 512], F32)
    fms = [P.sb("fm%d" % i, [128, 512], BF16) for i in range(4)]
    tms = [P.sb("tm%d" % i, [128, NTM], F32) for i in range(2)]
    css = [P.sb("cs%d" % i, [128, 512], F32) for i in range(2)]
    rt = P.sb("rt", [128, 4, 256], F32)
    ps_ss = P.ps("ps_ss")
    pss = [P.ps("ps%d" % i) for i in range(6)]

    hTv = hT.rearrange("(k p) t -> p k t", p=128)
    wv = w.rearrange("(k p) c -> p k c", p=128)
    P.op("dve", "memset", dict(ap=ones_bf[:, :], constant=1.0), w=[("ones",)])
    P.op("dve", "memset", dict(ap=eps_t[:, :], constant=EPS), w=[("eps",)])
    P.dma(g_sb[:, :], g[:, :], w=[("g",)])
    for i in range(2):
        P.op("dve", "memset", dict(ap=tbs[i][:, :, :], constant=1.0), w=[("tb", i, 0), ("tb", i, 1)])
    for k in range(8):
        P.dma(w_sb[:, k, :], wv[:, k, :], w=[("w", k)], q="pool")
    scale_of = {"qa": 0.125, "qb": 32.0 ** -0.5}
    fm_groups = []
    for n in FM_ORDER:
        for r0 in range(0, _SZ[n], 128):
            fm_groups.append((FM_OFF[n] + r0, min(128, _SZ[n] - r0), scale_of.get(n, 1.0)))
    tm_groups = [(0, 512), (512, 512), (1024, 512), (1536, 264)]
    pcount = [0]
    fcount = [0]
    sbcount = [0]

    def nextps():
        i = pcount[0] % 6
        pcount[0] += 1
        return i

    for ti, (tok0, ntok) in enumerate(tiles_of(ntiles)):
        b = ti % 2
        ht, uT = hts[b], uTs[b]
        P.dma(ht[:, :, :ntok], hTv[:, :, tok0:tok0 + ntok], w=[("ht", b)])
        emit_rmsnorm_T(P, "uT", b, ht, uT, ntok, g_sb, sq, ps_ss, rstd, sd, ones_bf, eps_t)
        for (r0, M, sc) in fm_groups:
            pi = nextps()
            ps = pss[pi]
            for k in range(8):
                P.op("pe", "matmul", dict(out=ps[:M, :ntok], lhsT=w_sb[:, k, r0:r0 + M], rhs=uT[:, k, :ntok],
                                          start=(k == 0), stop=(k == 7)),
                     r=[("uT", b), ("w", k)], w=[("ps", pi)])
            fi = fcount[0] % 4
            fcount[0] += 1
            fm = fms[fi]
            P.op("act", "activation", dict(out=fm[:M, :ntok], in_=ps[:M, :ntok], func=AF.Copy, scale=sc),
                 r=[("ps", pi)], w=[("fm", fi)])
            P.dma(fmo[r0:r0 + M, tok0:tok0 + ntok], fm[:M, :ntok], r=[("fm", fi)])
        nsb = max(1, ntok // 128)
        for s in range(nsb):
            nt = min(128, ntok)
            t0 = tok0 + s * 128
            si = sbcount[0] % 2
            sbcount[0] += 1
            tm, cst = tms[si], css[si]
            P.dma(cst[:nt, :], cs[t0:t0 + nt, :], w=[("cs", si)])
            for gi, (c0, ncol) in enumerate(tm_groups):
                pi = nextps()
                ps = pss[pi]
                for k in range(8):
                    P.op("pe", "matmul", dict(out=ps[:nt, :ncol], lhsT=uT[:, k, s * 128:s * 128 + nt],
                                              rhs=w_sb[:, k, NFM + c0:NFM + c0 + ncol],
                                              start=(k == 0), stop=(k == 7)),
                         r=[("uT", b), ("w", k)], w=[("ps", pi)])
                if gi == 0 or gi == 3:
                    P.op("act", "activation", dict(out=tm[:nt, c0:c0 + ncol], in_=ps[:nt, :ncol], func=AF.Copy),
                         r=[("ps", pi)], w=[("tm", si, gi)])
                    if gi == 0:
                        P.op("dve", "tensor_copy", dict(out=tbs[si][:nt, 0:8, 0:64],
                                                        in_=ps[:nt, 0:512].rearrange("p (h d) -> p h d", d=64)),
                             r=[("ps", pi)], w=[("tb", si, 0)])
                elif gi == 1:
                    P.op("dve", "tensor_copy", dict(out=tm[:nt, c0:c0 + ncol], in_=ps[:nt, :ncol]),
                         r=[("ps", pi)], w=[("tm", si, gi)])
                    P.op("act", "activation", dict(out=tbs[si][:nt, 8:12, 0:64],
                                                   in_=ps[:nt, 0:256].rearrange("p (h d) -> p h d", d=64), func=AF.Copy),
                         r=[("ps", pi)], w=[("tb", si, 1)])
                else:
                    psv = ps[:nt, 0:512].rearrange("p (h d) -> p h d", d=64)
                    x1, x2 = psv[:, :, 0:32], psv[:, :, 32:64]
                    cosv = cst[:nt, 0:256].rearrange("p (h d) -> p h d", d=32)
                    sinv = cst[:nt, 256:512].rearrange("p (h d) -> p h d", d=32)
                    tv = [rt[:nt, i, :].rearrange("p (h d) -> p h d", d=32) for i in range(4)]
                    ov = tm[:nt, 1024:1536].rearrange("p (h d) -> p h d", d=64)
                    for i, (a_, b_) in enumerate([(x1, cosv), (x2, sinv), (x1, sinv), (x2, cosv)]):
                        P.op("dve", "tensor_tensor", dict(out=tv[i], in0=a_, in1=b_, op=ALU.mult),
                             r=[("ps", pi), ("cs", si)], w=[("rt", i)])
                    P.op("pool", "tensor_tensor", dict(out=ov[:, :, 0:32], in0=tv[0], in1=tv[1], op=ALU.subtract),
                         r=[("rt", 0), ("rt", 1)], w=[("tm", si, gi, 0)])
                    P.op("pool", "tensor_tensor", dict(out=ov[:, :, 32:64], in0=tv[2], in1=tv[3], op=ALU.add),
                         r=[("rt", 2), ("rt", 3)], w=[("tm", si, gi, 1)])
            P.dma(tmo[t0:t0 + nt, :], tm[:nt, :],
                  r=[("tm", si, 0), ("tm", si, 1), ("tm", si, 2, 0), ("tm", si, 2, 1), ("tm", si, 3)])
            P.dma(tmb[t0:t0 + nt, :], tbs[si][:nt, :, :].rearrange("p h f -> p (h f)"),
                  r=[("tb", si, 0), ("tb", si, 1)])
    return P.finish()


def tiles256(ntiles):
    t = [(0, 16)]
    for i in range(16):
        t.append((16 + 256 * i, 256))
    return t[:ntiles]


def build_p3(ntiles=17, final=False):
    P = Prog()
    hT = P.dram("hT", [1024, NT], F32, "ExternalInput")
    mxT = P.dram("mxT", [1024, NT], BF16, "ExternalInput")
    wo = P.dram("wo", [1024, 1024], F32, "ExternalInput")
    w1 = P.dram("w1", [1024, 4096], F32, "ExternalInput")
    w2 = P.dram("w2", [4096, 1024], F32, "ExternalInput")
    g = P.dram("g", [128, 8], F32, "ExternalInput")
    gf = P.dram("gf", [128, 8], F32, "ExternalInput")
    hoT = P.dram("hoT", [1024, NT], F32, "ExternalOutput")
    wo_sb = P.sb("wo_sb", [128, 8, 1024], BF16)
    w1_sb = P.sb("w1_sb", [128, 8, 4096], BF16)
    w2_sb = P.sb("w2_sb", [128, 32, 1024], BF16)
    g_sb = P.sb("g_sb", [128, 8], F32)
    gf_sb = P.sb("gf_sb", [128, 8], F32)
    ones_bf = P.sb("ones_bf", [128, 128], BF16)
    eps_t = P.sb("eps_t", [128, 1], F32)
    ht = P.sb("ht", [128, 8, 256], F32)
    mx = P.sb("mx", [128, 8, 256], BF16)
    uT = P.sb("uT", [128, 8, 256], BF16)
    sq = P.sb("sq", [128, 8, 256], BF16)
    hid = P.sb("hid", [128, 32, 256], BF16)
    sd = P.sb("sd", [128, 256], F32)
    rstd = P.sb("rstd", [128, 256], F32)
    rl = [P.sb("rl%d" % i, [128, 256], F32) for i in range(2)]
    ps_ss = P.ps("ps_ss")
    pss = [P.ps("ps%d" % i) for i in range(6)]
    hTv = hT.rearrange("(k p) t -> p k t", p=128)
    hoTv = hoT.rearrange("(k p) t -> p k t", p=128)
    mxv = mxT.rearrange("(k p) t -> p k t", p=128)
    P.op("dve", "memset", dict(ap=ones_bf[:, :], constant=1.0), w=[("ones",)])
    P.op("dve", "memset", dict(ap=eps_t[:, :], constant=EPS), w=[("eps",)])
    P.dma(g_sb[:, :], g[:, :], w=[("g",)])
    P.dma(gf_sb[:, :], gf[:, :], w=[("gf",)])
    wov = wo.rearrange("(k p) c -> p k c", p=128)
    w1v = w1.rearrange("(k p) c -> p k c", p=128)
    w2v = w2.rearrange("(f p) c -> p f c", p=128)
    for k in range(8):
        P.dma(wo_sb[:, k, :], wov[:, k, :], w=[("wo", k)], q="pool")
    for k in range(8):
        P.dma(w1_sb[:, k, :], w1v[:, k, :], w=[("w1", k)], q="pool")
    for f in range(0, 32, 4):
        P.dma(w2_sb[:, f:f + 4, :], w2v[:, f:f + 4, :], w=[("w2", f // 4)], q="pool")
    pc = [0]

    def nextps():
        i = pc[0] % 6
        pc[0] += 1
        return i

    rc = 0
    for ti, (tok0, ntok) in enumerate(tiles256(ntiles)):
        P.dma(ht[:, :, :ntok], hTv[:, :, tok0:tok0 + ntok], w=[("ht", 0)])
        P.dma(mx[:, :, :ntok], mxv[:, :, tok0:tok0 + ntok], w=[("mx",)])
        for m in range(8):
            pi = nextps()
            ps = pss[pi]
            for k in range(8):
                P.op("pe", "matmul", dict(out=ps[:, :ntok], lhsT=wo_sb[:, k, m * 128:(m + 1) * 128], rhs=mx[:, k, :ntok],
                                          start=(k == 0), stop=(k == 7)),
                     r=[("mx",), ("wo", k)], w=[("ps", pi)])
            P.op("dve", "tensor_tensor", dict(out=ht[:, m, :ntok], in0=ht[:, m, :ntok], in1=ps[:, :ntok], op=ALU.add),
                 r=[("ps", pi), ("ht", 0)], w=[("ht", 0)])
        emit_rmsnorm_T(P, "uT", 0, ht, uT, ntok, g_sb, sq, ps_ss, rstd, sd, ones_bf, eps_t)
        for f in range(32):
            pi = nextps()
            ps = pss[pi]
            for k in range(8):
                P.op("pe", "matmul", dict(out=ps[:, :ntok], lhsT=w1_sb[:, k, f * 128:(f + 1) * 128], rhs=uT[:, k, :ntok],
                                          start=(k == 0), stop=(k == 7)),
                     r=[("uT", 0), ("w1", k)], w=[("ps", pi)])
            ri = rc % 2
            rc += 1
            P.op("act", "activation", dict(out=rl[ri][:, :ntok], in_=ps[:, :ntok], func=AF.Relu),
                 r=[("ps", pi)], w=[("rl", ri)])
            P.op("pool", "tensor_tensor", dict(out=hid[:, f, :ntok], in0=rl[ri][:, :ntok], in1=rl[ri][:, :ntok], op=ALU.mult),
                 r=[("rl", ri)], w=[("hid", f)])
        for m in range(8):
            pi = nextps()
            ps = pss[pi]
            for f in range(32):
                P.op("pe", "matmul", dict(out=ps[:, :ntok], lhsT=w2_sb[:, f, m * 128:(m + 1) * 128], rhs=hid[:, f, :ntok],
                                          start=(f == 0), stop=(f == 31)),
                     r=[("hid", f), ("w2", f // 4)], w=[("ps", pi)])
            P.op("dve", "tensor_tensor", dict(out=ht[:, m, :ntok], in0=ht[:, m, :ntok], in1=ps[:, :ntok], op=ALU.add),
                 r=[("ps", pi), ("ht", 0)], w=[("ht", 0)])
        if final:
            P.op("act", "activation", dict(out=sq[:, :, :ntok], in_=ht[:, :, :ntok], func=AF.Square),
                 r=[("ht", 0)], w=[("sq",)])
            for k in range(8):
                P.op("pe", "matmul", dict(out=ps_ss[:, :ntok], lhsT=ones_bf[:, :], rhs=sq[:, k, :ntok],
                                          start=(k == 0), stop=(k == 7)),
                     r=[("sq",), ("ones",)], w=[("ps_ss",)])
            P.op("act", "activation", dict(out=sd[:, :ntok], in_=ps_ss[:, :ntok], func=AF.Sqrt,
                                           bias=eps_t[:, 0:1], scale=1.0 / 1024.0),
                 r=[("ps_ss",), ("eps",)], w=[("sd",)])
            P.op("dve", "reciprocal", dict(out=rstd[:, :ntok], in_=sd[:, :ntok]), r=[("sd",)], w=[("rstd",)])
            for k in range(8):
                P.op("dve", "scalar_tensor_tensor", dict(out=ht[:, k, :ntok], in0=ht[:, k, :ntok],
                                                         scalar=gf_sb[:, k:k + 1], in1=rstd[:, :ntok],
                                                         op0=ALU.mult, op1=ALU.mult),
                     r=[("ht", 0), ("rstd",), ("gf",)], w=[("ht", 0)])
        P.dma(hoTv[:, :, tok0:tok0 + ntok], ht[:, :, :ntok], r=[("ht", 0)])
    return P.finish()


NBIS = 24
BIG = 30000.0
C_IDX = (8 ** -0.5) * (64 ** -0.5)


def build_p2ab(njobs=NJ, do_meta=True, lambda_init=0.2, nbis=NBIS):
    P = Prog()
    D = P.dram
    kaT = D("kaT", [512, TALL], BF16, "ExternalInput")
    kbT = D("kbT", [256, TALL], BF16, "ExternalInput")
    kiT2 = D("kiT2", [128, TALL], BF16, "ExternalInput")
    va = D("va", [TALL, 520], BF16, "ExternalInput")
    vb = D("vb", [TALL, 260], BF16, "ExternalInput")
    qaz = D("qaz", [8, 128, NT], BF16, "ExternalInput")
    qbz = D("qbz", [8, 128, NT], BF16, "ExternalInput")
    qiz = D("qiz", [8, 128, NT], BF16, "ExternalInput")
    wi = D("wi", [NT, 8], F32, "ExternalInput")
    dmask = D("dmask", [128, 512], F32, "ExternalInput")
    nbd = D("nb", [12, 128, 1024], BF16, "ExternalInput")
    nbm0 = D("nbm0", [12, 128, 16], BF16, "ExternalInput")
    nbmm = D("nbmm", [12, 16, 16], BF16, "ExternalInput")
    b31 = D("b31", [128, 12], F32, "ExternalInput")
    ident = D("ident", [128, 128], BF16, "ExternalInput")
    lamb = D("lamb", [128, 128], F32, "ExternalInput")
    dnb = D("dnb", [128, 64], F32, "ExternalInput")
    ab = D("ab", [NT, 768], BF16, "ExternalOutput")

    S = P.sb
    acc = S("acc", [128, TALL], F32)
    MB = S("MB", [128, TALL], BF16)
    MBN = S("MBN", [128, 8, 1024], BF16)
    MBNm = S("MBNm", [128, 8, 16], BF16)
    NB = S("NB", [128, 12, 1024], BF16)
    NBm0 = S("NBm0", [128, 12, 16], BF16)
    NBmm = S("NBmm", [16, 12, 16], BF16)
    dm = S("dm", [128, 512], F32)
    dtmp = S("dtmp", [128, 512], F32)
    b31s = S("b31s", [128, 12], F32)
    zcol = S("zcol", [128, 1], F32)
    half = S("half", [128, 1], F32)
    idn = S("idn", [128, 128], BF16)
    lam_in = S("lam_in", [128, 128], F32)
    dn = S("dn", [128, 64], F32)
    kis = [S("ki%d" % i, [128, 512], BF16) for i in range(2)]
    kas = [S("ka%d" % i, [128, 4, 512], BF16) for i in range(2)]
    kbs = [S("kb%d" % i, [128, 2, 512], BF16) for i in range(2)]
    vas = [S("va%d" % i, [128, 4, 520], BF16) for i in range(2)]
    vbs = [S("vb%d" % i, [128, 4, 260], BF16) for i in range(2)]
    qa = S("qa", [128, 8, 128], BF16)
    qb = S("qb", [128, 8, 128], BF16)
    qi = S("qi", [128, 8, 128], BF16)
    wis = S("wis", [128, 8], F32)
    absw = S("absw", [128, 8], F32)
    sgn = S("sgn", [128, 8], F32)
    rbuf = [S("r%d" % i, [128, 512], F32) for i in range(3)]
    pTs = [S("pT%d" % i, [128, 512], BF16) for i in range(3)]
    sm = S("sm", [128, 32], F32)
    smu = S("smu", [128, 4], U32)
    abo = S("abo", [128, 768], BF16)
    fin = S("fin", [128, 512], F32)
    psI = [P.ps("psI%d" % i) for i in range(2)]
    psS = [P.ps("psS%d" % i) for i in range(2)]
    OA = [P.ps("OA%d" % i) for i in range(2)]
    OB = [P.ps("OB%d" % i) for i in range(2)]
    LO, HI, MID, CNT, RMIN, RTMP, LAM, NLAM = 0, 1, 2, 3, 4, 5, 6, 7

    def smc(i):
        return sm[:, i:i + 1]

    P.dma(dm[:, :], dmask[:, :], w=[("dm",)])
    P.dma(NB[:, :, :], nbd.rearrange("h p k -> p h k"), w=[("NB",)])
    P.dma(NBm0[:, :, :], nbm0.rearrange("h p k -> p h k"), w=[("NBm0",)])
    P.dma(NBmm[:, :, :], nbmm.rearrange("h p k -> p h k"), w=[("NBmm",)])
    P.dma(b31s[:, :], b31[:, :], w=[("b31",)])
    P.dma(idn[:, :], ident[:, :], w=[("idn",)])
    P.dma(lam_in[:, :], lamb[:, :], w=[("lam_in",)])
    P.dma(dn[:, :], dnb[:, :], w=[("dn",)])
    P.op("dve", "memset", dict(ap=zcol[:, :], constant=0.0), w=[("zcol",)])
    P.op("dve", "memset", dict(ap=half[:, :], constant=0.5), w=[("half",)])
    P.op("dve", "tensor_tensor", dict(out=fin[:, 0:32], in0=lam_in[:, 0:32], in1=lam_in[:, 32:64], op=ALU.mult),
         r=[("lam_in",)], w=[("fin",)])
    P.op("dve", "tensor_tensor", dict(out=fin[:, 32:64], in0=lam_in[:, 64:96], in1=lam_in[:, 96:128], op=ALU.mult),
         r=[("lam_in",)], w=[("fin",)])
    P.op("dve", "tensor_reduce", dict(out=sm[:, 8:10], in_=fin[:, 0:64].rearrange("p (a b) -> p a b", b=32),
                                      axis=AX.X, op=ALU.add), r=[("fin",)], w=[("sm", "l")])
    P.op("act", "activation", dict(out=sm[:, 10:12], in_=sm[:, 8:10], func=AF.Exp), r=[("sm", "l")], w=[("sm", "l2")])
    P.op("dve", "tensor_tensor", dict(out=smc(LAM), in0=sm[:, 11:12], in1=sm[:, 10:11], op=ALU.subtract),
         r=[("sm", "l2")], w=[("sm", "lam")])
    P.op("dve", "tensor_scalar", dict(out=smc(NLAM), in0=smc(LAM), scalar1=-float(lambda_init), scalar2=None,
                                      op0=ALU.add), r=[("sm", "lam")], w=[("sm", "nlam")])
    P.op("dve", "tensor_scalar", dict(out=dn[:, :], in0=dn[:, :], scalar1=float(1.0 - lambda_init), scalar2=None,
                                      op0=ALU.mult), r=[("dn",)], w=[("dn",)])

    tcount = [0]
    rcount = [0]
    pcount = [0]
    scount = [0]
    icount = [0]

    def attend(nq, tiles, meta_mode, j):
        for i in range(2):
            P.op("dve", "memset", dict(ap=OA[i][:nq, 0:260], constant=0.0), w=[("OA", i)])
            P.op("dve", "memset", dict(ap=OB[i][:nq, 0:260], constant=0.0), w=[("OB", i)])
        steps = [("t", g, near) for (g, near) in tiles] + [("m", None, None)]
        for (kind, g, near) in steps:
            tb = tcount[0] % 2
            tcount[0] += 1
            ka_t, kb_t, va_t, vb_t = kas[tb], kbs[tb], vas[tb], vbs[tb]
            if kind == "t":
                k0 = 16 + 512 * g
                nkb, nk = 4, 128
                P.dma(ka_t[:, :, :], kaT[:, k0:k0 + 512].rearrange("(c p) t -> p c t", p=128), w=[("ka", tb)])
                P.dma(kb_t[:, :, :], kbT[:, k0:k0 + 512].rearrange("(c p) t -> p c t", p=128), w=[("kb", tb)])
                P.dma(va_t[:, :, :], va[k0:k0 + 512, :].rearrange("(b p) f -> p b f", p=128), w=[("va", tb)])
                P.dma(vb_t[:, :, :], vb[k0:k0 + 512, :].rearrange("(b p) f -> p b f", p=128), w=[("vb", tb)])
            else:
                nkb, nk = 1, 16
                P.dma(ka_t[:, :, 0:16], kaT[:, 0:16].rearrange("(c p) t -> p c t", p=128), w=[("ka", tb)])
                P.dma(kb_t[:, :, 0:16], kbT[:, 0:16].rearrange("(c p) t -> p c t", p=128), w=[("kb", tb)])
                P.dma(va_t[0:16, 0, :], va[0:16, :], w=[("va", tb)])
                P.dma(vb_t[0:16, 0, :], vb[0:16, :], w=[("vb", tb)])
            for hh in range(16):
                isA = hh < 8
                if isA:
                    h = hh
                    qz = qa[:, h, :nq]
                    kop = lambda blk, h=h: ka_t[:, h // 2, blk * 128:blk * 128 + nk]
                    kkey, qkey, vkey = ("ka", tb), ("qa",), ("va", tb)
                    vop = lambda blk, h=h: va_t[:nk, blk, h * 65:(h + 1) * 65]
                    Oap = OA[h // 4][:nq, (h % 4) * 65:(h % 4 + 1) * 65]
                    Okey = ("OA", h // 4)
                    hb = h
                else:
                    hc = hh - 8
                    h, c = hc // 2, hc % 2
                    qz = qb[:, hc, :nq]
                    kop = lambda blk, h=h: kb_t[:, h // 2, blk * 128:blk * 128 + nk]
                    kkey, qkey, vkey = ("kb", tb), ("qb",), ("vb", tb)
                    vop = lambda blk, h=h: vb_t[:nk, blk, h * 65:(h + 1) * 65]
                    Oap = OB[hc // 4][:nq, (hc % 4) * 65:(hc % 4 + 1) * 65]
                    Okey = ("OB", hc // 4)
                    hb = 8 + h
                si = scount[0] % 2
                scount[0] += 1
                ps = psS[si]
                def mop(blk):
                    if kind == "t":
                        if near is not None:
                            if isA:
                                return MBN[:nq, h, near * 512 + blk * 128: near * 512 + (blk + 1) * 128], ("MBN", h)
                            return NB[:nq, hb, near * 512 + blk * 128: near * 512 + (blk + 1) * 128], ("NB",)
                        if isA:
                            return MB[:nq, g * 512 + blk * 128: g * 512 + (blk + 1) * 128], ("MB",)
                        return None, None
                    if meta_mode == "mm":
                        return NBmm[:nq, hb, :], ("NBmm",)
                    if meta_mode == "j0":
                        if isA:
                            return MBNm[:nq, h, :], ("MBNm",)
                        return NBm0[:nq, hb, :], ("NBm0",)
                    if isA:
                        return MB[:nq, 512 * (j + 1): 512 * (j + 1) + 16], ("MB",)
                    return None, None
                first = True
                for blk in range(nkb):
                    P.op("pe", "matmul", dict(out=ps[:nk, blk * 128:blk * 128 + nq], lhsT=kop(blk), rhs=qz,
                                              start=first, stop=False, skip_group_check=True),
                         r=[kkey, qkey], w=[("psS", si)])
                    first = False
                for blk in range(nkb):
                    m_ap, m_key = mop(blk)
                    if m_ap is not None:
                        P.op("pe", "matmul", dict(out=ps[:nk, blk * 128:blk * 128 + nq], lhsT=m_ap, rhs=idn[:nq, :nq],
                                                  start=False, stop=False, skip_group_check=True),
                             r=[m_key, ("idn",)], w=[("psS", si)])
                usebias = (kind == "t" and near is None) or (kind == "m" and meta_mode == "far")
                bias_ap = b31s[:nk, hb:hb + 1] if usebias else zcol[:nk, 0:1]
                pi = pcount[0] % 3
                pcount[0] += 1
                pT = pTs[pi]
                ncol = (nkb - 1) * 128 + nq
                P.op("act", "activation", dict(out=pT[:nk, :ncol], in_=ps[:nk, :ncol], func=AF.Exp, bias=bias_ap),
                     r=[("psS", si), ("b31",), ("zcol",)], w=[("pT", pi)])
                for blk in range(nkb):
                    P.op("pe", "matmul", dict(out=Oap, lhsT=pT[:nk, blk * 128:blk * 128 + nq], rhs=vop(blk),
                                              start=False, stop=False, skip_group_check=True),
                         r=[("pT", pi), vkey], w=[Okey])

    def finalize(nq, tok0):
        for i in range(2):
            ov = OA[i][:nq, 0:260].rearrange("p (h f) -> p h f", f=65)
            P.op("dve", "reciprocal", dict(out=sm[:nq, 12 + 4 * i:16 + 4 * i], in_=ov[:, :, 64]),
                 r=[("OA", i)], w=[("sm", "ra", i)])
            for hl in range(4):
                h = 4 * i + hl
                P.op("dve", "tensor_scalar", dict(out=abo[:nq, h * 64:(h + 1) * 64], in0=ov[:, hl, 0:64],
                                                  scalar1=sm[:nq, 12 + h:13 + h], scalar2=None, op0=ALU.mult),
                     r=[("OA", i), ("sm", "ra", i)], w=[("abo", h)])
        for i in range(2):
            ov = OB[i][:nq, 0:260].rearrange("p (h f) -> p h f", f=65)
            P.op("dve", "reciprocal", dict(out=sm[:nq, 20 + 4 * i:24 + 4 * i], in_=ov[:, :, 64]),
                 r=[("OB", i)], w=[("sm", "rb", i)])
        P.op("dve", "tensor_scalar", dict(out=sm[:nq, 20:28].rearrange("p (h c) -> p h c", c=2)[:, :, 1],
                                          in0=sm[:nq, 20:28].rearrange("p (h c) -> p h c", c=2)[:, :, 1],
                                          scalar1=sm[:nq, NLAM:NLAM + 1], scalar2=None, op0=ALU.mult),
             r=[("sm", "rb", 0), ("sm", "rb", 1), ("sm", "nlam")], w=[("sm", "rb", 0), ("sm", "rb", 1)])
        for h in range(4):
            i = h // 2
            ov = OB[i][:nq, 0:260].rearrange("p (h f) -> p h f", f=65)
            c0, c1 = (2 * h) % 4, (2 * h + 1) % 4
            t0 = fin[:nq, h * 64:(h + 1) * 64]
            bh = fin[:nq, 256 + h * 64:256 + (h + 1) * 64]
            P.op("dve", "tensor_scalar", dict(out=t0, in0=ov[:, c0, 0:64], scalar1=sm[:nq, 20 + 2 * h:21 + 2 * h],
                                              scalar2=None, op0=ALU.mult),
                 r=[("OB", i), ("sm", "rb", i)], w=[("fin", h)])
            P.op("dve", "scalar_tensor_tensor", dict(out=bh, in0=ov[:, c1, 0:64], scalar=sm[:nq, 21 + 2 * h:22 + 2 * h],
                                                     in1=t0, op0=ALU.mult, op1=ALU.add),
                 r=[("OB", i), ("sm", "rb", i), ("fin", h)], w=[("finb", h)])
            P.op("dve", "tensor_tensor", dict(out=t0, in0=bh, in1=bh, op=ALU.mult), r=[("finb", h)], w=[("fin", h)])
            P.op("dve", "tensor_reduce", dict(out=sm[:nq, 28 + h:29 + h], in_=t0, axis=AX.X, op=ALU.add),
                 r=[("fin", h)], w=[("sm", "ss", h)])
            P.op("act", "activation", dict(out=sm[:nq, 28 + h:29 + h], in_=sm[:nq, 28 + h:29 + h], func=AF.Sqrt,
                                           bias=epsc[:nq, 0:1], scale=1.0 / 64.0),
                 r=[("sm", "ss", h), ("epsc",)], w=[("sm", "ss", h)])
            P.op("dve", "reciprocal", dict(out=sm[:nq, 28 + h:29 + h], in_=sm[:nq, 28 + h:29 + h]),
                 r=[("sm", "ss", h)], w=[("sm", "ss", h)])
            P.op("dve", "scalar_tensor_tensor", dict(out=abo[:nq, 512 + h * 64:512 + (h + 1) * 64], in0=bh,
                                                     scalar=sm[:nq, 28 + h:29 + h], in1=dn[:nq, :],
                                                     op0=ALU.mult, op1=ALU.mult),
                 r=[("finb", h), ("sm", "ss", h), ("dn",)], w=[("abo", 8 + h)])
        P.dma(ab[tok0:tok0 + nq, :], abo[:nq, :], r=[("abo", k) for k in range(12)])

    epsc = S("epsc", [128, 1], F32)
    P.op("dve", "memset", dict(ap=epsc[:, :], constant=EPS), w=[("epsc",)])

    def load_q(tok0, nq, need_idx):
        P.dma(qa[:, :, :nq], qaz[:, :, tok0:tok0 + nq].rearrange("h p t -> p h t"), w=[("qa",)])
        P.dma(qb[:, :, :nq], qbz[:, :, tok0:tok0 + nq].rearrange("h p t -> p h t"), w=[("qb",)])
        if need_idx:
            P.dma(qi[:, :, :nq], qiz[:, :, tok0:tok0 + nq].rearrange("h p t -> p h t"), w=[("qi",)])
            P.dma(wis[:nq, :], wi[tok0:tok0 + nq, :], w=[("wis",)])

    if do_meta:
        load_q(0, 16, False)
        attend(16, [], "mm", None)
        finalize(16, 0)

    for j in range(njobs):
        tok0 = 16 + 128 * j
        nq = 128
        n = 512 * (j + 1) + 16
        load_q(tok0, nq, True)
        P.op("act", "activation", dict(out=absw[:, :], in_=wis[:, :], func=AF.Abs, scale=float(C_IDX)),
             r=[("wis",)], w=[("absw",)])
        P.op("act", "activation", dict(out=sgn[:, :], in_=wis[:, :], func=AF.Sign), r=[("wis",)], w=[("sgn",)])
        acckeys = []
        for g in list(range(j + 1)) + ["m"]:
            kb_ = icount[0] % 2
            icount[0] += 1
            ki_t = kis[kb_]
            if g == "m":
                nk, c0 = 16, 512 * (j + 1)
                P.dma(ki_t[:, 0:16], kiT2[:, 0:16], w=[("ki", kb_)])
            else:
                nk, c0 = 512, 512 * g
                k0 = 16 + 512 * g
                P.dma(ki_t[:, :], kiT2[:, k0:k0 + 512], w=[("ki", kb_)])
            akey = ("acc", g)
            acckeys.append(akey)
            for h in range(8):
                pi = (icount[0] * 8 + h) % 2
                ps = psI[pi]
                P.op("pe", "matmul", dict(out=ps[:, :nk], lhsT=qi[:, h, :], rhs=ki_t[:, :nk], start=True, stop=True),
                     r=[("qi",), ("ki", kb_)], w=[("psI", pi)])
                ri = rcount[0] % 3
                rcount[0] += 1
                rb = rbuf[ri]
                P.op("act", "activation", dict(out=rb[:, :nk], in_=ps[:, :nk], func=AF.Relu, scale=absw[:, h:h + 1]),
                     r=[("psI", pi), ("absw",)], w=[("r", ri)])
                if h == 0:
                    P.op("dve", "tensor_scalar", dict(out=acc[:, c0:c0 + nk], in0=rb[:, :nk], scalar1=sgn[:, 0:1],
                                                      scalar2=None, op0=ALU.mult),
                         r=[("r", ri), ("sgn",)], w=[akey])
                else:
                    P.op("dve", "scalar_tensor_tensor", dict(out=acc[:, c0:c0 + nk], in0=rb[:, :nk],
                                                             scalar=sgn[:, h:h + 1], in1=acc[:, c0:c0 + nk],
                                                             op0=ALU.mult, op1=ALU.add),
                         r=[("r", ri), ("sgn",), akey], w=[akey])
        dkey = ("acc", j)
        d0 = 512 * j
        P.op("dve", "scalar_tensor_tensor", dict(out=dtmp[:, :], in0=dm[:, :], scalar=-1.0, in1=acc[:, d0:d0 + 512],
                                                 op0=ALU.mult, op1=ALU.add), r=[("dm",), dkey], w=[("dtmp",)])
        P.op("dve", "tensor_reduce", dict(out=smc(RMIN), in_=dtmp[:, :], axis=AX.X, op=ALU.min),
             r=[("dtmp",)], w=[("sm", "rmin")])
        P.op("dve", "tensor_tensor", dict(out=acc[:, d0:d0 + 512], in0=acc[:, d0:d0 + 512], in1=dm[:, :], op=ALU.add),
             r=[("dm",), dkey], w=[dkey])
        P.op("dve", "tensor_reduce", dict(out=smc(RTMP), in_=acc[:, d0 + 512:n], axis=AX.X, op=ALU.min),
             r=acckeys, w=[("sm", "rtmp")])
        P.op("dve", "tensor_tensor", dict(out=smc(RMIN), in0=smc(RMIN), in1=smc(RTMP), op=ALU.min),
             r=[("sm", "rmin"), ("sm", "rtmp")], w=[("sm", "rmin")])
        if j > 0:
            P.op("dve", "tensor_reduce", dict(out=smc(RTMP), in_=acc[:, 0:d0], axis=AX.X, op=ALU.min),
                 r=acckeys, w=[("sm", "rtmp")])
            P.op("dve", "tensor_tensor", dict(out=smc(RMIN), in0=smc(RMIN), in1=smc(RTMP), op=ALU.min),
                 r=[("sm", "rmin"), ("sm", "rtmp")], w=[("sm", "rmin")])
        P.op("dve", "tensor_reduce", dict(out=smc(HI), in_=acc[:, 0:n], axis=AX.X, op=ALU.max),
             r=acckeys, w=[("sm", "hi")])
        P.op("dve", "tensor_copy", dict(out=smc(LO), in_=smc(RMIN)), r=[("sm", "rmin")], w=[("sm", "lo")])
        for it in range(nbis):
            P.op("dve", "scalar_tensor_tensor", dict(out=smc(MID), in0=smc(LO), scalar=smc(HI), in1=half[:, :],
                                                     op0=ALU.add, op1=ALU.mult),
                 r=[("sm", "lo"), ("sm", "hi"), ("half",)], w=[("sm", "mid")])
            P.op("dve", "tensor_scalar", dict(out=MB[:, 0:n], in0=acc[:, 0:n], scalar1=smc(MID), scalar2=0.0,
                                              op0=ALU.is_ge, op1=ALU.add, accum_out=smc(CNT)),
                 r=acckeys + [("sm", "mid")], w=[("MB",), ("sm", "cnt")])
            P.op("dve", "tensor_single_scalar", dict(out=smu[:, 0:1], in_=smc(CNT), scalar=255.5, op=ALU.is_ge),
                 r=[("sm", "cnt")], w=[("smu", 0)])
            P.op("dve", "tensor_single_scalar", dict(out=smu[:, 1:2], in_=smc(CNT), scalar=255.5, op=ALU.is_lt),
                 r=[("sm", "cnt")], w=[("smu", 1)])
            P.op("dve", "copy_predicated", dict(out=smc(LO), mask=smu[:, 0:1], data=smc(MID)),
                 r=[("smu", 0), ("sm", "mid")], w=[("sm", "lo")])
            P.op("dve", "copy_predicated", dict(out=smc(HI), mask=smu[:, 1:2], data=smc(MID)),
                 r=[("smu", 1), ("sm", "mid")], w=[("sm", "hi")])
        P.op("dve", "tensor_scalar", dict(out=MB[:, 0:n], in0=acc[:, 0:n], scalar1=smc(LO), scalar2=-BIG,
                                          op0=ALU.is_lt, op1=ALU.mult),
             r=acckeys + [("sm", "lo")], w=[("MB",)])
        if j == 0:
            tiles = [(0, 1)]
            for h in range(8):
                P.op("pool", "tensor_tensor", dict(out=MBN[:, h, 512:1024], in0=MB[:, 0:512], in1=NB[:, h, 512:1024],
                                                   op=ALU.add), r=[("MB",), ("NB",)], w=[("MBN", h)])
            for h in range(8):
                P.op("pool", "tensor_tensor", dict(out=MBNm[:, h, :], in0=MB[:, 512:528], in1=NBm0[:, h, :], op=ALU.add),
                     r=[("MB",), ("NBm0",)], w=[("MBNm",)])
            meta_mode = "j0"
        else:
            tiles = [(g, None) for g in range(j - 1)] + [(j - 1, 0), (j, 1)]
            for h in range(8):
                P.op("pool", "tensor_tensor", dict(out=MBN[:, h, :], in0=MB[:, 512 * (j - 1):512 * (j + 1)],
                                                   in1=NB[:, h, :], op=ALU.add),
                     r=[("MB",), ("NB",)], w=[("MBN", h)])
            meta_mode = "far"
        attend(nq, tiles, meta_mode, j)
        finalize(nq, tok0)
    return P.finish()


GAM = [1.0 - 2.0 ** (-5.0 - h) for h in range(4)]


def build_p2c(nblocks=128, do_meta=True):
    P = Prog()
    D = P.dram
    kc_all = D("kc_all", [TALL, 256], F32, "ExternalInput")
    vc_all = D("vc_all", [TALL, 256], F32, "ExternalInput")
    qcT = D("qcT", [4, 64, NT], F32, "ExternalInput")
    kcT = D("kcT", [4, 64, NT], F32, "ExternalInput")
    vc_loc = D("vc_loc", [NT, 256], F32, "ExternalInput")
    gc_loc = D("gc_loc", [NT, 256], F32, "ExternalInput")
    DTd = D("DT", [4, 128, 128], F32, "ExternalInput")
    QDd = D("QD", [64, 4, 128], F32, "ExternalInput")
    kdd = D("kd", [128, 4], F32, "ExternalInput")
    kd16d = D("kd16", [16, 4], F32, "ExternalInput")
    ohd = D("oh", [128, 4], F32, "ExternalInput")
    rnd = D("rn", [128, 256], F32, "ExternalInput")
    co = D("co", [NT, 256], BF16, "ExternalOutput")
    S = P.sb
    DT = S("DT", [128, 4, 128], F32)
    QD = S("QD", [64, 4, 128], F32)
    kd = S("kd", [128, 4], F32)
    kd16 = S("kd16", [16, 4], F32)
    oh = S("oh", [128, 4], F32)
    rn = S("rn", [128, 256], F32)
    epsc = S("epsc", [128, 1], F32)
    kcb = [S("kcb%d" % i, [128, 256], F32) for i in range(2)]
    vcb = [S("vcb%d" % i, [128, 256], F32) for i in range(2)]
    kdec = [S("kdec%d" % i, [128, 256], F32) for i in range(2)]
    ring = S("ring", [64, 4, 256], F32)
    ssel = S("ssel", [64, 256], F32)
    qT = S("qT", [64, 4, 128], F32)
    kT = S("kT", [64, 4, 128], F32)
    qd = S("qd", [64, 4, 128], F32)
    vl = S("vl", [128, 256], F32)
    gl = S("gl", [128, 256], F32)
    PT = [S("PT%d" % i, [128, 128], F32) for i in range(2)]
    ret = S("ret", [128, 256], F32)
    junk = S("junk", [128, 64], F32)
    ss = S("ss", [128, 4], F32)
    yb = S("yb", [128, 256], F32)
    sg = S("sg", [128, 256], F32)
    cob = S("cob", [128, 256], BF16)
    psU = [P.ps("psU%d" % i) for i in range(2)]
    psA = [P.ps("psA%d" % i) for i in range(2)]
    psO = P.ps("psO")
    P.dma(DT[:, :, :], DTd.rearrange("h j i -> j h i"), w=[("DT",)])
    P.dma(QD[:, :, :], QDd[:, :, :], w=[("QD",)])
    P.dma(kd[:, :], kdd[:, :], w=[("kd",)])
    P.dma(kd16[:, :], kd16d[:, :], w=[("kd16",)])
    P.dma(oh[:, :], ohd[:, :], w=[("oh",)])
    P.dma(rn[:, :], rnd[:, :], w=[("rn",)])
    P.op("dve", "memset", dict(ap=epsc[:, :], constant=EPS), w=[("epsc",)])
    acount = [0]

    def ret_block(tok0, L, use_state):
        P.dma(qT[:, :, :L], qcT[:, :, tok0:tok0 + L].rearrange("h d t -> d h t"), w=[("qT",)])
        P.dma(kT[:, :, :L], kcT[:, :, tok0:tok0 + L].rearrange("h d t -> d h t"), w=[("kT",)])
        P.dma(vl[:L, :], vc_loc[tok0:tok0 + L, :], w=[("vl",)])
        P.dma(gl[:L, :], gc_loc[tok0:tok0 + L, :], w=[("gl",)])
        if use_state:
            P.op("dve", "tensor_scalar", dict(out=ssel[:, :], in0=ring[:, 0, :], scalar1=oh[:64, 0:1], scalar2=None,
                                              op0=ALU.mult), r=[("ring", 0), ("oh",)], w=[("ssel",)])
            for c in range(1, 4):
                P.op("dve", "scalar_tensor_tensor", dict(out=ssel[:, :], in0=ring[:, c, :], scalar=oh[:64, c:c + 1],
                                                         in1=ssel[:, :], op0=ALU.mult, op1=ALU.add),
                     r=[("ring", c), ("oh",), ("ssel",)], w=[("ssel",)])
            P.op("dve", "tensor_tensor", dict(out=qd[:, :, :L], in0=qT[:, :, :L], in1=QD[:, :, :L], op=ALU.mult),
                 r=[("qT",), ("QD",)], w=[("qd",)])
        for h in range(4):
            ai = acount[0] % 2
            acount[0] += 1
            P.op("pe", "matmul", dict(out=psA[ai][:L, :L], lhsT=kT[:, h, :L], rhs=qT[:, h, :L], start=True, stop=True),
                 r=[("kT",), ("qT",)], w=[("psA", ai)])
            P.op("dve", "tensor_tensor", dict(out=PT[ai][:L, :L], in0=psA[ai][:L, :L], in1=DT[:L, h, :L], op=ALU.mult),
                 r=[("psA", ai), ("DT",)], w=[("PT", ai)])
            P.op("pe", "matmul", dict(out=psO[:L, h * 64:(h + 1) * 64], lhsT=PT[ai][:L, :L], rhs=vl[:L, h * 64:(h + 1) * 64],
                                      start=(h == 0), stop=(not use_state), skip_group_check=True),
                 r=[("PT", ai), ("vl",)], w=[("psO",)])
            if use_state:
                P.op("pe", "matmul", dict(out=psO[:L, h * 64:(h + 1) * 64], lhsT=qd[:, h, :L],
                                          rhs=ssel[:, h * 64:(h + 1) * 64], start=False, stop=True,
                                          skip_group_check=True),
                     r=[("qd",), ("ssel",)], w=[("psO",)])
        P.op("act", "activation", dict(out=ret[:L, :], in_=psO[:L, 0:256], func=AF.Copy), r=[("psO",)], w=[("ret",)])
        P.op("dve", "tensor_tensor", dict(out=yb[:L, :], in0=ret[:L, :], in1=ret[:L, :], op=ALU.mult),
             r=[("ret",)], w=[("yb", h) for h in range(4)])
        P.op("dve", "tensor_reduce", dict(out=ss[:L, :], in_=yb[:L, :].rearrange("p (h d) -> p h d", d=64),
                                          axis=AX.X, op=ALU.add),
             r=[("yb", h) for h in range(4)], w=[("ss", h) for h in range(4)])
        P.op("act", "activation", dict(out=ss[:L, :], in_=ss[:L, :], func=AF.Sqrt, bias=epsc[:L, 0:1], scale=1.0 / 64.0),
             r=[("ss", h) for h in range(4)] + [("epsc",)], w=[("ss", h) for h in range(4)])
        P.op("dve", "reciprocal", dict(out=ss[:L, :], in_=ss[:L, :]), r=[("ss", h) for h in range(4)],
             w=[("ss", h) for h in range(4)])
        for h in range(4):
            P.op("dve", "scalar_tensor_tensor", dict(out=yb[:L, h * 64:(h + 1) * 64], in0=ret[:L, h * 64:(h + 1) * 64],
                                                     scalar=ss[:L, h:h + 1], in1=rn[:L, h * 64:(h + 1) * 64],
                                                     op0=ALU.mult, op1=ALU.mult),
                 r=[("ret",), ("ss", h), ("rn",)], w=[("yb", h)])
        P.op("act", "activation", dict(out=sg[:L, :], in_=gl[:L, :], func=AF.Silu), r=[("gl",)], w=[("sg",)])
        P.op("pool", "tensor_tensor", dict(out=cob[:L, :], in0=yb[:L, :], in1=sg[:L, :], op=ALU.mult),
             r=[("yb", h) for h in range(4)] + [("sg",)], w=[("cob",)])
        P.dma(co[tok0:tok0 + L, :], cob[:L, :], r=[("cob",)])

    if do_meta:
        ret_block(0, 16, False)
    for B in range(nblocks):
        bi = B % 2
        if B == 0:
            k0, L, kdt = 0, 16, kd16
        else:
            k0, L, kdt = 16 + 128 * (B - 1), 128, kd
        P.dma(kcb[bi][:L, :], kc_all[k0:k0 + L, :], w=[("kcb", bi)])
        P.dma(vcb[bi][:L, :], vc_all[k0:k0 + L, :], w=[("vcb", bi)])
        for h in range(4):
            P.op("pool", "tensor_scalar", dict(out=kdec[bi][:L, h * 64:(h + 1) * 64], in0=kcb[bi][:L, h * 64:(h + 1) * 64],
                                               scalar1=kdt[:L, h:h + 1], scalar2=None, op0=ALU.mult),
                 r=[("kcb", bi), ("kd",), ("kd16",)], w=[("kdec", bi, h)])
        for h in range(4):
            P.op("pe", "matmul", dict(out=psU[bi][:64, h * 64:(h + 1) * 64], lhsT=kdec[bi][:L, h * 64:(h + 1) * 64],
                                      rhs=vcb[bi][:L, h * 64:(h + 1) * 64], start=True, stop=True),
                 r=[("kdec", bi, h), ("vcb", bi)], w=[("psU", bi)])
        slot, pslot = B % 4, (B - 1) % 4
        if B == 0:
            P.op("dve", "tensor_copy", dict(out=ring[:, slot, :], in_=psU[bi][:64, 0:256]),
                 r=[("psU", bi)], w=[("ring", slot)])
        else:
            for h in range(4):
                P.op("dve", "scalar_tensor_tensor", dict(out=ring[:, slot, h * 64:(h + 1) * 64],
                                                         in0=ring[:, pslot, h * 64:(h + 1) * 64],
                                                         scalar=float(GAM[h] ** L), in1=psU[bi][:64, h * 64:(h + 1) * 64],
                                                         op0=ALU.mult, op1=ALU.add),
                     r=[("ring", pslot), ("psU", bi)], w=[("ring", slot)])
        if B % 4 == 3:
            j = B // 4
            ret_block(16 + 128 * j, 128, True)
    return P.finish()


import math as _math

try:
    import ml_dtypes as _mld
    NP_BF16 = _mld.bfloat16
except Exception:
    NP_BF16 = None


def rel_bucket_np(dist):
    n = np.maximum(dist, 0)
    nf = np.maximum(n, 1).astype(np.float32)
    large = 16 + (np.log(nf / np.float32(16)) / np.float32(_math.log(128 / 16)) * np.float32(16)).astype(np.int32)
    large = np.minimum(large, 31)
    return np.where(n < 16, n, large)


def local_positions(cc):
    pos = [np.arange(16)]
    for j in range(NJ):
        pos.append(16 + 128 * (4 * j + cc) + np.arange(128))
    return np.concatenate(pos)


def rope_table(cc):
    pos = local_positions(cc).astype(np.float32)
    inv = (np.float32(10000.0) ** (-np.arange(0, 64, 2, dtype=np.float32) / np.float32(64))).astype(np.float32)
    ang = pos[:, None] * inv[None, :]
    cos = np.cos(ang).astype(np.float32)
    sin = np.sin(ang).astype(np.float32)
    return np.ascontiguousarray(np.concatenate([np.tile(cos, (1, 8)), np.tile(sin, (1, 8))], axis=1))


def attn_consts(rel_bias, cc):
    i = np.arange(128)
    nb = np.empty((12, 128, 1024), np.float32)
    dmask = np.empty((128, 512), np.float32)
    for near in range(2):
        for cp in range(4):
            dR = (4 + cc - cp) if near == 0 else (cc - cp)
            dist = 128 * dR + i[:, None] - i[None, :]
            vis = dist >= 0
            bk = rel_bucket_np(dist)
            for hb in range(12):
                vals = rel_bias[bk, hb]
                nb[hb, :, near * 512 + cp * 128: near * 512 + (cp + 1) * 128] = np.where(vis, vals, np.float32(-BIG))
            if near == 1:
                dmask[:, cp * 128:(cp + 1) * 128] = np.where(vis, np.float32(0.0), np.float32(-BIG))
    s = np.arange(16)
    dist = 16 + 128 * cc + i[:, None] - s[None, :]
    bk = rel_bucket_np(dist)
    nbm0 = np.stack([rel_bias[bk, hb] for hb in range(12)]).astype(np.float32)
    dist = s[:, None] - s[None, :]
    bk = rel_bucket_np(dist)
    nbmm = np.stack([np.where(dist >= 0, rel_bias[bk, hb], np.float32(-BIG)) for hb in range(12)]).astype(np.float32)
    b31 = np.ascontiguousarray(np.broadcast_to(rel_bias[31][None, :], (128, 12))).astype(np.float32)
    return dict(dmask=dmask, nb=nb.astype(NP_BF16), nbm0=nbm0.astype(NP_BF16), nbmm=nbmm.astype(NP_BF16), b31=b31)


def ret_consts():
    i = np.arange(128)
    DT = np.zeros((4, 128, 128), np.float64)
    QD = np.zeros((64, 4, 128), np.float64)
    kd = np.zeros((128, 4), np.float64)
    kd16 = np.zeros((16, 4), np.float64)
    for h in range(4):
        g = GAM[h]
        d = i[None, :] - i[:, None]
        DT[h] = np.where(d >= 0, g ** np.maximum(d, 0), 0.0) / 8.0
        QD[:, h, :] = (g ** (i + 1.0))[None, :]
        kd[:, h] = g ** (127.0 - i) / 8.0
        kd16[:, h] = g ** (15.0 - np.arange(16)) / 8.0
    return dict(DT=DT.astype(np.float32), QD=QD.astype(np.float32), kd=kd.astype(np.float32), kd16=kd16.astype(np.float32))


def to_global(locs, axis):
    locs = [np.moveaxis(a, axis, 0) for a in locs]
    meta = locs[0][:16]
    real = np.stack([a[16:].reshape((NJ, 128) + a.shape[1:]) for a in locs], axis=1)
    real = real.reshape((NJ * 4 * 128,) + locs[0].shape[1:])
    out = np.concatenate([meta, real], axis=0)
    return np.ascontiguousarray(np.moveaxis(out, 0, axis))


def zero_other(chunk, lo, hi):
    z = np.zeros_like(chunk)
    z[lo:hi] = chunk[lo:hi]
    return z


_PROGS = {}


def get_prog(name, *args):
    key = (name,) + args
    if key not in _PROGS:
        _PROGS[key] = {"p1": build_p1, "p2ab": build_p2ab, "p2c": build_p2c, "p3": build_p3}[name](*args)
    return _PROGS[key]


def garr(v):
    return np.ascontiguousarray(np.asarray(v, np.float32).reshape(8, 128).T)


def run_layer(l, hTs, inp, cfg, dbg=None):
    ncore = 8
    rel_bias = np.asarray(inp["rel_bias"], np.float32)
    lambda_init = 0.8 - 0.6 * _math.exp(-0.3 * l)
    wperm = np.ascontiguousarray(np.asarray(inp["w_in"][l], np.float32)[:, W_PERM])
    gm = garr(inp["norm_mix"][l])
    ropes = [rope_table(cc) for cc in range(4)]
    p1 = get_prog("p1", cfg.get("p1_tiles", 9))
    r1 = run(p1, [{"hT": hTs[c], "w": wperm, "g": gm, "cs": ropes[c % 4]} for c in range(ncore)])
    if dbg is not None:
        dbg["r1"] = r1
    rc = ret_consts()
    lamb = np.ascontiguousarray(np.broadcast_to(np.asarray(inp["diff_lambda"][l], np.float32).reshape(1, 128), (128, 128)))
    dnb = np.ascontiguousarray(np.broadcast_to(np.asarray(inp["diff_norm"][l], np.float32).reshape(1, 64), (128, 64)))
    rnb = np.ascontiguousarray(np.broadcast_to(np.asarray(inp["ret_norm"][l], np.float32).reshape(1, 256), (128, 256)))
    ident = np.eye(128, dtype=np.float32).astype(NP_BF16)
    in_ab, in_c = [], []
    gl = {}
    for b in range(2):
        cores = [4 * b + cc for cc in range(4)]
        fm_g = to_global([r1[c]["fmo"] for c in cores], 1)
        tmb_g = to_global([r1[c]["tmb"] for c in cores], 0)
        tmo_g = to_global([r1[c]["tmo"] for c in cores], 0)
        gl[b] = dict(
            kaT=np.ascontiguousarray(fm_g[FM_OFF["ka"]:FM_OFF["ka"] + 512]),
            kbT=np.ascontiguousarray(fm_g[FM_OFF["kb"]:FM_OFF["kb"] + 256]),
            kiT2=np.ascontiguousarray(np.concatenate([fm_g[FM_OFF["ki"]:FM_OFF["ki"] + 64]] * 2, axis=0)),
            va=np.ascontiguousarray(tmb_g[:, 0:520]), vb=np.ascontiguousarray(tmb_g[:, 520:780]),
            kc_all=np.ascontiguousarray(tmo_g[:, TM_OFF["kc"]:TM_OFF["kc"] + 256]),
            vc_all=np.ascontiguousarray(tmo_g[:, TM_OFF["vc"]:TM_OFF["vc"] + 256]))
    for c in range(ncore):
        b, cc = c // 4, c % 4
        fm, tm = r1[c]["fmo"], r1[c]["tmo"]
        qaT = fm[FM_OFF["qa"]:FM_OFF["qa"] + 512]
        qbT = fm[FM_OFF["qb"]:FM_OFF["qb"] + 256]
        qiT = fm[FM_OFF["qi"]:FM_OFF["qi"] + 512]
        qaz = np.stack([zero_other(qaT[(h // 2) * 128:(h // 2 + 1) * 128], (h % 2) * 64, (h % 2) * 64 + 64) for h in range(8)])
        qiz = np.stack([zero_other(qiT[(h // 2) * 128:(h // 2 + 1) * 128], (h % 2) * 64, (h % 2) * 64 + 64) for h in range(8)])
        qbz = np.stack([zero_other(qbT[(hc // 4) * 128:(hc // 4 + 1) * 128], ((hc // 2) % 2) * 64 + (hc % 2) * 32,
                                   ((hc // 2) % 2) * 64 + (hc % 2) * 32 + 32) for hc in range(8)])
        ac = attn_consts(rel_bias, cc)
        d = dict(gl[b])
        kc_all, vc_all = d.pop("kc_all"), d.pop("vc_all")
        d.update(qaz=qaz, qbz=qbz, qiz=qiz, wi=np.ascontiguousarray(tm[:, TM_OFF["wi"]:TM_OFF["wi"] + 8]),
                 ident=ident, lamb=lamb, dnb=dnb, **ac)
        in_ab.append(d)
        qc = tm[:, TM_OFF["qc"]:TM_OFF["qc"] + 256]
        kc = tm[:, TM_OFF["kc"]:TM_OFF["kc"] + 256]
        oh = np.zeros((128, 4), np.float32)
        oh[:, cc] = 1.0
        in_c.append(dict(kc_all=kc_all, vc_all=vc_all,
                         qcT=np.ascontiguousarray(qc.T.reshape(4, 64, NT)), kcT=np.ascontiguousarray(kc.T.reshape(4, 64, NT)),
                         vc_loc=np.ascontiguousarray(tm[:, TM_OFF["vc"]:TM_OFF["vc"] + 256]),
                         gc_loc=np.ascontiguousarray(tm[:, TM_OFF["gc"]:TM_OFF["gc"] + 256]),
                         oh=oh, rn=rnb, **rc))
    p2ab = get_prog("p2ab", cfg.get("njobs", NJ), True, lambda_init, cfg.get("nbis", NBIS))
    rab = run(p2ab, in_ab)
    p2c = get_prog("p2c", cfg.get("nblocks", 128), True)
    rcc = run(p2c, in_c)
    if dbg is not None:
        dbg["rab"], dbg["rc"] = rab, rcc
    p3 = get_prog("p3", cfg.get("p3_tiles", 17), l == 1)
    in3 = []
    for c in range(ncore):
        mixT = np.ascontiguousarray(np.concatenate([rab[c]["ab"], rcc[c]["co"]], axis=1).T)
        in3.append(dict(hT=hTs[c], mxT=mixT, wo=np.asarray(inp["w_out"][l], np.float32),
                        w1=np.asarray(inp["w_ff1"][l], np.float32), w2=np.asarray(inp["w_ff2"][l], np.float32),
                        g=garr(inp["norm_ff"][l]), gf=garr(inp["final_norm"])))
    r3 = run(p3, in3)
    return [r3[c]["hoT"] for c in range(ncore)]


def shard_x(x, meta):
    hTs = []
    for c in range(8):
        b, cc = c // 4, c % 4
        xb = np.asarray(x[b], np.float32).reshape(NJ, 4, 128, 1024)[:, cc].reshape(NJ * 128, 1024)
        hTs.append(np.ascontiguousarray(np.concatenate([np.asarray(meta, np.float32), xb], axis=0).T))
    return hTs


def kernel(x, meta, rel_bias, w_in, norm_mix, diff_lambda, diff_norm, ret_norm, w_out, norm_ff, w_ff1, w_ff2, final_norm):
    inp = dict(x=x, meta=meta, rel_bias=rel_bias, w_in=w_in, norm_mix=norm_mix, diff_lambda=diff_lambda,
               diff_norm=diff_norm, ret_norm=ret_norm, w_out=w_out, norm_ff=norm_ff, w_ff1=w_ff1, w_ff2=w_ff2,
               final_norm=final_norm)
    inp = {k: np.asarray(v) for k, v in inp.items()}
    hTs = shard_x(inp["x"], inp["meta"])
    for l in range(2):
        hTs = run_layer(l, hTs, inp, {})
    out = np.empty((2, 16384, 1024), np.float32)
    for c in range(8):
        b, cc = c // 4, c % 4
        o = hTs[c][:, 16:].T.reshape(NJ, 128, 1024)
        out[b].reshape(NJ, 4, 128, 1024)[:, cc] = o
    return out
```

```python
import contextlib
import math as _math
import numpy as np
import concourse.bass as bass
import concourse.mybir as mybir
from concourse.bass_utils import run_bass_kernel_spmd

try:
    import ml_dtypes as _mld
    NP_BF16 = _mld.bfloat16
except Exception:
    NP_BF16 = None

F32 = mybir.dt.float32
BF16 = mybir.dt.bfloat16
U32 = mybir.dt.uint32
AF = mybir.ActivationFunctionType
ALU = mybir.AluOpType
AX = mybir.AxisListType

NSLOT = 24


class Prog:
    def __init__(self):
        self.nc = nc = bass.Bass("TRN2", target_bir_lowering=False)
        self.top = contextlib.ExitStack()
        self.cur = self.top
        self.engs = {"pe": nc.tensor, "act": nc.scalar, "dve": nc.vector, "pool": nc.gpsimd, "sp": nc.sync}
        self.sems = {e: self.top.enter_context(nc.semaphore("s_" + e)) for e in ("pe", "act", "dve", "pool")}
        self.cnt = {e: 0 for e in self.sems}
        self.slots = {q: [self.top.enter_context(nc.semaphore("d_%s_%d" % (q, s))) for s in range(NSLOT)]
                      for q in ("sp", "pool")}
        self.slotval = {q: [0] * NSLOT for q in self.slots}
        self.slotnext = {q: 0 for q in self.slots}
        self.ccs = []
        self.waited = {e: {} for e in self.engs}
        self.lastw = {}
        self.readers = {}
        self.nph = 0
        self.nuniq = 0

    def dram(self, name, shape, dt, kind):
        return self.nc.dram_tensor(name, list(shape), dt, kind=kind).ap()

    def sb(self, name, shape, dt):
        self.nuniq += 1
        return self.cur.enter_context(self.nc.sbuf_tensor("s%d_%s" % (self.nuniq, name), list(shape), dt))

    def ps(self, name, shape=(128, 512), dt=F32):
        self.nuniq += 1
        return self.cur.enter_context(self.nc.psum_tensor("p%d_%s" % (self.nuniq, name), list(shape), dt))

    @contextlib.contextmanager
    def phase(self):
        old = self.cur
        self.cur = contextlib.ExitStack()
        try:
            yield
        finally:
            self.barrier()
            self.cur.close()
            self.cur = old

    @staticmethod
    def _excl(k):
        return isinstance(k, tuple) and isinstance(k[0], str) and (k[0].startswith("ps") or k[0] in ("OA", "OB"))

    def _deps(self, eng, r, w, is_compute):
        w = tuple(w) + tuple(k for k in r if self._excl(k) and k not in w)
        toks = []
        for k in r:
            if k in self.lastw:
                toks.append(self.lastw[k])
        for k in w:
            if k in self.lastw:
                t = self.lastw[k]
                toks.append(t)
            for t in self.readers.get(k, ()):
                toks.append(t)
        need = {}
        for (sem, val, src) in toks:
            if is_compute and eng == "pe" and src == "pe":
                continue
            if is_compute and src == eng and val <= self.cnt[eng] - 4:
                continue
            if need.get(id(sem), (None, 0))[1] < val:
                need[id(sem)] = (sem, val)
        return w, need

    def _emit_waits(self, eng, need):
        E = self.engs[eng]
        wd = self.waited[eng]
        for sid, (sem, val) in need.items():
            if wd.get(sid, 0) >= val:
                continue
            E.wait_ge(sem, val)
            wd[sid] = val

    def _record(self, r, w, tok):
        for k in w:
            self.lastw[k] = tok
            self.readers[k] = []
        for k in r:
            if k not in w:
                self.readers.setdefault(k, []).append(tok)

    def op(self, eng, name, kw, r=(), w=()):
        w, need = self._deps(eng, r, w, True)
        self._emit_waits(eng, need)
        ins = getattr(self.engs[eng], name)(**kw)
        self.cnt[eng] += 1
        ins.then_inc(self.sems[eng], 1)
        self._record(r, w, (self.sems[eng], self.cnt[eng], eng))

    def dma(self, out, in_, r=(), w=(), q="sp", **kw):
        w, need = self._deps(q, r, w, False)
        si = self.slotnext[q]
        self.slotnext[q] = (si + 1) % NSLOT
        sem = self.slots[q][si]
        pv = self.slotval[q][si]
        if pv > 0 and need.get(id(sem), (None, 0))[1] < pv:
            need[id(sem)] = (sem, pv)
        self._emit_waits(q, need)
        ins = self.engs[q].dma_start(out=out, in_=in_, **kw)
        self.slotval[q][si] += 16
        ins.then_inc(sem, 16)
        self._record(r, w, (sem, self.slotval[q][si], "dma"))

    def coll(self, in_ap, out_ap, groups, r=()):
        _, need = self._deps("pool", r, (), False)
        self._emit_waits("pool", need)
        if not self.ccs:
            self.ccs = [self.top.enter_context(self.nc.semaphore("cc%d" % i)) for i in range(8)]
            self.ccval = [0] * 8
            self.ccn = 0
        i = self.ccn % 8
        self.ccn += 1
        sem = self.ccs[i]
        if self.ccval[i] > 0 and self.waited["pool"].get(id(sem), 0) < self.ccval[i]:
            self.nc.gpsimd.wait_ge(sem, self.ccval[i])
            self.waited["pool"][id(sem)] = self.ccval[i]
        ins = self.nc.gpsimd.collective_compute("AllGather", ALU.bypass, replica_groups=groups,
                                                ins=[in_ap], outs=[out_ap])
        ins.then_inc(sem)
        self.ccval[i] += 1

    def barrier(self):
        for eng, E in self.engs.items():
            wd = self.waited[eng]
            for e2, sem in self.sems.items():
                if self.cnt[e2] > 0 and wd.get(id(sem), 0) < self.cnt[e2]:
                    E.wait_ge(sem, self.cnt[e2])
                    wd[id(sem)] = self.cnt[e2]
            for q in self.slots:
                for si in range(NSLOT):
                    v = self.slotval[q][si]
                    sem = self.slots[q][si]
                    if v > 0 and wd.get(id(sem), 0) < v:
                        E.wait_ge(sem, v)
                        wd[id(sem)] = v
            for i, sem in enumerate(self.ccs):
                v = self.ccval[i]
                if v > 0 and wd.get(id(sem), 0) < v:
                    E.wait_ge(sem, v)
                    wd[id(sem)] = v
        self.lastw.clear()
        self.readers.clear()

    def finish(self):
        self.barrier()
        self.top.close()
        return self.nc


def run(prog_nc, in_maps):
    res = run_bass_kernel_spmd(prog_nc, in_maps, core_ids=list(range(len(in_maps))))
    return res.results


NT = 4112
NJ = 32
TALL = 16400
EPS = 1e-6
NBIS = 24
BIG = 30000.0
C_IDX = (8 ** -0.5) * (64 ** -0.5)
GAM = [1.0 - 2.0 ** (-5.0 - h) for h in range(4)]
GROUPS = [[0, 1, 2, 3], [4, 5, 6, 7]]
_OFF = dict(qa=0, ka=512, va=1024, qi=1536, ki=2048, wi=2112, qb=2120, kb=2376, vb=2632,
            qc=2888, kc=3144, vc=3400, gc=3656)
_SZ = dict(qa=512, ka=512, va=512, qi=512, ki=64, wi=8, qb=256, kb=256, vb=256, qc=256,
           kc=256, vc=256, gc=256)
FM_ORDER = ["qa", "qi", "qb", "ka", "kb", "ki"]
TM_ORDER = ["va", "vb", "kc", "vc", "qc", "gc", "wi"]
FM_OFF, TM_OFF = {}, {}
_o = 0
for _n in FM_ORDER:
    FM_OFF[_n] = _o
    _o += _SZ[_n]
NFM = _o
KROW0 = FM_OFF["ka"]
NKROW = NFM - KROW0
_o = 0
for _n in TM_ORDER:
    TM_OFF[_n] = _o
    _o += _SZ[_n]
NTM = _o
W_PERM = np.concatenate([np.arange(_OFF[n], _OFF[n] + _SZ[n]) for n in FM_ORDER + TM_ORDER])
NVB = 780
NKC = 512
NQC = 520


def tiles_of(ntiles):
    t = [(0, 16)]
    for i in range(8):
        t.append((16 + 512 * i, 512))
    return t[:ntiles]


def tiles256(ntiles):
    t = [(0, 16)]
    for i in range(16):
        t.append((16 + 256 * i, 256))
    return t[:ntiles]


def emit_rmsnorm_T(P, ht, out, ntok, g_sb, sq, ps_ss, rstd, sd, ones_bf, eps_t, htkey, outkey):
    P.op("act", "activation", dict(out=sq[:, :, :ntok], in_=ht[:, :, :ntok], func=AF.Square),
         r=[htkey], w=[("sq",)])
    for k in range(8):
        P.op("pe", "matmul", dict(out=ps_ss[:, :ntok], lhsT=ones_bf[:, :], rhs=sq[:, k, :ntok],
                                  start=(k == 0), stop=(k == 7)),
             r=[("sq",), ("ones",)], w=[("ps_ss",)])
    P.op("act", "activation", dict(out=sd[:, :ntok], in_=ps_ss[:, :ntok], func=AF.Sqrt,
                                   bias=eps_t[:, 0:1], scale=1.0 / 1024.0),
         r=[("ps_ss",), ("eps",)], w=[("sd",)])
    P.op("dve", "reciprocal", dict(out=rstd[:, :ntok], in_=sd[:, :ntok]), r=[("sd",)], w=[("rstd",)])
    for k in range(8):
        P.op("dve", "scalar_tensor_tensor", dict(out=out[:, k, :ntok], in0=ht[:, k, :ntok],
                                                 scalar=g_sb[:, k:k + 1], in1=rstd[:, :ntok],
                                                 op0=ALU.mult, op1=ALU.mult),
             r=[htkey, ("rstd",), ("g",)], w=[outkey])


def tile_of_tok(t0):
    if t0 < 16:
        return 0, t0
    return 1 + (t0 - 16) // 512, (t0 - 16) % 512


def emit_p1(P, hT, w, g, cs, fmo, ksl, tmb, tmk, tmq, gK, gV, gKC, ntiles=9):
    w_sb = P.sb("w_sb", [128, 8, 3912], BF16)
    g_sb = P.sb("g_sb", [128, 8], F32)
    ones_bf = P.sb("ones_bf", [128, 128], BF16)
    eps_t = P.sb("eps_t", [128, 1], F32)
    hts = [P.sb("ht%d" % i, [128, 8, 512], F32) for i in range(2)]
    uTs = [P.sb("uT%d" % i, [128, 8, 512], BF16) for i in range(2)]
    sq = P.sb("sq", [128, 8, 512], BF16)
    sd = P.sb("sd", [128, 512], F32)
    rstd = P.sb("rstd", [128, 512], F32)
    fms = [P.sb("fm%d" % i, [128, 512], BF16) for i in range(4)]
    tks = [P.sb("tk%d" % i, [128, NKC], F32) for i in range(2)]
    tqs = [P.sb("tq%d" % i, [128, NQC], F32) for i in range(2)]
    tbs = [P.sb("tb%d" % i, [128, 12, 65], BF16) for i in range(2)]
    css = [P.sb("cs%d" % i, [128, 512], F32) for i in range(2)]
    rt = P.sb("rt", [128, 4, 256], F32)
    ps_ss = P.ps("ps_ss")
    pss = [P.ps("ps%d" % i) for i in range(6)]
    hTv = hT.rearrange("(k p) t -> p k t", p=128)
    wv = w.rearrange("(k p) c -> p k c", p=128)
    P.op("dve", "memset", dict(ap=ones_bf[:, :], constant=1.0), w=[("ones",)])
    P.op("dve", "memset", dict(ap=eps_t[:, :], constant=EPS), w=[("eps",)])
    P.dma(g_sb[:, :], g[:, :], w=[("g",)])
    for i in range(2):
        P.op("dve", "memset", dict(ap=tbs[i][:, :, :], constant=1.0), w=[("tb", i, 0), ("tb", i, 1)])
    for k in range(8):
        P.dma(w_sb[:, k, :], wv[:, k, :], w=[("w", k)], q="pool")
    scale_of = {"qa": 0.125, "qb": 32.0 ** -0.5}
    fm_groups = []
    for n in FM_ORDER:
        for r0 in range(0, _SZ[n], 128):
            fm_groups.append((FM_OFF[n] + r0, min(128, _SZ[n] - r0), scale_of.get(n, 1.0)))
    tm_groups = [(0, 512), (512, 512), (1024, 512), (1536, 264)]
    pc = [0]
    fc = [0]
    sc_ = [0]

    def nextps():
        i = pc[0] % 6
        pc[0] += 1
        return i

    for ti, (tok0, ntok) in enumerate(tiles_of(ntiles)):
        b = ti % 2
        ht, uT = hts[b], uTs[b]
        P.dma(ht[:, :, :ntok], hTv[:, :, tok0:tok0 + ntok], w=[("ht", b)])
        emit_rmsnorm_T(P, ht, uT, ntok, g_sb, sq, ps_ss, rstd, sd, ones_bf, eps_t, ("ht", b), ("uT", b))
        for (r0, M, sc) in fm_groups:
            pi = nextps()
            ps = pss[pi]
            for k in range(8):
                P.op("pe", "matmul", dict(out=ps[:M, :ntok], lhsT=w_sb[:, k, r0:r0 + M], rhs=uT[:, k, :ntok],
                                          start=(k == 0), stop=(k == 7)),
                     r=[("uT", b), ("w", k)], w=[("ps", pi)])
            fi = fc[0] % 4
            fc[0] += 1
            fm = fms[fi]
            P.op("act", "activation", dict(out=fm[:M, :ntok], in_=ps[:M, :ntok], func=AF.Copy, scale=sc),
                 r=[("ps", pi)], w=[("fm", fi)])
            if r0 >= KROW0:
                P.dma(ksl[ti][r0 - KROW0:r0 - KROW0 + M, 0:ntok], fm[:M, :ntok], r=[("fm", fi)], w=[("dK", ti, r0)])
            else:
                P.dma(fmo[r0:r0 + M, tok0:tok0 + ntok], fm[:M, :ntok], r=[("fm", fi)])
        nsb = max(1, ntok // 128)
        for s in range(nsb):
            nt = min(128, ntok)
            t0 = tok0 + s * 128
            si = sc_[0] % 2
            sc_[0] += 1
            tk, tq, tb, cst = tks[si], tqs[si], tbs[si], css[si]
            P.dma(cst[:nt, :], cs[t0:t0 + nt, :], w=[("cs", si)])
            for gi, (c0, ncol) in enumerate(tm_groups):
                pi = nextps()
                ps = pss[pi]
                for k in range(8):
                    P.op("pe", "matmul", dict(out=ps[:nt, :ncol], lhsT=uT[:, k, s * 128:s * 128 + nt],
                                              rhs=w_sb[:, k, NFM + c0:NFM + c0 + ncol],
                                              start=(k == 0), stop=(k == 7)),
                         r=[("uT", b), ("w", k)], w=[("ps", pi)])
                if gi == 0:
                    P.op("dve", "tensor_copy", dict(out=tb[:nt, 0:8, 0:64],
                                                    in_=ps[:nt, 0:512].rearrange("p (h d) -> p h d", d=64)),
                         r=[("ps", pi)], w=[("tb", si, 0)])
                elif gi == 1:
                    P.op("act", "activation", dict(out=tb[:nt, 8:12, 0:64],
                                                   in_=ps[:nt, 0:256].rearrange("p (h d) -> p h d", d=64), func=AF.Copy),
                         r=[("ps", pi)], w=[("tb", si, 1)])
                    emit_rotary(P, ps[:nt, 256:512], tk[:nt, 0:256], cst, nt, rt, ("ps", pi), ("cs", si), ("tk", si, 0))
                elif gi == 2:
                    P.op("act", "activation", dict(out=tk[:nt, 256:512], in_=ps[:nt, 0:256], func=AF.Copy),
                         r=[("ps", pi)], w=[("tk", si, 1)])
                    emit_rotary(P, ps[:nt, 256:512], tq[:nt, 0:256], cst, nt, rt, ("ps", pi), ("cs", si), ("tq", si, 0))
                else:
                    P.op("act", "activation", dict(out=tq[:nt, 256:520], in_=ps[:nt, 0:264], func=AF.Copy),
                         r=[("ps", pi)], w=[("tq", si, 1)])
            P.dma(tmb[ti][s * 128:s * 128 + nt, :], tb[:nt, :, :].rearrange("p h f -> p (h f)"),
                  r=[("tb", si, 0), ("tb", si, 1)], w=[("dV", ti, s)])
            P.dma(tmk[ti][s * 128:s * 128 + nt, :], tk[:nt, :], r=[("tk", si, 0), ("tk", si, 1)], w=[("dC", ti, s)])
            P.dma(tmq[t0:t0 + nt, :], tq[:nt, :], r=[("tq", si, 0), ("tq", si, 1)])
        P.coll(ksl[ti][:, :], gK[ti][:, :], GROUPS, r=[("dK", ti, r0) for (r0, M, sc) in fm_groups if r0 >= KROW0])
        P.coll(tmb[ti][:, :], gV[ti][:, :], GROUPS, r=[("dV", ti, s) for s in range(nsb)])
        P.coll(tmk[ti][:, :], gKC[ti][:, :], GROUPS, r=[("dC", ti, s) for s in range(nsb)])


def emit_rotary(P, ps256, out256, cst, nt, rt, pskey, cskey, outkey):
    psv = ps256.rearrange("p (h d) -> p h d", d=64)
    x1, x2 = psv[:, :, 0:32], psv[:, :, 32:64]
    cosv = cst[:nt, 0:128].rearrange("p (h d) -> p h d", d=32)
    sinv = cst[:nt, 256:384].rearrange("p (h d) -> p h d", d=32)
    tv = [rt[:nt, i, 0:128].rearrange("p (h d) -> p h d", d=32) for i in range(4)]
    ov = out256.rearrange("p (h d) -> p h d", d=64)
    for i, (a_, b_) in enumerate([(x1, cosv), (x2, sinv), (x1, sinv), (x2, cosv)]):
        P.op("dve", "tensor_tensor", dict(out=tv[i], in0=a_, in1=b_, op=ALU.mult),
             r=[pskey, cskey], w=[("rt", i)])
    P.op("pool", "tensor_tensor", dict(out=ov[:, :, 0:32], in0=tv[0], in1=tv[1], op=ALU.subtract),
         r=[("rt", 0), ("rt", 1)], w=[outkey])
    P.op("pool", "tensor_tensor", dict(out=ov[:, :, 32:64], in0=tv[2], in1=tv[3], op=ALU.add),
         r=[("rt", 2), ("rt", 3), outkey], w=[outkey])


def emit_p3(P, hT, mix, wo, w1, w2, g, gf, hoT, identb, ntiles=17, final=False):
    wo_sb = P.sb("wo_sb", [128, 8, 1024], BF16)
    w1_sb = P.sb("w1_sb", [128, 8, 4096], BF16)
    w2_sb = P.sb("w2_sb", [128, 32, 1024], BF16)
    g_sb = P.sb("g_sb", [128, 8], F32)
    gf_sb = P.sb("gf_sb", [128, 8], F32)
    ones_bf = P.sb("ones_bf", [128, 128], BF16)
    idb = P.sb("idb", [128, 128], BF16)
    eps_t = P.sb("eps_t", [128, 1], F32)
    ht = P.sb("ht", [128, 8, 256], F32)
    mts = [P.sb("mt%d" % i, [128, 1024], BF16) for i in range(2)]
    mx = P.sb("mx", [128, 8, 256], BF16)
    uT = P.sb("uT", [128, 8, 256], BF16)
    sq = P.sb("sq", [128, 8, 256], BF16)
    hid = P.sb("hid", [128, 32, 256], BF16)
    sd = P.sb("sd", [128, 256], F32)
    rstd = P.sb("rstd", [128, 256], F32)
    rl = [P.sb("rl%d" % i, [128, 256], F32) for i in range(2)]
    ps_ss = P.ps("ps_ss")
    pss = [P.ps("ps%d" % i) for i in range(5)]
    ptr = [P.ps("ptr%d" % i, [128, 512], BF16) for i in range(2)]
    hTv = hT.rearrange("(k p) t -> p k t", p=128)
    hoTv = hoT.rearrange("(k p) t -> p k t", p=128)
    P.op("dve", "memset", dict(ap=ones_bf[:, :], constant=1.0), w=[("ones",)])
    P.op("dve", "memset", dict(ap=eps_t[:, :], constant=EPS), w=[("eps",)])
    P.dma(g_sb[:, :], g[:, :], w=[("g",)])
    P.dma(gf_sb[:, :], gf[:, :], w=[("gf",)])
    P.dma(idb[:, :], identb[:, :], w=[("idb",)])
    wov = wo.rearrange("(k p) c -> p k c", p=128)
    w1v = w1.rearrange("(k p) c -> p k c", p=128)
    w2v = w2.rearrange("(f p) c -> p f c", p=128)
    for k in range(8):
        P.dma(wo_sb[:, k, :], wov[:, k, :], w=[("wo", k)], q="pool")
    for k in range(8):
        P.dma(w1_sb[:, k, :], w1v[:, k, :], w=[("w1", k)], q="pool")
    for f in range(0, 32, 4):
        P.dma(w2_sb[:, f:f + 4, :], w2v[:, f:f + 4, :], w=[("w2", f // 4)], q="pool")
    pc = [0]

    def nextps():
        i = pc[0] % 5
        pc[0] += 1
        return i

    rc = 0
    mc = 0
    tc = 0
    for ti, (tok0, ntok) in enumerate(tiles256(ntiles)):
        P.dma(ht[:, :, :ntok], hTv[:, :, tok0:tok0 + ntok], w=[("ht",)])
        nsb = max(1, ntok // 128)
        for s in range(nsb):
            nt = min(128, ntok)
            mi = mc % 2
            mc += 1
            mt = mts[mi]
            P.dma(mt[:nt, :], mix[tok0 + s * 128: tok0 + s * 128 + nt, :], w=[("mt", mi)])
            for kq in range(2):
                ti2 = tc % 2
                tc += 1
                pt = ptr[ti2]
                for i in range(4):
                    kk = 4 * kq + i
                    P.op("pe", "transpose", dict(out=pt[:, i * 128:i * 128 + nt], in_=mt[:nt, kk * 128:(kk + 1) * 128],
                                                 identity=idb[:nt, :nt]),
                         r=[("mt", mi), ("idb",)], w=[("ptr", ti2)])
                P.op("act", "activation", dict(out=mx[:, 4 * kq:4 * kq + 4, s * 128:s * 128 + nt],
                                               in_=pt[:, :].rearrange("p (c t) -> p c t", t=128)[:, :, :nt], func=AF.Copy),
                     r=[("ptr", ti2)], w=[("mx", s, kq)])
        mxkeys = [("mx", s, kq) for s in range(nsb) for kq in range(2)]
        for m in range(8):
            pi = nextps()
            ps = pss[pi]
            for k in range(8):
                P.op("pe", "matmul", dict(out=ps[:, :ntok], lhsT=wo_sb[:, k, m * 128:(m + 1) * 128], rhs=mx[:, k, :ntok],
                                          start=(k == 0), stop=(k == 7)),
                     r=mxkeys + [("wo", k)], w=[("ps", pi)])
            P.op("dve", "tensor_tensor", dict(out=ht[:, m, :ntok], in0=ht[:, m, :ntok], in1=ps[:, :ntok], op=ALU.add),
                 r=[("ps", pi), ("ht",)], w=[("ht",)])
        emit_rmsnorm_T(P, ht, uT, ntok, g_sb, sq, ps_ss, rstd, sd, ones_bf, eps_t, ("ht",), ("uT",))
        for f in range(32):
            pi = nextps()
            ps = pss[pi]
            for k in range(8):
                P.op("pe", "matmul", dict(out=ps[:, :ntok], lhsT=w1_sb[:, k, f * 128:(f + 1) * 128], rhs=uT[:, k, :ntok],
                                          start=(k == 0), stop=(k == 7)),
                     r=[("uT",), ("w1", k)], w=[("ps", pi)])
            ri = rc % 2
            rc += 1
            P.op("act", "activation", dict(out=rl[ri][:, :ntok], in_=ps[:, :ntok], func=AF.Relu),
                 r=[("ps", pi)], w=[("rl", ri)])
            P.op("pool", "tensor_tensor", dict(out=hid[:, f, :ntok], in0=rl[ri][:, :ntok], in1=rl[ri][:, :ntok], op=ALU.mult),
                 r=[("rl", ri)], w=[("hid", f)])
        for m in range(8):
            pi = nextps()
            ps = pss[pi]
            for f in range(32):
                P.op("pe", "matmul", dict(out=ps[:, :ntok], lhsT=w2_sb[:, f, m * 128:(m + 1) * 128], rhs=hid[:, f, :ntok],
                                          start=(f == 0), stop=(f == 31)),
                     r=[("hid", f), ("w2", f // 4)], w=[("ps", pi)])
            P.op("dve", "tensor_tensor", dict(out=ht[:, m, :ntok], in0=ht[:, m, :ntok], in1=ps[:, :ntok], op=ALU.add),
                 r=[("ps", pi), ("ht",)], w=[("ht",)])
        if final:
            emit_rmsnorm_T(P, ht, ht, ntok, gf_sb, sq, ps_ss, rstd, sd, ones_bf, eps_t, ("ht",), ("ht",))
        P.dma(hoTv[:, :, tok0:tok0 + ntok], ht[:, :, :ntok], r=[("ht",)])


def emit_p2ab(P, fm, gK, gV, tmq, mix, cst, njobs=NJ, do_meta=True, lambda_init=0.2, nbis=NBIS):
    S = P.sb
    acc = S("acc", [128, TALL], F32)
    MB = S("MB", [128, TALL], BF16)
    MBN = S("MBN", [128, 8, 1024], BF16)
    MBNm = S("MBNm", [128, 8, 16], BF16)
    NB = S("NB", [128, 12, 1024], BF16)
    NBm0 = S("NBm0", [128, 12, 16], BF16)
    NBmm = S("NBmm", [16, 12, 16], BF16)
    dm = S("dm", [128, 512], F32)
    dtmp = S("dtmp", [128, 512], F32)
    b31s = S("b31s", [128, 12], F32)
    zcol = S("zcol", [128, 1], F32)
    half = S("half", [128, 1], F32)
    epsc = S("epsc", [128, 1], F32)
    idn = S("idn", [128, 128], BF16)
    lam_in = S("lam_in", [128, 128], F32)
    dn = S("dn", [128, 64], F32)
    kis = [S("ki%d" % i, [128, 512], BF16) for i in range(2)]
    kas = [S("ka%d" % i, [128, 4, 512], BF16) for i in range(2)]
    kbs = [S("kb%d" % i, [128, 2, 512], BF16) for i in range(2)]
    vas = [S("va%d" % i, [128, 4, 520], BF16) for i in range(2)]
    vbs = [S("vb%d" % i, [128, 4, 260], BF16) for i in range(2)]
    qa = S("qa", [128, 8, 128], BF16)
    qb = S("qb", [128, 8, 128], BF16)
    qi = S("qi", [128, 8, 128], BF16)
    wis = S("wis", [128, 8], F32)
    absw = S("absw", [128, 8], F32)
    sgn = S("sgn", [128, 8], F32)
    rbuf = [S("r%d" % i, [128, 512], F32) for i in range(3)]
    pTs = [S("pT%d" % i, [128, 512], BF16) for i in range(3)]
    sm = S("sm", [128, 32], F32)
    smu = S("smu", [128, 4], U32)
    abo = S("abo", [128, 768], BF16)
    fin = S("fin", [128, 512], F32)
    psI = [P.ps("psI%d" % i) for i in range(2)]
    psS = [P.ps("psS%d" % i) for i in range(2)]
    OA = [P.ps("OA%d" % i) for i in range(2)]
    OB = [P.ps("OB%d" % i) for i in range(2)]
    LO, HI, MID, CNT, RMIN, RTMP, LAM, NLAM = 0, 1, 2, 3, 4, 5, 6, 7
    gKv = [a.rearrange("(r f) t -> r f t", r=4) for a in gK]
    gVv = [a.rearrange("(r t) f -> r t f", r=4) for a in gV]
    KA0, KB0, KI0 = 0, FM_OFF["kb"] - KROW0, FM_OFF["ki"] - KROW0

    def smc(i):
        return sm[:, i:i + 1]

    P.dma(dm[:, :], cst["dmask"][:, :], w=[("dm",)])
    P.dma(NB[:, :, :], cst["nb"].rearrange("h p k -> p h k"), w=[("NB",)])
    P.dma(NBm0[:, :, :], cst["nbm0"].rearrange("h p k -> p h k"), w=[("NBm0",)])
    P.dma(NBmm[:, :, :], cst["nbmm"].rearrange("h p k -> p h k"), w=[("NBmm",)])
    P.dma(b31s[:, :], cst["b31"][:, :], w=[("b31",)])
    P.dma(idn[:, :], cst["identb"][:, :], w=[("idn",)])
    P.dma(lam_in[:, :], cst["lamb"][:, :], w=[("lam_in",)])
    P.dma(dn[:, :], cst["dnb"][:, :], w=[("dn",)])
    P.op("dve", "memset", dict(ap=zcol[:, :], constant=0.0), w=[("zcol",)])
    P.op("dve", "memset", dict(ap=half[:, :], constant=0.5), w=[("half",)])
    P.op("dve", "memset", dict(ap=epsc[:, :], constant=EPS), w=[("epsc",)])
    P.op("dve", "memset", dict(ap=qa[:, :, :], constant=0.0), w=[("qa", h) for h in range(8)])
    P.op("dve", "memset", dict(ap=qb[:, :, :], constant=0.0), w=[("qb", h) for h in range(8)])
    P.op("dve", "memset", dict(ap=qi[:, :, :], constant=0.0), w=[("qi", h) for h in range(8)])
    P.op("dve", "tensor_tensor", dict(out=fin[:, 0:32], in0=lam_in[:, 0:32], in1=lam_in[:, 32:64], op=ALU.mult),
         r=[("lam_in",)], w=[("fin",)])
    P.op("dve", "tensor_tensor", dict(out=fin[:, 32:64], in0=lam_in[:, 64:96], in1=lam_in[:, 96:128], op=ALU.mult),
         r=[("lam_in",), ("fin",)], w=[("fin",)])
    P.op("dve", "tensor_reduce", dict(out=sm[:, 8:10], in_=fin[:, 0:64].rearrange("p (a b) -> p a b", b=32),
                                      axis=AX.X, op=ALU.add), r=[("fin",)], w=[("sm", "l")])
    P.op("act", "activation", dict(out=sm[:, 10:12], in_=sm[:, 8:10], func=AF.Exp), r=[("sm", "l")], w=[("sm", "l2")])
    P.op("dve", "tensor_tensor", dict(out=smc(LAM), in0=sm[:, 11:12], in1=sm[:, 10:11], op=ALU.subtract),
         r=[("sm", "l2")], w=[("sm", "lam")])
    P.op("dve", "tensor_scalar", dict(out=smc(NLAM), in0=smc(LAM), scalar1=-float(lambda_init), scalar2=None,
                                      op0=ALU.add), r=[("sm", "lam")], w=[("sm", "nlam")])
    P.op("dve", "tensor_scalar", dict(out=dn[:, :], in0=dn[:, :], scalar1=float(1.0 - lambda_init), scalar2=None,
                                      op0=ALU.mult), r=[("dn",)], w=[("dn",)])

    tcount = [0]
    rcount = [0]
    pcount = [0]
    scount = [0]
    icount = [0]

    def attend(nq, tiles, meta_mode, j):
        for i in range(2):
            P.op("dve", "memset", dict(ap=OA[i][:nq, 0:260], constant=0.0), w=[("OA", i)])
            P.op("dve", "memset", dict(ap=OB[i][:nq, 0:260], constant=0.0), w=[("OB", i)])
        steps = [("t", g, near) for (g, near) in tiles] + [("m", None, None)]
        for (kind, g, near) in steps:
            tb = tcount[0] % 2
            tcount[0] += 1
            ka_t, kb_t, va_t, vb_t = kas[tb], kbs[tb], vas[tb], vbs[tb]
            if kind == "t":
                tix, t0 = tile_of_tok(16 + 128 * g)
                nkb, nk = 4, 128
                allr = lambda nm: [(nm, tb, r_) for r_ in range(4)]
                for c_ in range(4):
                    P.dma(ka_t[:, c_, :].rearrange("p (r t) -> p r t", r=4),
                          gKv[tix][:, KA0 + c_ * 128:KA0 + (c_ + 1) * 128, t0:t0 + 128].rearrange("r p t -> p r t"),
                          w=[("ka", tb, c_ + 10)])
                for c_ in range(2):
                    P.dma(kb_t[:, c_, :].rearrange("p (r t) -> p r t", r=4),
                          gKv[tix][:, KB0 + c_ * 128:KB0 + (c_ + 1) * 128, t0:t0 + 128].rearrange("r p t -> p r t"),
                          w=[("kb", tb, c_ + 10)])
                P.dma(va_t[:, :, :], gVv[tix][:, t0:t0 + 128, 0:520].rearrange("r p f -> p r f"), w=allr("va"))
                P.dma(vb_t[:, :, :], gVv[tix][:, t0:t0 + 128, 520:780].rearrange("r p f -> p r f"), w=allr("vb"))
            else:
                nkb, nk = 1, 16
                P.dma(ka_t[:, :, 0:16], gKv[0][0, KA0:KA0 + 512, 0:16].rearrange("(c p) t -> p c t", p=128), w=[("ka", tb, 0)])
                P.dma(kb_t[:, :, 0:16], gKv[0][0, KB0:KB0 + 256, 0:16].rearrange("(c p) t -> p c t", p=128), w=[("kb", tb, 0)])
                P.dma(va_t[0:16, 0, :], gVv[0][0, 0:16, 0:520], w=[("va", tb, 0)])
                P.dma(vb_t[0:16, 0, :], gVv[0][0, 0:16, 520:780], w=[("vb", tb, 0)])
            for hh in range(16):
                isA = hh < 8
                if isA:
                    h = hh
                    qz, qkey = qa[:, h, :nq], ("qa", h)
                    kt, kname, vt, vname = ka_t, "ka", va_t, "va"
                    kc_ = h // 2
                    Oap = OA[h // 4][:nq, (h % 4) * 65:(h % 4 + 1) * 65]
                    Okey = ("OA", h // 4)
                    hb = h
                else:
                    hc = hh - 8
                    h = hc // 2
                    qz, qkey = qb[:, hc, :nq], ("qb", hc)
                    kt, kname, vt, vname = kb_t, "kb", vb_t, "vb"
                    kc_ = h // 2
                    Oap = OB[hc // 4][:nq, (hc % 4) * 65:(hc % 4 + 1) * 65]
                    Okey = ("OB", hc // 4)
                    hb = 8 + h
                si = scount[0] % 2
                scount[0] += 1
                ps = psS[si]

                def mop(blk):
                    if kind == "t":
                        if near is not None:
                            if isA:
                                return MBN[:nq, h, near * 512 + blk * 128: near * 512 + (blk + 1) * 128], ("MBN", h)
                            return NB[:nq, hb, near * 512 + blk * 128: near * 512 + (blk + 1) * 128], ("NB",)
                        if isA:
                            return MB[:nq, g * 512 + blk * 128: g * 512 + (blk + 1) * 128], ("MB",)
                        return None, None
                    if meta_mode == "mm":
                        return NBmm[:nq, hb, :], ("NBmm",)
                    if meta_mode == "j0":
                        if isA:
                            return MBNm[:nq, h, :], ("MBNm",)
                        return NBm0[:nq, hb, :], ("NBm0",)
                    if isA:
                        return MB[:nq, 512 * (j + 1): 512 * (j + 1) + 16], ("MB",)
                    return None, None
                first = True
                for blk in range(nkb):
                    P.op("pe", "matmul", dict(out=ps[:nk, blk * 128:blk * 128 + nq],
                                              lhsT=kt[:, kc_, blk * 128:blk * 128 + nk], rhs=qz,
                                              start=first, stop=False, skip_group_check=True),
                         r=[(kname, tb, kc_ + 10 if kind == "t" else 0), qkey], w=[("psS", si)])
                    first = False
                for blk in range(nkb):
                    m_ap, m_key = mop(blk)
                    if m_ap is not None:
                        P.op("pe", "matmul", dict(out=ps[:nk, blk * 128:blk * 128 + nq], lhsT=m_ap, rhs=idn[:nq, :nq],
                                                  start=False, stop=False, skip_group_check=True),
                             r=[m_key, ("idn",)], w=[("psS", si)])
                usebias = (kind == "t" and near is None) or (kind == "m" and meta_mode == "far")
                bias_ap = b31s[:nk, hb:hb + 1] if usebias else zcol[:nk, 0:1]
                pi = pcount[0] % 3
                pcount[0] += 1
                pT = pTs[pi]
                ncol = (nkb - 1) * 128 + nq
                P.op("act", "activation", dict(out=pT[:nk, :ncol], in_=ps[:nk, :ncol], func=AF.Exp, bias=bias_ap),
                     r=[("psS", si), ("b31",), ("zcol",)], w=[("pT", pi)])
                for blk in range(nkb):
                    P.op("pe", "matmul", dict(out=Oap, lhsT=pT[:nk, blk * 128:blk * 128 + nq],
                                              rhs=vt[:nk, blk, h * 65:(h + 1) * 65],
                                              start=False, stop=False, skip_group_check=True),
                         r=[("pT", pi), (vname, tb, blk)], w=[Okey])

    def finalize(nq, tok0):
        for i in range(2):
            ov = OA[i][:nq, 0:260].rearrange("p (h f) -> p h f", f=65)
            P.op("dve", "reciprocal", dict(out=sm[:nq, 12 + 4 * i:16 + 4 * i], in_=ov[:, :, 64]),
                 r=[("OA", i)], w=[("sm", "ra", i)])
            for hl in range(4):
                h = 4 * i + hl
                P.op("dve", "tensor_scalar", dict(out=abo[:nq, h * 64:(h + 1) * 64], in0=ov[:, hl, 0:64],
                                                  scalar1=sm[:nq, 12 + h:13 + h], scalar2=None, op0=ALU.mult),
                     r=[("OA", i), ("sm", "ra", i)], w=[("abo", h)])
        for i in range(2):
            ov = OB[i][:nq, 0:260].rearrange("p (h f) -> p h f", f=65)
            P.op("dve", "reciprocal", dict(out=sm[:nq, 20 + 4 * i:24 + 4 * i], in_=ov[:, :, 64]),
                 r=[("OB", i)], w=[("sm", "rb", i)])
        rv = sm[:nq, 20:28].rearrange("p (h c) -> p h c", c=2)[:, :, 1]
        P.op("dve", "tensor_scalar", dict(out=rv, in0=rv, scalar1=sm[:nq, NLAM:NLAM + 1], scalar2=None, op0=ALU.mult),
             r=[("sm", "rb", 0), ("sm", "rb", 1), ("sm", "nlam")], w=[("sm", "rb", 0), ("sm", "rb", 1)])
        for h in range(4):
            i = h // 2
            ov = OB[i][:nq, 0:260].rearrange("p (h f) -> p h f", f=65)
            c0, c1 = (2 * h) % 4, (2 * h + 1) % 4
            t0 = fin[:nq, h * 64:(h + 1) * 64]
            bh = fin[:nq, 256 + h * 64:256 + (h + 1) * 64]
            P.op("dve", "tensor_scalar", dict(out=t0, in0=ov[:, c0, 0:64], scalar1=sm[:nq, 20 + 2 * h:21 + 2 * h],
                                              scalar2=None, op0=ALU.mult),
                 r=[("OB", i), ("sm", "rb", i)], w=[("fin", h)])
            P.op("dve", "scalar_tensor_tensor", dict(out=bh, in0=ov[:, c1, 0:64], scalar=sm[:nq, 21 + 2 * h:22 + 2 * h],
                                                     in1=t0, op0=ALU.mult, op1=ALU.add),
                 r=[("OB", i), ("sm", "rb", i), ("fin", h)], w=[("finb", h)])
            P.op("dve", "tensor_tensor", dict(out=t0, in0=bh, in1=bh, op=ALU.mult), r=[("finb", h)], w=[("fin", h)])
            P.op("dve", "tensor_reduce", dict(out=sm[:nq, 28 + h:29 + h], in_=t0, axis=AX.X, op=ALU.add),
                 r=[("fin", h)], w=[("sm", "ss", h)])
            P.op("act", "activation", dict(out=sm[:nq, 28 + h:29 + h], in_=sm[:nq, 28 + h:29 + h], func=AF.Sqrt,
                                           bias=epsc[:nq, 0:1], scale=1.0 / 64.0),
                 r=[("sm", "ss", h), ("epsc",)], w=[("sm", "ss", h)])
            P.op("dve", "reciprocal", dict(out=sm[:nq, 28 + h:29 + h], in_=sm[:nq, 28 + h:29 + h]),
                 r=[("sm", "ss", h)], w=[("sm", "ss", h)])
            P.op("dve", "scalar_tensor_tensor", dict(out=abo[:nq, 512 + h * 64:512 + (h + 1) * 64], in0=bh,
                                                     scalar=sm[:nq, 28 + h:29 + h], in1=dn[:nq, :],
                                                     op0=ALU.mult, op1=ALU.mult),
                 r=[("finb", h), ("sm", "ss", h), ("dn",)], w=[("abo", 8 + h)])
        P.dma(mix[tok0:tok0 + nq, 0:768], abo[:nq, :], r=[("abo", k) for k in range(12)])

    def load_q(tok0, nq, need_idx):
        for h in range(8):
            P.dma(qa[(h % 2) * 64:(h % 2) * 64 + 64, h, :nq],
                  fm[FM_OFF["qa"] + h * 64:FM_OFF["qa"] + (h + 1) * 64, tok0:tok0 + nq], w=[("qa", h)])
        for hc in range(8):
            h, c = hc // 2, hc % 2
            p0 = (h % 2) * 64 + c * 32
            r0 = FM_OFF["qb"] + h * 64 + c * 32
            P.dma(qb[p0:p0 + 32, hc, :nq], fm[r0:r0 + 32, tok0:tok0 + nq], w=[("qb", hc)])
        if need_idx:
            for h in range(8):
                P.dma(qi[(h % 2) * 64:(h % 2) * 64 + 64, h, :nq],
                      fm[FM_OFF["qi"] + h * 64:FM_OFF["qi"] + (h + 1) * 64, tok0:tok0 + nq], w=[("qi", h)])
            P.dma(wis[:nq, :], tmq[tok0:tok0 + nq, 512:520], w=[("wis",)])

    if do_meta:
        load_q(0, 16, False)
        attend(16, [], "mm", None)
        finalize(16, 0)

    for j in range(njobs):
        tok0 = 16 + 128 * j
        nq = 128
        n = 512 * (j + 1) + 16
        load_q(tok0, nq, True)
        P.op("act", "activation", dict(out=absw[:, :], in_=wis[:, :], func=AF.Abs, scale=float(C_IDX)),
             r=[("wis",)], w=[("absw",)])
        P.op("act", "activation", dict(out=sgn[:, :], in_=wis[:, :], func=AF.Sign), r=[("wis",)], w=[("sgn",)])
        acckeys = []
        for g in list(range(j + 1)) + ["m"]:
            kb_ = icount[0] % 2
            icount[0] += 1
            ki_t = kis[kb_]
            if g == "m":
                nk, c0 = 16, 512 * (j + 1)
                for e in range(2):
                    P.dma(ki_t[e * 64:(e + 1) * 64, 0:16], gKv[0][0, KI0:KI0 + 64, 0:16], w=[("ki", kb_, e)])
            else:
                nk, c0 = 512, 512 * g
                tix, t0 = tile_of_tok(16 + 128 * g)
                for e in range(2):
                    P.dma(ki_t[e * 64:(e + 1) * 64, :].rearrange("p (r t) -> p r t", r=4),
                          gKv[tix][:, KI0:KI0 + 64, t0:t0 + 128].rearrange("r p t -> p r t"),
                          w=[("ki", kb_, e, r_) for r_ in range(4)])
            kikeys = [("ki", kb_, e) for e in range(2)] + [("ki", kb_, e, r_) for e in range(2) for r_ in range(4)]
            akey = ("acc", g)
            acckeys.append(akey)
            for h in range(8):
                pi = (icount[0] * 8 + h) % 2
                ps = psI[pi]
                P.op("pe", "matmul", dict(out=ps[:, :nk], lhsT=qi[:, h, :], rhs=ki_t[:, :nk], start=True, stop=True),
                     r=[("qi", h)] + kikeys, w=[("psI", pi)])
                ri = rcount[0] % 3
                rcount[0] += 1
                rb = rbuf[ri]
                P.op("act", "activation", dict(out=rb[:, :nk], in_=ps[:, :nk], func=AF.Relu, scale=absw[:, h:h + 1]),
                     r=[("psI", pi), ("absw",)], w=[("r", ri)])
                if h == 0:
                    P.op("dve", "tensor_scalar", dict(out=acc[:, c0:c0 + nk], in0=rb[:, :nk], scalar1=sgn[:, 0:1],
                                                      scalar2=None, op0=ALU.mult),
                         r=[("r", ri), ("sgn",)], w=[akey])
                else:
                    P.op("dve", "scalar_tensor_tensor", dict(out=acc[:, c0:c0 + nk], in0=rb[:, :nk],
                                                             scalar=sgn[:, h:h + 1], in1=acc[:, c0:c0 + nk],
                                                             op0=ALU.mult, op1=ALU.add),
                         r=[("r", ri), ("sgn",), akey], w=[akey])
        dkey = ("acc", j)
        d0 = 512 * j
        P.op("dve", "scalar_tensor_tensor", dict(out=dtmp[:, :], in0=dm[:, :], scalar=-1.0, in1=acc[:, d0:d0 + 512],
                                                 op0=ALU.mult, op1=ALU.add), r=[("dm",), dkey], w=[("dtmp",)])
        P.op("dve", "tensor_reduce", dict(out=smc(RMIN), in_=dtmp[:, :], axis=AX.X, op=ALU.min),
             r=[("dtmp",)], w=[("sm", "rmin")])
        P.op("dve", "tensor_tensor", dict(out=acc[:, d0:d0 + 512], in0=acc[:, d0:d0 + 512], in1=dm[:, :], op=ALU.add),
             r=[("dm",), dkey], w=[dkey])
        P.op("dve", "tensor_reduce", dict(out=smc(RTMP), in_=acc[:, d0 + 512:n], axis=AX.X, op=ALU.min),
             r=acckeys, w=[("sm", "rtmp")])
        P.op("dve", "tensor_tensor", dict(out=smc(RMIN), in0=smc(RMIN), in1=smc(RTMP), op=ALU.min),
             r=[("sm", "rmin"), ("sm", "rtmp")], w=[("sm", "rmin")])
        if j > 0:
            P.op("dve", "tensor_reduce", dict(out=smc(RTMP), in_=acc[:, 0:d0], axis=AX.X, op=ALU.min),
                 r=acckeys, w=[("sm", "rtmp")])
            P.op("dve", "tensor_tensor", dict(out=smc(RMIN), in0=smc(RMIN), in1=smc(RTMP), op=ALU.min),
                 r=[("sm", "rmin"), ("sm", "rtmp")], w=[("sm", "rmin")])
        P.op("dve", "tensor_reduce", dict(out=smc(HI), in_=acc[:, 0:n], axis=AX.X, op=ALU.max),
             r=acckeys, w=[("sm", "hi")])
        P.op("dve", "tensor_copy", dict(out=smc(LO), in_=smc(RMIN)), r=[("sm", "rmin")], w=[("sm", "lo")])
        for it in range(nbis):
            P.op("dve", "scalar_tensor_tensor", dict(out=smc(MID), in0=smc(LO), scalar=smc(HI), in1=half[:, :],
                                                     op0=ALU.add, op1=ALU.mult),
                 r=[("sm", "lo"), ("sm", "hi"), ("half",)], w=[("sm", "mid")])
            P.op("dve", "tensor_scalar", dict(out=MB[:, 0:n], in0=acc[:, 0:n], scalar1=smc(MID), scalar2=0.0,
                                              op0=ALU.is_ge, op1=ALU.add, accum_out=smc(CNT)),
                 r=acckeys + [("sm", "mid")], w=[("MB",), ("sm", "cnt")])
            P.op("dve", "tensor_single_scalar", dict(out=smu[:, 0:1], in_=smc(CNT), scalar=255.5, op=ALU.is_ge),
                 r=[("sm", "cnt")], w=[("smu", 0)])
            P.op("dve", "tensor_single_scalar", dict(out=smu[:, 1:2], in_=smc(CNT), scalar=255.5, op=ALU.is_lt),
                 r=[("sm", "cnt")], w=[("smu", 1)])
            P.op("dve", "copy_predicated", dict(out=smc(LO), mask=smu[:, 0:1], data=smc(MID)),
                 r=[("smu", 0), ("sm", "mid")], w=[("sm", "lo")])
            P.op("dve", "copy_predicated", dict(out=smc(HI), mask=smu[:, 1:2], data=smc(MID)),
                 r=[("smu", 1), ("sm", "mid")], w=[("sm", "hi")])
        P.op("dve", "tensor_scalar", dict(out=MB[:, 0:n], in0=acc[:, 0:n], scalar1=smc(LO), scalar2=-BIG,
                                          op0=ALU.is_lt, op1=ALU.mult),
             r=acckeys + [("sm", "lo")], w=[("MB",)])
        if j == 0:
            tiles = [(0, 1)]
            for h in range(8):
                P.op("pool", "tensor_tensor", dict(out=MBN[:, h, 512:1024], in0=MB[:, 0:512], in1=NB[:, h, 512:1024],
                                                   op=ALU.add), r=[("MB",), ("NB",)], w=[("MBN", h)])
            for h in range(8):
                P.op("pool", "tensor_tensor", dict(out=MBNm[:, h, :], in0=MB[:, 512:528], in1=NBm0[:, h, :], op=ALU.add),
                     r=[("MB",), ("NBm0",), ("MBNm",)], w=[("MBNm",)])
            meta_mode = "j0"
        else:
            tiles = [(g, None) for g in range(j - 1)] + [(j - 1, 0), (j, 1)]
            for h in range(8):
                P.op("pool", "tensor_tensor", dict(out=MBN[:, h, :], in0=MB[:, 512 * (j - 1):512 * (j + 1)],
                                                   in1=NB[:, h, :], op=ALU.add),
                     r=[("MB",), ("NB",)], w=[("MBN", h)])
            meta_mode = "far"
        attend(nq, tiles, meta_mode, j)
        finalize(nq, tok0)


def emit_p2c(P, gKC, tmq, tmk, mix, cst, nblocks=128, do_meta=True):
    S = P.sb
    DT = S("DT", [128, 4, 128], F32)
    QD = S("QD", [64, 4, 128], F32)
    kd = S("kd", [128, 4], F32)
    kd16 = S("kd16", [16, 4], F32)
    oh = S("oh", [128, 4], F32)
    rn = S("rn", [128, 256], F32)
    idf = S("idf", [128, 128], F32)
    epsc = S("epsc", [128, 1], F32)
    kvb = [S("kvb%d" % i, [128, 512], F32) for i in range(2)]
    kdec = [S("kdec%d" % i, [128, 256], F32) for i in range(2)]
    ring = S("ring", [64, 4, 256], F32)
    ssel = S("ssel", [64, 256], F32)
    qtk = S("qtk", [128, 520], F32)
    ktk = S("ktk", [128, 512], F32)
    qT = S("qT", [64, 4, 128], F32)
    kT = S("kT", [64, 4, 128], F32)
    qd = S("qd", [64, 4, 128], F32)
    PT = [S("PT%d" % i, [128, 128], F32) for i in range(2)]
    ret = S("ret", [128, 256], F32)
    ss = S("ss", [128, 4], F32)
    yb = S("yb", [128, 256], F32)
    sg = S("sg", [128, 256], F32)
    cob = S("cob", [128, 256], BF16)
    psU = [P.ps("psU%d" % i) for i in range(2)]
    psA = [P.ps("psA%d" % i) for i in range(2)]
    psT = [P.ps("psT%d" % i) for i in range(2)]
    psO = P.ps("psO")
    gv = [a.rearrange("(r t) f -> r t f", r=4) for a in gKC]
    P.dma(DT[:, :, :], cst["DT"].rearrange("h j i -> j h i"), w=[("DT",)])
    P.dma(QD[:, :, :], cst["QD"][:, :, :], w=[("QD",)])
    P.dma(kd[:, :], cst["kd"][:, :], w=[("kd",)])
    P.dma(kd16[:, :], cst["kd16"][:, :], w=[("kd16",)])
    P.dma(oh[:, :], cst["oh"][:, :], w=[("oh",)])
    P.dma(rn[:, :], cst["rn"][:, :], w=[("rn",)])
    P.dma(idf[:, :], cst["identf"][:, :], w=[("idf",)])
    P.op("dve", "memset", dict(ap=epsc[:, :], constant=EPS), w=[("epsc",)])
    acount = [0]
    tcount = [0]

    def ret_block(tok0, L, use_state):
        P.dma(qtk[:L, :], tmq[tok0:tok0 + L, :], w=[("qtk",)])
        tix_, o_ = tile_of_tok(tok0)
        P.dma(ktk[:L, :], tmk[tix_][o_:o_ + L, :], w=[("ktk",)])
        for (src, skey, dst, dkey) in ((qtk, ("qtk",), qT, "qT"), (ktk, ("ktk",), kT, "kT")):
            for h in range(4):
                ti = tcount[0] % 2
                tcount[0] += 1
                P.op("pe", "transpose", dict(out=psT[ti][:64, :L], in_=src[:L, h * 64:(h + 1) * 64], identity=idf[:L, :L]),
                     r=[skey, ("idf",)], w=[("psT", ti)])
                P.op("act", "activation", dict(out=dst[:, h, :L], in_=psT[ti][:64, :L], func=AF.Copy),
                     r=[("psT", ti)], w=[(dkey, h)])
        qTk = [("qT", h) for h in range(4)]
        kTk = [("kT", h) for h in range(4)]
        if use_state:
            P.op("dve", "tensor_scalar", dict(out=ssel[:, :], in0=ring[:, 0, :], scalar1=oh[:64, 0:1], scalar2=None,
                                              op0=ALU.mult), r=[("ring", 0), ("oh",)], w=[("ssel",)])
            for c in range(1, 4):
                P.op("dve", "scalar_tensor_tensor", dict(out=ssel[:, :], in0=ring[:, c, :], scalar=oh[:64, c:c + 1],
                                                         in1=ssel[:, :], op0=ALU.mult, op1=ALU.add),
                     r=[("ring", c), ("oh",), ("ssel",)], w=[("ssel",)])
            P.op("dve", "tensor_tensor", dict(out=qd[:, :, :L], in0=qT[:, :, :L], in1=QD[:, :, :L], op=ALU.mult),
                 r=qTk + [("QD",)], w=[("qd",)])
        for h in range(4):
            ai = acount[0] % 2
            acount[0] += 1
            P.op("pe", "matmul", dict(out=psA[ai][:L, :L], lhsT=kT[:, h, :L], rhs=qT[:, h, :L], start=True, stop=True),
                 r=[("kT", h), ("qT", h)], w=[("psA", ai)])
            P.op("dve", "tensor_tensor", dict(out=PT[ai][:L, :L], in0=psA[ai][:L, :L], in1=DT[:L, h, :L], op=ALU.mult),
                 r=[("psA", ai), ("DT",)], w=[("PT", ai)])
            P.op("pe", "matmul", dict(out=psO[:L, h * 64:(h + 1) * 64], lhsT=PT[ai][:L, :L],
                                      rhs=ktk[:L, 256 + h * 64:256 + (h + 1) * 64],
                                      start=(h == 0), stop=(not use_state), skip_group_check=True),
                 r=[("PT", ai), ("ktk",)], w=[("psO",)])
            if use_state:
                P.op("pe", "matmul", dict(out=psO[:L, h * 64:(h + 1) * 64], lhsT=qd[:, h, :L],
                                          rhs=ssel[:, h * 64:(h + 1) * 64], start=False, stop=True,
                                          skip_group_check=True),
                     r=[("qd",), ("ssel",)], w=[("psO",)])
        P.op("act", "activation", dict(out=ret[:L, :], in_=psO[:L, 0:256], func=AF.Copy), r=[("psO",)], w=[("ret",)])
        ybk = [("yb", h) for h in range(4)]
        ssk = [("ss", h) for h in range(4)]
        P.op("dve", "tensor_tensor", dict(out=yb[:L, :], in0=ret[:L, :], in1=ret[:L, :], op=ALU.mult),
             r=[("ret",)], w=ybk)
        P.op("dve", "tensor_reduce", dict(out=ss[:L, :], in_=yb[:L, :].rearrange("p (h d) -> p h d", d=64),
                                          axis=AX.X, op=ALU.add), r=ybk, w=ssk)
        P.op("act", "activation", dict(out=ss[:L, :], in_=ss[:L, :], func=AF.Sqrt, bias=epsc[:L, 0:1], scale=1.0 / 64.0),
             r=ssk + [("epsc",)], w=ssk)
        P.op("dve", "reciprocal", dict(out=ss[:L, :], in_=ss[:L, :]), r=ssk, w=ssk)
        for h in range(4):
            P.op("dve", "scalar_tensor_tensor", dict(out=yb[:L, h * 64:(h + 1) * 64], in0=ret[:L, h * 64:(h + 1) * 64],
                                                     scalar=ss[:L, h:h + 1], in1=rn[:L, h * 64:(h + 1) * 64],
                                                     op0=ALU.mult, op1=ALU.mult),
                 r=[("ret",), ("ss", h), ("rn",)], w=[("yb", h)])
        P.op("act", "activation", dict(out=sg[:L, :], in_=qtk[:L, 256:512], func=AF.Silu), r=[("qtk",)], w=[("sg",)])
        P.op("pool", "tensor_tensor", dict(out=cob[:L, :], in0=yb[:L, :], in1=sg[:L, :], op=ALU.mult),
             r=ybk + [("sg",)], w=[("cob",)])
        P.dma(mix[tok0:tok0 + L, 768:1024], cob[:L, :], r=[("cob",)])

    if do_meta:
        ret_block(0, 16, False)
    for B in range(nblocks):
        bi = B % 2
        if B == 0:
            rk, t0, L, kdt = 0, 0, 16, kd16
        else:
            rk, t0, L, kdt = (B - 1) % 4, 16 + 128 * ((B - 1) // 4), 128, kd
        tix_, o_ = tile_of_tok(t0)
        P.dma(kvb[bi][:L, :], gv[tix_][rk, o_:o_ + L, :], w=[("kvb", bi)])
        for h in range(4):
            P.op("pool", "tensor_scalar", dict(out=kdec[bi][:L, h * 64:(h + 1) * 64], in0=kvb[bi][:L, h * 64:(h + 1) * 64],
                                               scalar1=kdt[:L, h:h + 1], scalar2=None, op0=ALU.mult),
                 r=[("kvb", bi), ("kd",), ("kd16",)], w=[("kdec", bi, h)])
        for h in range(4):
            P.op("pe", "matmul", dict(out=psU[bi][:64, h * 64:(h + 1) * 64], lhsT=kdec[bi][:L, h * 64:(h + 1) * 64],
                                      rhs=kvb[bi][:L, 256 + h * 64:256 + (h + 1) * 64], start=True, stop=True),
                 r=[("kdec", bi, h), ("kvb", bi)], w=[("psU", bi)])
        slot, pslot = B % 4, (B - 1) % 4
        if B == 0:
            P.op("dve", "tensor_copy", dict(out=ring[:, slot, :], in_=psU[bi][:64, 0:256]),
                 r=[("psU", bi)], w=[("ring", slot)])
        else:
            for h in range(4):
                P.op("dve", "scalar_tensor_tensor", dict(out=ring[:, slot, h * 64:(h + 1) * 64],
                                                         in0=ring[:, pslot, h * 64:(h + 1) * 64],
                                                         scalar=float(GAM[h] ** L), in1=psU[bi][:64, h * 64:(h + 1) * 64],
                                                         op0=ALU.mult, op1=ALU.add),
                     r=[("ring", pslot), ("psU", bi), ("ring", slot)], w=[("ring", slot)])
        if B % 4 == 3:
            ret_block(16 + 128 * (B // 4), 128, True)


def build_fused(cfg=None):
    cfg = cfg or {}
    P = Prog()
    D = P.dram
    x_in = D("hT0", [1024, NT], F32, "ExternalInput") if 0 in cfg.get("layers", (0, 1)) else None
    cs = D("cs", [NT, 512], F32, "ExternalInput")
    out = D("outT", [1024, NT], F32, "ExternalOutput")
    cst = dict(
        dmask=D("dmask", [128, 512], F32, "ExternalInput"),
        nb=D("nb", [12, 128, 1024], BF16, "ExternalInput"),
        nbm0=D("nbm0", [12, 128, 16], BF16, "ExternalInput"),
        nbmm=D("nbmm", [12, 16, 16], BF16, "ExternalInput"),
        b31=D("b31", [128, 12], F32, "ExternalInput"),
        identb=D("identb", [128, 128], BF16, "ExternalInput"),
        identf=D("identf", [128, 128], F32, "ExternalInput"),
        DT=D("DT", [4, 128, 128], F32, "ExternalInput"),
        QD=D("QD", [64, 4, 128], F32, "ExternalInput"),
        kd=D("kd", [128, 4], F32, "ExternalInput"),
        kd16=D("kd16", [16, 4], F32, "ExternalInput"),
        oh=D("oh", [128, 4], F32, "ExternalInput"),
    )
    gf = D("gf", [128, 8], F32, "ExternalInput")
    layers = cfg.get("layers", (0, 1))
    hmid = None
    if len(layers) == 2:
        hmid = D("hmid", [1024, NT], F32, "Internal")
    elif layers[0] == 1:
        hmid = D("hmid", [1024, NT], F32, "ExternalInput")
    hT = x_in if layers[0] == 0 else hmid
    for l in layers:
        w_in = D("w_in%d" % l, [1024, 3912], F32, "ExternalInput")
        gm = D("gm%d" % l, [128, 8], F32, "ExternalInput")
        wo = D("wo%d" % l, [1024, 1024], F32, "ExternalInput")
        w1 = D("w1_%d" % l, [1024, 4096], F32, "ExternalInput")
        w2 = D("w2_%d" % l, [4096, 1024], F32, "ExternalInput")
        gff = D("gff%d" % l, [128, 8], F32, "ExternalInput")
        cl = dict(cst)
        cl["lamb"] = D("lamb%d" % l, [128, 128], F32, "ExternalInput")
        cl["dnb"] = D("dnb%d" % l, [128, 64], F32, "ExternalInput")
        cl["rn"] = D("rn%d" % l, [128, 256], F32, "ExternalInput")
        fmo = D("fmo%d" % l, [NFM, NT], BF16, "Internal")
        tls = tiles_of(9)
        ksl = [D("ksl%d_%d" % (l, i), [NKROW, n_], BF16, "Internal") for i, (_, n_) in enumerate(tls)]
        tmb = [D("tmb%d_%d" % (l, i), [n_, NVB], BF16, "Internal") for i, (_, n_) in enumerate(tls)]
        tmk = [D("tmk%d_%d" % (l, i), [n_, NKC], F32, "Internal") for i, (_, n_) in enumerate(tls)]
        tmq = D("tmq%d" % l, [NT, NQC], F32, "Internal")
        gK = [D("gK%d_%d" % (l, i), [4 * NKROW, n_], BF16, "Internal") for i, (_, n_) in enumerate(tls)]
        gV = [D("gV%d_%d" % (l, i), [4 * n_, NVB], BF16, "Internal") for i, (_, n_) in enumerate(tls)]
        gKC = [D("gKC%d_%d" % (l, i), [4 * n_, NKC], F32, "Internal") for i, (_, n_) in enumerate(tls)]
        mix = D("mix%d" % l, [NT, 1024], BF16, "Internal")
        lambda_init = 0.8 - 0.6 * _math.exp(-0.3 * l)
        with P.phase():
            emit_p1(P, hT, w_in, gm, cs, fmo, ksl, tmb, tmk, tmq, gK, gV, gKC, cfg.get("p1_tiles", 9))
        with P.phase():
            emit_p2ab(P, fmo, gK, gV, tmq, mix, cl, cfg.get("njobs", NJ), True, lambda_init, cfg.get("nbis", NBIS))
        with P.phase():
            emit_p2c(P, gKC, tmq, tmk, mix, cl, cfg.get("nblocks", 128), True)
        with P.phase():
            emit_p3(P, hT, mix, wo, w1, w2, gff, gf, out if (l == 1 or len(layers) == 1) else hmid, cst["identb"],
                    cfg.get("p3_tiles", 17), final=(l == 1))
        hT = hmid
    return P.finish()


def rel_bucket_np(dist):
    n = np.maximum(dist, 0)
    nf = np.maximum(n, 1).astype(np.float32)
    large = 16 + (np.log(nf / np.float32(16)) / np.float32(_math.log(128 / 16)) * np.float32(16)).astype(np.int32)
    large = np.minimum(large, 31)
    return np.where(n < 16, n, large)


def local_positions(cc):
    pos = [np.arange(16)]
    for j in range(NJ):
        pos.append(16 + 128 * (4 * j + cc) + np.arange(128))
    return np.concatenate(pos)


def rope_table(cc):
    pos = local_positions(cc).astype(np.float32)
    inv = (np.float32(10000.0) ** (-np.arange(0, 64, 2, dtype=np.float32) / np.float32(64))).astype(np.float32)
    ang = pos[:, None] * inv[None, :]
    cos = np.cos(ang).astype(np.float32)
    sin = np.sin(ang).astype(np.float32)
    return np.ascontiguousarray(np.concatenate([np.tile(cos, (1, 8)), np.tile(sin, (1, 8))], axis=1))


def attn_consts(rel_bias, cc):
    i = np.arange(128)
    nb = np.empty((12, 128, 1024), np.float32)
    dmask = np.empty((128, 512), np.float32)
    for near in range(2):
        for cp in range(4):
            dR = (4 + cc - cp) if near == 0 else (cc - cp)
            dist = 128 * dR + i[:, None] - i[None, :]
            vis = dist >= 0
            bk = rel_bucket_np(dist)
            for hb in range(12):
                vals = rel_bias[bk, hb]
                nb[hb, :, near * 512 + cp * 128: near * 512 + (cp + 1) * 128] = np.where(vis, vals, np.float32(-BIG))
            if near == 1:
                dmask[:, cp * 128:(cp + 1) * 128] = np.where(vis, np.float32(0.0), np.float32(-BIG))
    s = np.arange(16)
    dist = 16 + 128 * cc + i[:, None] - s[None, :]
    bk = rel_bucket_np(dist)
    nbm0 = np.stack([rel_bias[bk, hb] for hb in range(12)]).astype(np.float32)
    dist = s[:, None] - s[None, :]
    bk = rel_bucket_np(dist)
    nbmm = np.stack([np.where(dist >= 0, rel_bias[bk, hb], np.float32(-BIG)) for hb in range(12)]).astype(np.float32)
    b31 = np.ascontiguousarray(np.broadcast_to(rel_bias[31][None, :], (128, 12))).astype(np.float32)
    return dict(dmask=dmask, nb=nb.astype(NP_BF16), nbm0=nbm0.astype(NP_BF16), nbmm=nbmm.astype(NP_BF16), b31=b31)


def ret_consts():
    i = np.arange(128)
    DT = np.zeros((4, 128, 128), np.float64)
    QD = np.zeros((64, 4, 128), np.float64)
    kd = np.zeros((128, 4), np.float64)
    kd16 = np.zeros((16, 4), np.float64)
    for h in range(4):
        g = GAM[h]
        d = i[None, :] - i[:, None]
        DT[h] = np.where(d >= 0, g ** np.maximum(d, 0), 0.0) / 8.0
        QD[:, h, :] = (g ** (i + 1.0))[None, :]
        kd[:, h] = g ** (127.0 - i) / 8.0
        kd16[:, h] = g ** (15.0 - np.arange(16)) / 8.0
    return dict(DT=DT.astype(np.float32), QD=QD.astype(np.float32), kd=kd.astype(np.float32), kd16=kd16.astype(np.float32))


def garr(v):
    return np.ascontiguousarray(np.asarray(v, np.float32).reshape(8, 128).T)


def shard_x(x, meta):
    hTs = []
    for c in range(8):
        b, cc = c // 4, c % 4
        xb = np.asarray(x[b], np.float32).reshape(NJ, 4, 128, 1024)[:, cc].reshape(NJ * 128, 1024)
        hTs.append(np.ascontiguousarray(np.concatenate([np.asarray(meta, np.float32), xb], axis=0).T))
    return hTs


def make_in_maps(inp):
    rel_bias = np.asarray(inp["rel_bias"], np.float32)
    hTs = shard_x(inp["x"], inp["meta"])
    rc = ret_consts()
    common = dict(identb=np.eye(128, dtype=np.float32).astype(NP_BF16), identf=np.eye(128, dtype=np.float32),
                  gf=garr(inp["final_norm"]), **rc)
    for l in range(2):
        common["w_in%d" % l] = np.ascontiguousarray(np.asarray(inp["w_in"][l], np.float32)[:, W_PERM])
        common["gm%d" % l] = garr(inp["norm_mix"][l])
        common["wo%d" % l] = np.ascontiguousarray(np.asarray(inp["w_out"][l], np.float32))
        common["w1_%d" % l] = np.ascontiguousarray(np.asarray(inp["w_ff1"][l], np.float32))
        common["w2_%d" % l] = np.ascontiguousarray(np.asarray(inp["w_ff2"][l], np.float32))
        common["gff%d" % l] = garr(inp["norm_ff"][l])
        common["lamb%d" % l] = np.ascontiguousarray(np.broadcast_to(
            np.asarray(inp["diff_lambda"][l], np.float32).reshape(1, 128), (128, 128)))
        common["dnb%d" % l] = np.ascontiguousarray(np.broadcast_to(
            np.asarray(inp["diff_norm"][l], np.float32).reshape(1, 64), (128, 64)))
        common["rn%d" % l] = np.ascontiguousarray(np.broadcast_to(
            np.asarray(inp["ret_norm"][l], np.float32).reshape(1, 256), (128, 256)))
    ropes = [rope_table(cc) for cc in range(4)]
    acs = [attn_consts(rel_bias, cc) for cc in range(4)]
    maps = []
    for c in range(8):
        cc = c % 4
        oh = np.zeros((128, 4), np.float32)
        oh[:, cc] = 1.0
        m = dict(common)
        m.update(acs[cc])
        m.update(hT0=hTs[c], cs=ropes[cc], oh=oh)
        maps.append(m)
    return maps


_NC_CACHE = {}


N_LAUNCH = 2


def run_fused(inp, cfg=None):
    cfg = dict(cfg or {})
    maps = make_in_maps(inp)
    if N_LAUNCH == 1:
        plans = [(0, 1)]
    else:
        plans = [(0,), (1,)]
    outs = None
    for layers in plans:
        c2 = dict(cfg)
        c2["layers"] = layers
        key = tuple(sorted(c2.items()))
        if key not in _NC_CACHE:
            _NC_CACHE[key] = build_fused(c2)
        if outs is not None:
            for c in range(8):
                maps[c]["hmid"] = outs[c]
        per_layer = ["w_in%d", "gm%d", "wo%d", "w1_%d", "w2_%d", "gff%d", "lamb%d", "dnb%d", "rn%d"]
        drop = set()
        for l in (0, 1):
            if l not in layers:
                drop |= {n % l for n in per_layer}
        if 0 not in layers:
            drop.add("hT0")
        if len(layers) == 2:
            drop.add("hmid")
        use = [{k: v for k, v in m.items() if k not in drop} for m in maps]
        res = run(_NC_CACHE[key], use)
        outs = [r["outT"] for r in res]
    return outs


def kernel(x, meta, rel_bias, w_in, norm_mix, diff_lambda, diff_norm, ret_norm, w_out, norm_ff, w_ff1, w_ff2, final_norm):
    inp = dict(x=x, meta=meta, rel_bias=rel_bias, w_in=w_in, norm_mix=norm_mix, diff_lambda=diff_lambda,
               diff_norm=diff_norm, ret_norm=ret_norm, w_out=w_out, norm_ff=norm_ff, w_ff1=w_ff1, w_ff2=w_ff2,
               final_norm=final_norm)
    inp = {k: np.asarray(v) for k, v in inp.items()}
    outs = run_fused(inp)
    out = np.empty((2, 16384, 1024), np.float32)
    for c in range(8):
        b, cc = c // 4, c % 4
        out[b].reshape(NJ, 4, 128, 1024)[:, cc] = outs[c][:, 16:].T.reshape(NJ, 128, 1024)
    return out
```

```python
import contextlib
import math as _math
import numpy as np
import concourse.bass as bass
import concourse.mybir as mybir
from concourse.bass_utils import run_bass_kernel_spmd

try:
    import ml_dtypes as _mld
    NP_BF16 = _mld.bfloat16
except Exception:
    NP_BF16 = None

F32 = mybir.dt.float32
BF16 = mybir.dt.bfloat16
U32 = mybir.dt.uint32
AF = mybir.ActivationFunctionType
ALU = mybir.AluOpType
AX = mybir.AxisListType

NSLOT = 24


class Prog:
    def __init__(self):
        self.nc = nc = bass.Bass("TRN2", target_bir_lowering=False)
        self.top = contextlib.ExitStack()
        self.cur = self.top
        self.engs = {"pe": nc.tensor, "act": nc.scalar, "dve": nc.vector, "pool": nc.gpsimd, "sp": nc.sync}
        self.sems = {e: self.top.enter_context(nc.semaphore("s_" + e)) for e in ("pe", "act", "dve", "pool")}
        self.cnt = {e: 0 for e in self.sems}
        self.slots = {q: [self.top.enter_context(nc.semaphore("d_%s_%d" % (q, s))) for s in range(NSLOT)]
                      for q in ("sp", "pool")}
        self.slotval = {q: [0] * NSLOT for q in self.slots}
        self.slotnext = {q: 0 for q in self.slots}
        self.ccs = []
        self.waited = {e: {} for e in self.engs}
        self.lastw = {}
        self.readers = {}
        self.nph = 0
        self.nuniq = 0

    def dram(self, name, shape, dt, kind):
        return self.nc.dram_tensor(name, list(shape), dt, kind=kind).ap()

    def sb(self, name, shape, dt):
        self.nuniq += 1
        return self.cur.enter_context(self.nc.sbuf_tensor("s%d_%s" % (self.nuniq, name), list(shape), dt))

    def ps(self, name, shape=(128, 512), dt=F32):
        self.nuniq += 1
        return self.cur.enter_context(self.nc.psum_tensor("p%d_%s" % (self.nuniq, name), list(shape), dt))

    @contextlib.contextmanager
    def phase(self):
        old = self.cur
        self.cur = contextlib.ExitStack()
        try:
            yield
        finally:
            self.barrier()
            self.cur.close()
            self.cur = old

    @staticmethod
    def _excl(k):
        return isinstance(k, tuple) and isinstance(k[0], str) and (k[0].startswith("ps") or k[0] in ("OA", "OB"))

    def _deps(self, eng, r, w, is_compute):
        w = tuple(w) + tuple(k for k in r if self._excl(k) and k not in w)
        toks = []
        for k in r:
            if k in self.lastw:
                toks.append(self.lastw[k])
        for k in w:
            if k in self.lastw:
                t = self.lastw[k]
                toks.append(t)
            for t in self.readers.get(k, ()):
                toks.append(t)
        need = {}
        for (sem, val, src) in toks:
            if is_compute and eng == "pe" and src == "pe":
                continue
            if is_compute and src == eng and val <= self.cnt[eng] - 4:
                continue
            if need.get(id(sem), (None, 0))[1] < val:
                need[id(sem)] = (sem, val)
        return w, need

    def _emit_waits(self, eng, need):
        E = self.engs[eng]
        wd = self.waited[eng]
        for sid, (sem, val) in need.items():
            if wd.get(sid, 0) >= val:
                continue
            E.wait_ge(sem, val)
            wd[sid] = val

    def _record(self, r, w, tok):
        for k in w:
            self.lastw[k] = tok
            self.readers[k] = []
        for k in r:
            if k not in w:
                self.readers.setdefault(k, []).append(tok)

    def op(self, eng, name, kw, r=(), w=()):
        w, need = self._deps(eng, r, w, True)
        self._emit_waits(eng, need)
        ins = getattr(self.engs[eng], name)(**kw)
        self.cnt[eng] += 1
        ins.then_inc(self.sems[eng], 1)
        self._record(r, w, (self.sems[eng], self.cnt[eng], eng))

    def dma(self, out, in_, r=(), w=(), q="sp", **kw):
        w, need = self._deps(q, r, w, False)
        si = self.slotnext[q]
        self.slotnext[q] = (si + 1) % NSLOT
        sem = self.slots[q][si]
        pv = self.slotval[q][si]
        if pv > 0 and need.get(id(sem), (None, 0))[1] < pv:
            need[id(sem)] = (sem, pv)
        self._emit_waits(q, need)
        ins = self.engs[q].dma_start(out=out, in_=in_, **kw)
        self.slotval[q][si] += 16
        ins.then_inc(sem, 16)
        self._record(r, w, (sem, self.slotval[q][si], "dma"))

    def coll(self, in_ap, out_ap, groups, r=()):
        _, need = self._deps("pool", r, (), False)
        self._emit_waits("pool", need)
        if not self.ccs:
            self.ccs = [self.top.enter_context(self.nc.semaphore("cc%d" % i)) for i in range(8)]
            self.ccval = [0] * 8
            self.ccn = 0
        i = self.ccn % 8
        self.ccn += 1
        sem = self.ccs[i]
        if self.ccval[i] > 0 and self.waited["pool"].get(id(sem), 0) < self.ccval[i]:
            self.nc.gpsimd.wait_ge(sem, self.ccval[i])
            self.waited["pool"][id(sem)] = self.ccval[i]
        ins = self.nc.gpsimd.collective_compute("AllGather", ALU.bypass, replica_groups=groups,
                                                ins=[in_ap], outs=[out_ap])
        ins.then_inc(sem)
        self.ccval[i] += 1

    def barrier(self):
        for eng, E in self.engs.items():
            wd = self.waited[eng]
            for e2, sem in self.sems.items():
                if self.cnt[e2] > 0 and wd.get(id(sem), 0) < self.cnt[e2]:
                    E.wait_ge(sem, self.cnt[e2])
                    wd[id(sem)] = self.cnt[e2]
            for q in self.slots:
                for si in range(NSLOT):
                    v = self.slotval[q][si]
                    sem = self.slots[q][si]
                    if v > 0 and wd.get(id(sem), 0) < v:
                        E.wait_ge(sem, v)
                        wd[id(sem)] = v
            for i, sem in enumerate(self.ccs):
                v = self.ccval[i]
                if v > 0 and wd.get(id(sem), 0) < v:
                    E.wait_ge(sem, v)
                    wd[id(sem)] = v
        self.lastw.clear()
        self.readers.clear()

    def finish(self):
        self.barrier()
        self.top.close()
        return self.nc


def run(prog_nc, in_maps):
    res = run_bass_kernel_spmd(prog_nc, in_maps, core_ids=list(range(len(in_maps))))
    return res.results


NT = 4112
NJ = 32
TALL = 16400
EPS = 1e-6
NBIS = 24
BIG = 30000.0
C_IDX = (8 ** -0.5) * (64 ** -0.5)
GAM = [1.0 - 2.0 ** (-5.0 - h) for h in range(4)]
GROUPS = [[0, 1, 2, 3], [4, 5, 6, 7]]
_OFF = dict(qa=0, ka=512, va=1024, qi=1536, ki=2048, wi=2112, qb=2120, kb=2376, vb=2632,
            qc=2888, kc=3144, vc=3400, gc=3656)
_SZ = dict(qa=512, ka=512, va=512, qi=512, ki=64, wi=8, qb=256, kb=256, vb=256, qc=256,
           kc=256, vc=256, gc=256)
FM_ORDER = ["qa", "qi", "qb", "ka", "kb", "ki"]
TM_ORDER = ["va", "vb", "kc", "vc", "qc", "gc", "wi"]
FM_OFF, TM_OFF = {}, {}
_o = 0
for _n in FM_ORDER:
    FM_OFF[_n] = _o
    _o += _SZ[_n]
NFM = _o
KROW0 = FM_OFF["ka"]
NKROW = NFM - KROW0
_o = 0
for _n in TM_ORDER:
    TM_OFF[_n] = _o
    _o += _SZ[_n]
NTM = _o
W_PERM = np.concatenate([np.arange(_OFF[n], _OFF[n] + _SZ[n]) for n in FM_ORDER + TM_ORDER])
NVB = 780
NKC = 512
NQC = 520


def tiles_of(ntiles):
    t = [(0, 16)]
    for i in range(8):
        t.append((16 + 512 * i, 512))
    return t[:ntiles]


def tiles256(ntiles):
    t = [(0, 16)]
    for i in range(16):
        t.append((16 + 256 * i, 256))
    return t[:ntiles]


def emit_rmsnorm_T(P, ht, out, ntok, g_sb, sq, ps_ss, rstd, sd, ones_bf, eps_t, htkey, outkey):
    P.op("act", "activation", dict(out=sq[:, :, :ntok], in_=ht[:, :, :ntok], func=AF.Square),
         r=[htkey], w=[("sq",)])
    for k in range(8):
        P.op("pe", "matmul", dict(out=ps_ss[:, :ntok], lhsT=ones_bf[:, :], rhs=sq[:, k, :ntok],
                                  start=(k == 0), stop=(k == 7)),
             r=[("sq",), ("ones",)], w=[("ps_ss",)])
    P.op("act", "activation", dict(out=sd[:, :ntok], in_=ps_ss[:, :ntok], func=AF.Sqrt,
                                   bias=eps_t[:, 0:1], scale=1.0 / 1024.0),
         r=[("ps_ss",), ("eps",)], w=[("sd",)])
    P.op("dve", "reciprocal", dict(out=rstd[:, :ntok], in_=sd[:, :ntok]), r=[("sd",)], w=[("rstd",)])
    for k in range(8):
        P.op("dve", "scalar_tensor_tensor", dict(out=out[:, k, :ntok], in0=ht[:, k, :ntok],
                                                 scalar=g_sb[:, k:k + 1], in1=rstd[:, :ntok],
                                                 op0=ALU.mult, op1=ALU.mult),
             r=[htkey, ("rstd",), ("g",)], w=[outkey])


def tile_of_tok(t0):
    if t0 < 16:
        return 0, t0
    return 1 + (t0 - 16) // 512, (t0 - 16) % 512


def emit_p1(P, hT, w, g, cs, fmo, ksl, tmb, tmk, tmq, gK, gV, gKC, ntiles=9):
    w_sb = P.sb("w_sb", [128, 8, 3912], BF16)
    g_sb = P.sb("g_sb", [128, 8], F32)
    ones_bf = P.sb("ones_bf", [128, 128], BF16)
    eps_t = P.sb("eps_t", [128, 1], F32)
    hts = [P.sb("ht%d" % i, [128, 8, 512], F32) for i in range(2)]
    uTs = [P.sb("uT%d" % i, [128, 8, 512], BF16) for i in range(2)]
    sq = P.sb("sq", [128, 8, 512], BF16)
    sd = P.sb("sd", [128, 512], F32)
    rstd = P.sb("rstd", [128, 512], F32)
    fms = [P.sb("fm%d" % i, [128, 512], BF16) for i in range(4)]
    tks = [P.sb("tk%d" % i, [128, NKC], F32) for i in range(2)]
    tqs = [P.sb("tq%d" % i, [128, NQC], F32) for i in range(2)]
    tbs = [P.sb("tb%d" % i, [128, 12, 65], BF16) for i in range(2)]
    css = [P.sb("cs%d" % i, [128, 512], F32) for i in range(2)]
    rt = P.sb("rt", [128, 4, 256], F32)
    ps_ss = P.ps("ps_ss")
    pss = [P.ps("ps%d" % i) for i in range(6)]
    hTv = hT.rearrange("(k p) t -> p k t", p=128)
    wv = w.rearrange("(k p) c -> p k c", p=128)
    P.op("dve", "memset", dict(ap=ones_bf[:, :], constant=1.0), w=[("ones",)])
    P.op("dve", "memset", dict(ap=eps_t[:, :], constant=EPS), w=[("eps",)])
    P.dma(g_sb[:, :], g[:, :], w=[("g",)])
    for i in range(2):
        P.op("dve", "memset", dict(ap=tbs[i][:, :, :], constant=1.0), w=[("tb", i, 0), ("tb", i, 1)])
    for k in range(8):
        P.dma(w_sb[:, k, :], wv[:, k, :], w=[("w", k)], q="pool")
    scale_of = {"qa": 0.125, "qb": 32.0 ** -0.5}
    fm_groups = []
    for n in FM_ORDER:
        for r0 in range(0, _SZ[n], 128):
            fm_groups.append((FM_OFF[n] + r0, min(128, _SZ[n] - r0), scale_of.get(n, 1.0)))
    tm_groups = [(0, 512), (512, 512), (1024, 512), (1536, 264)]
    pc = [0]
    fc = [0]
    sc_ = [0]

    def nextps():
        i = pc[0] % 6
        pc[0] += 1
        return i

    for ti, (tok0, ntok) in enumerate(tiles_of(ntiles)):
        b = ti % 2
        ht, uT = hts[b], uTs[b]
        P.dma(ht[:, :, :ntok], hTv[:, :, tok0:tok0 + ntok], w=[("ht", b)])
        emit_rmsnorm_T(P, ht, uT, ntok, g_sb, sq, ps_ss, rstd, sd, ones_bf, eps_t, ("ht", b), ("uT", b))
        for (r0, M, sc) in fm_groups:
            pi = nextps()
            ps = pss[pi]
            for k in range(8):
                P.op("pe", "matmul", dict(out=ps[:M, :ntok], lhsT=w_sb[:, k, r0:r0 + M], rhs=uT[:, k, :ntok],
                                          start=(k == 0), stop=(k == 7)),
                     r=[("uT", b), ("w", k)], w=[("ps", pi)])
            fi = fc[0] % 4
            fc[0] += 1
            fm = fms[fi]
            P.op("act", "activation", dict(out=fm[:M, :ntok], in_=ps[:M, :ntok], func=AF.Copy, scale=sc),
                 r=[("ps", pi)], w=[("fm", fi)])
            if r0 >= KROW0:
                P.dma(ksl[ti][r0 - KROW0:r0 - KROW0 + M, 0:ntok], fm[:M, :ntok], r=[("fm", fi)], w=[("dK", ti, r0)])
            else:
                P.dma(fmo[r0:r0 + M, tok0:tok0 + ntok], fm[:M, :ntok], r=[("fm", fi)])
        nsb = max(1, ntok // 128)
        for s in range(nsb):
            nt = min(128, ntok)
            t0 = tok0 + s * 128
            si = sc_[0] % 2
            sc_[0] += 1
            tk, tq, tb, cst = tks[si], tqs[si], tbs[si], css[si]
            P.dma(cst[:nt, :], cs[t0:t0 + nt, :], w=[("cs", si)])
            for gi, (c0, ncol) in enumerate(tm_groups):
                pi = nextps()
                ps = pss[pi]
                for k in range(8):
                    P.op("pe", "matmul", dict(out=ps[:nt, :ncol], lhsT=uT[:, k, s * 128:s * 128 + nt],
                                              rhs=w_sb[:, k, NFM + c0:NFM + c0 + ncol],
                                              start=(k == 0), stop=(k == 7)),
                         r=[("uT", b), ("w", k)], w=[("ps", pi)])
                if gi == 0:
                    P.op("dve", "tensor_copy", dict(out=tb[:nt, 0:8, 0:64],
                                                    in_=ps[:nt, 0:512].rearrange("p (h d) -> p h d", d=64)),
                         r=[("ps", pi)], w=[("tb", si, 0)])
                elif gi == 1:
                    P.op("act", "activation", dict(out=tb[:nt, 8:12, 0:64],
                                                   in_=ps[:nt, 0:256].rearrange("p (h d) -> p h d", d=64), func=AF.Copy),
                         r=[("ps", pi)], w=[("tb", si, 1)])
                    emit_rotary(P, ps[:nt, 256:512], tk[:nt, 0:256], cst, nt, rt, ("ps", pi), ("cs", si), ("tk", si, 0))
                elif gi == 2:
                    P.op("act", "activation", dict(out=tk[:nt, 256:512], in_=ps[:nt, 0:256], func=AF.Copy),
                         r=[("ps", pi)], w=[("tk", si, 1)])
                    emit_rotary(P, ps[:nt, 256:512], tq[:nt, 0:256], cst, nt, rt, ("ps", pi), ("cs", si), ("tq", si, 0))
                else:
                    P.op("act", "activation", dict(out=tq[:nt, 256:520], in_=ps[:nt, 0:264], func=AF.Copy),
                         r=[("ps", pi)], w=[("tq", si, 1)])
            P.dma(tmb[ti][s * 128:s * 128 + nt, :], tb[:nt, :, :].rearrange("p h f -> p (h f)"),
                  r=[("tb", si, 0), ("tb", si, 1)], w=[("dV", ti, s)])
            P.dma(tmk[ti][s * 128:s * 128 + nt, :], tk[:nt, :], r=[("tk", si, 0), ("tk", si, 1)], w=[("dC", ti, s)])
            P.dma(tmq[t0:t0 + nt, :], tq[:nt, :], r=[("tq", si, 0), ("tq", si, 1)])
        P.coll(ksl[ti][:, :], gK[ti][:, :], GROUPS, r=[("dK", ti, r0) for (r0, M, sc) in fm_groups if r0 >= KROW0])
        P.coll(tmb[ti][:, :], gV[ti][:, :], GROUPS, r=[("dV", ti, s) for s in range(nsb)])
        P.coll(tmk[ti][:, :], gKC[ti][:, :], GROUPS, r=[("dC", ti, s) for s in range(nsb)])


def emit_rotary(P, ps256, out256, cst, nt, rt, pskey, cskey, outkey):
    psv = ps256.rearrange("p (h d) -> p h d", d=64)
    x1, x2 = psv[:, :, 0:32], psv[:, :, 32:64]
    cosv = cst[:nt, 0:128].rearrange("p (h d) -> p h d", d=32)
    sinv = cst[:nt, 256:384].rearrange("p (h d) -> p h d", d=32)
    tv = [rt[:nt, i, 0:128].rearrange("p (h d) -> p h d", d=32) for i in range(4)]
    ov = out256.rearrange("p (h d) -> p h d", d=64)
    for i, (a_, b_) in enumerate([(x1, cosv), (x2, sinv), (x1, sinv), (x2, cosv)]):
        P.op("dve", "tensor_tensor", dict(out=tv[i], in0=a_, in1=b_, op=ALU.mult),
             r=[pskey, cskey], w=[("rt", i)])
    P.op("pool", "tensor_tensor", dict(out=ov[:, :, 0:32], in0=tv[0], in1=tv[1], op=ALU.subtract),
         r=[("rt", 0), ("rt", 1)], w=[outkey])
    P.op("pool", "tensor_tensor", dict(out=ov[:, :, 32:64], in0=tv[2], in1=tv[3], op=ALU.add),
         r=[("rt", 2), ("rt", 3), outkey], w=[outkey])


def emit_p3(P, hT, mix, wo, w1, w2, g, gf, hoT, identb, ntiles=17, final=False):
    wo_sb = P.sb("wo_sb", [128, 8, 1024], BF16)
    w1_sb = P.sb("w1_sb", [128, 8, 4096], BF16)
    w2_sb = P.sb("w2_sb", [128, 32, 1024], BF16)
    g_sb = P.sb("g_sb", [128, 8], F32)
    gf_sb = P.sb("gf_sb", [128, 8], F32)
    ones_bf = P.sb("ones_bf", [128, 128], BF16)
    idb = P.sb("idb", [128, 128], BF16)
    eps_t = P.sb("eps_t", [128, 1], F32)
    ht = P.sb("ht", [128, 8, 256], F32)
    mts = [P.sb("mt%d" % i, [128, 1024], BF16) for i in range(2)]
    mx = P.sb("mx", [128, 8, 256], BF16)
    uT = P.sb("uT", [128, 8, 256], BF16)
    sq = P.sb("sq", [128, 8, 256], BF16)
    hid = P.sb("hid", [128, 32, 256], BF16)
    sd = P.sb("sd", [128, 256], F32)
    rstd = P.sb("rstd", [128, 256], F32)
    rl = [P.sb("rl%d" % i, [128, 256], F32) for i in range(2)]
    ps_ss = P.ps("ps_ss")
    pss = [P.ps("ps%d" % i) for i in range(5)]
    ptr = [P.ps("ptr%d" % i, [128, 512], BF16) for i in range(2)]
    hTv = hT.rearrange("(k p) t -> p k t", p=128)
    hoTv = hoT.rearrange("(k p) t -> p k t", p=128)
    P.op("dve", "memset", dict(ap=ones_bf[:, :], constant=1.0), w=[("ones",)])
    P.op("dve", "memset", dict(ap=eps_t[:, :], constant=EPS), w=[("eps",)])
    P.dma(g_sb[:, :], g[:, :], w=[("g",)])
    P.dma(gf_sb[:, :], gf[:, :], w=[("gf",)])
    P.dma(idb[:, :], identb[:, :], w=[("idb",)])
    wov = wo.rearrange("(k p) c -> p k c", p=128)
    w1v = w1.rearrange("(k p) c -> p k c", p=128)
    w2v = w2.rearrange("(f p) c -> p f c", p=128)
    for k in range(8):
        P.dma(wo_sb[:, k, :], wov[:, k, :], w=[("wo", k)], q="pool")
    for k in range(8):
        P.dma(w1_sb[:, k, :], w1v[:, k, :], w=[("w1", k)], q="pool")
    for f in range(0, 32, 4):
        P.dma(w2_sb[:, f:f + 4, :], w2v[:, f:f + 4, :], w=[("w2", f // 4)], q="pool")
    pc = [0]

    def nextps():
        i = pc[0] % 5
        pc[0] += 1
        return i

    rc = 0
    mc = 0
    tc = 0
    for ti, (tok0, ntok) in enumerate(tiles256(ntiles)):
        P.dma(ht[:, :, :ntok], hTv[:, :, tok0:tok0 + ntok], w=[("ht",)])
        nsb = max(1, ntok // 128)
        for s in range(nsb):
            nt = min(128, ntok)
            mi = mc % 2
            mc += 1
            mt = mts[mi]
            P.dma(mt[:nt, :], mix[tok0 + s * 128: tok0 + s * 128 + nt, :], w=[("mt", mi)])
            for kq in range(2):
                ti2 = tc % 2
                tc += 1
                pt = ptr[ti2]
                for i in range(4):
                    kk = 4 * kq + i
                    P.op("pe", "transpose", dict(out=pt[:, i * 128:i * 128 + nt], in_=mt[:nt, kk * 128:(kk + 1) * 128],
                                                 identity=idb[:nt, :nt]),
                         r=[("mt", mi), ("idb",)], w=[("ptr", ti2)])
                P.op("act", "activation", dict(out=mx[:, 4 * kq:4 * kq + 4, s * 128:s * 128 + nt],
                                               in_=pt[:, :].rearrange("p (c t) -> p c t", t=128)[:, :, :nt], func=AF.Copy),
                     r=[("ptr", ti2)], w=[("mx", s, kq)])
        mxkeys = [("mx", s, kq) for s in range(nsb) for kq in range(2)]
        for m in range(8):
            pi = nextps()
            ps = pss[pi]
            for k in range(8):
                P.op("pe", "matmul", dict(out=ps[:, :ntok], lhsT=wo_sb[:, k, m * 128:(m + 1) * 128], rhs=mx[:, k, :ntok],
                                          start=(k == 0), stop=(k == 7)),
                     r=mxkeys + [("wo", k)], w=[("ps", pi)])
            P.op("dve", "tensor_tensor", dict(out=ht[:, m, :ntok], in0=ht[:, m, :ntok], in1=ps[:, :ntok], op=ALU.add),
                 r=[("ps", pi), ("ht",)], w=[("ht",)])
        emit_rmsnorm_T(P, ht, uT, ntok, g_sb, sq, ps_ss, rstd, sd, ones_bf, eps_t, ("ht",), ("uT",))
        for f in range(32):
            pi = nextps()
            ps = pss[pi]
            for k in range(8):
                P.op("pe", "matmul", dict(out=ps[:, :ntok], lhsT=w1_sb[:, k, f * 128:(f + 1) * 128], rhs=uT[:, k, :ntok],
                                          start=(k == 0), stop=(k == 7)),
                     r=[("uT",), ("w1", k)], w=[("ps", pi)])
            ri = rc % 2
            rc += 1
            P.op("act", "activation", dict(out=rl[ri][:, :ntok], in_=ps[:, :ntok], func=AF.Relu),
                 r=[("ps", pi)], w=[("rl", ri)])
            P.op("pool", "tensor_tensor", dict(out=hid[:, f, :ntok], in0=rl[ri][:, :ntok], in1=rl[ri][:, :ntok], op=ALU.mult),
                 r=[("rl", ri)], w=[("hid", f)])
        for m in range(8):
            pi = nextps()
            ps = pss[pi]
            for f in range(32):
                P.op("pe", "matmul", dict(out=ps[:, :ntok], lhsT=w2_sb[:, f, m * 128:(m + 1) * 128], rhs=hid[:, f, :ntok],
                                          start=(f == 0), stop=(f == 31)),
                     r=[("hid", f), ("w2", f // 4)], w=[("ps", pi)])
            P.op("dve", "tensor_tensor", dict(out=ht[:, m, :ntok], in0=ht[:, m, :ntok], in1=ps[:, :ntok], op=ALU.add),
                 r=[("ps", pi), ("ht",)], w=[("ht",)])
        if final:
            emit_rmsnorm_T(P, ht, ht, ntok, gf_sb, sq, ps_ss, rstd, sd, ones_bf, eps_t, ("ht",), ("ht",))
        P.dma(hoTv[:, :, tok0:tok0 + ntok], ht[:, :, :ntok], r=[("ht",)])


def emit_p2ab(P, fm, gK, gV, tmq, mix, cst, njobs=NJ, do_meta=True, lambda_init=0.2, nbis=NBIS):
    S = P.sb
    acc = S("acc", [128, TALL], F32)
    MB = S("MB", [128, TALL], BF16)
    MBN = S("MBN", [128, 8, 1024], BF16)
    MBNm = S("MBNm", [128, 8, 16], BF16)
    NB = S("NB", [128, 12, 1024], BF16)
    NBm0 = S("NBm0", [128, 12, 16], BF16)
    NBmm = S("NBmm", [16, 12, 16], BF16)
    dm = S("dm", [128, 512], F32)
    dtmp = S("dtmp", [128, 512], F32)
    b31s = S("b31s", [128, 12], F32)
    zcol = S("zcol", [128, 1], F32)
    half = S("half", [128, 1], F32)
    epsc = S("epsc", [128, 1], F32)
    idn = S("idn", [128, 128], BF16)
    lam_in = S("lam_in", [128, 128], F32)
    dn = S("dn", [128, 64], F32)
    kis = [S("ki%d" % i, [128, 512], BF16) for i in range(2)]
    kas = [S("ka%d" % i, [128, 4, 512], BF16) for i in range(2)]
    kbs = [S("kb%d" % i, [128, 2, 512], BF16) for i in range(2)]
    vas = [S("va%d" % i, [128, 4, 520], BF16) for i in range(2)]
    vbs = [S("vb%d" % i, [128, 4, 260], BF16) for i in range(2)]
    qa = S("qa", [128, 8, 128], BF16)
    qb = S("qb", [128, 8, 128], BF16)
    qi = S("qi", [128, 8, 128], BF16)
    wis = S("wis", [128, 8], F32)
    absw = S("absw", [128, 8], F32)
    sgn = S("sgn", [128, 8], F32)
    rbuf = [S("r%d" % i, [128, 512], F32) for i in range(3)]
    pTs = [S("pT%d" % i, [128, 512], BF16) for i in range(3)]
    sm = S("sm", [128, 32], F32)
    smu = S("smu", [128, 4], U32)
    abo = S("abo", [128, 768], BF16)
    fin = S("fin", [128, 512], F32)
    psI = [P.ps("psI%d" % i) for i in range(2)]
    psS = [P.ps("psS%d" % i) for i in range(2)]
    OA = [P.ps("OA%d" % i) for i in range(2)]
    OB = [P.ps("OB%d" % i) for i in range(2)]
    LO, HI, MID, CNT, RMIN, RTMP, LAM, NLAM = 0, 1, 2, 3, 4, 5, 6, 7
    gKv = [a.rearrange("(r f) t -> r f t", r=4) for a in gK]
    gVv = [a.rearrange("(r t) f -> r t f", r=4) for a in gV]
    KA0, KB0, KI0 = 0, FM_OFF["kb"] - KROW0, FM_OFF["ki"] - KROW0

    def smc(i):
        return sm[:, i:i + 1]

    P.dma(dm[:, :], cst["dmask"][:, :], w=[("dm",)])
    P.dma(NB[:, :, :], cst["nb"].rearrange("h p k -> p h k"), w=[("NB",)])
    P.dma(NBm0[:, :, :], cst["nbm0"].rearrange("h p k -> p h k"), w=[("NBm0",)])
    P.dma(NBmm[:, :, :], cst["nbmm"].rearrange("h p k -> p h k"), w=[("NBmm",)])
    P.dma(b31s[:, :], cst["b31"][:, :], w=[("b31",)])
    P.dma(idn[:, :], cst["identb"][:, :], w=[("idn",)])
    P.dma(lam_in[:, :], cst["lamb"][:, :], w=[("lam_in",)])
    P.dma(dn[:, :], cst["dnb"][:, :], w=[("dn",)])
    P.op("dve", "memset", dict(ap=zcol[:, :], constant=0.0), w=[("zcol",)])
    P.op("dve", "memset", dict(ap=half[:, :], constant=0.5), w=[("half",)])
    P.op("dve", "memset", dict(ap=epsc[:, :], constant=EPS), w=[("epsc",)])
    P.op("dve", "memset", dict(ap=qa[:, :, :], constant=0.0), w=[("qa", h) for h in range(8)])
    P.op("dve", "memset", dict(ap=qb[:, :, :], constant=0.0), w=[("qb", h) for h in range(8)])
    P.op("dve", "memset", dict(ap=qi[:, :, :], constant=0.0), w=[("qi", h) for h in range(8)])
    P.op("dve", "tensor_tensor", dict(out=fin[:, 0:32], in0=lam_in[:, 0:32], in1=lam_in[:, 32:64], op=ALU.mult),
         r=[("lam_in",)], w=[("fin",)])
    P.op("dve", "tensor_tensor", dict(out=fin[:, 32:64], in0=lam_in[:, 64:96], in1=lam_in[:, 96:128], op=ALU.mult),
         r=[("lam_in",), ("fin",)], w=[("fin",)])
    P.op("dve", "tensor_reduce", dict(out=sm[:, 8:10], in_=fin[:, 0:64].rearrange("p (a b) -> p a b", b=32),
                                      axis=AX.X, op=ALU.add), r=[("fin",)], w=[("sm", "l")])
    P.op("act", "activation", dict(out=sm[:, 10:12], in_=sm[:, 8:10], func=AF.Exp), r=[("sm", "l")], w=[("sm", "l2")])
    P.op("dve", "tensor_tensor", dict(out=smc(LAM), in0=sm[:, 11:12], in1=sm[:, 10:11], op=ALU.subtract),
         r=[("sm", "l2")], w=[("sm", "lam")])
    P.op("dve", "tensor_scalar", dict(out=smc(NLAM), in0=smc(LAM), scalar1=-float(lambda_init), scalar2=None,
                                      op0=ALU.add), r=[("sm", "lam")], w=[("sm", "nlam")])
    P.op("dve", "tensor_scalar", dict(out=dn[:, :], in0=dn[:, :], scalar1=float(1.0 - lambda_init), scalar2=None,
                                      op0=ALU.mult), r=[("dn",)], w=[("dn",)])

    tcount = [0]
    rcount = [0]
    pcount = [0]
    scount = [0]
    icount = [0]

    pend = [None]

    def attend(nq, tiles, meta_mode, j):
        for i in range(2):
            P.op("dve", "memset", dict(ap=OA[i][:nq, 0:260], constant=0.0), w=[("OA", i)])
            P.op("dve", "memset", dict(ap=OB[i][:nq, 0:260], constant=0.0), w=[("OB", i)])
        steps = [("t", g, near) for (g, near) in tiles] + [("m", None, None)]
        for (kind, g, near) in steps:
            tb = tcount[0] % 2
            tcount[0] += 1
            ka_t, kb_t, va_t, vb_t = kas[tb], kbs[tb], vas[tb], vbs[tb]
            if kind == "t":
                tix, t0 = tile_of_tok(16 + 128 * g)
                nkb, nk = 4, 128
                allr = lambda nm: [(nm, tb, r_) for r_ in range(4)]
                for c_ in range(4):
                    P.dma(ka_t[:, c_, :].rearrange("p (r t) -> p r t", r=4),
                          gKv[tix][:, KA0 + c_ * 128:KA0 + (c_ + 1) * 128, t0:t0 + 128].rearrange("r p t -> p r t"),
                          w=[("ka", tb, c_ + 10)])
                for c_ in range(2):
                    P.dma(kb_t[:, c_, :].rearrange("p (r t) -> p r t", r=4),
                          gKv[tix][:, KB0 + c_ * 128:KB0 + (c_ + 1) * 128, t0:t0 + 128].rearrange("r p t -> p r t"),
                          w=[("kb", tb, c_ + 10)])
                P.dma(va_t[:, :, :], gVv[tix][:, t0:t0 + 128, 0:520].rearrange("r p f -> p r f"), w=allr("va"))
                P.dma(vb_t[:, :, :], gVv[tix][:, t0:t0 + 128, 520:780].rearrange("r p f -> p r f"), w=allr("vb"))
            else:
                nkb, nk = 1, 16
                P.dma(ka_t[:, :, 0:16], gKv[0][0, KA0:KA0 + 512, 0:16].rearrange("(c p) t -> p c t", p=128),
                      w=[("ka", tb, c_ + 10) for c_ in range(4)])
                P.dma(kb_t[:, :, 0:16], gKv[0][0, KB0:KB0 + 256, 0:16].rearrange("(c p) t -> p c t", p=128),
                      w=[("kb", tb, c_ + 10) for c_ in range(2)])
                P.dma(va_t[0:16, 0, :], gVv[0][0, 0:16, 0:520], w=[("va", tb, r_) for r_ in range(4)])
                P.dma(vb_t[0:16, 0, :], gVv[0][0, 0:16, 520:780], w=[("vb", tb, r_) for r_ in range(4)])
            for hh in range(16):
                isA = hh < 8
                if isA:
                    h = hh
                    qz, qkey = qa[:, h, :nq], ("qa", h)
                    kt, kname, vt, vname = ka_t, "ka", va_t, "va"
                    kc_ = h // 2
                    Oap = OA[h // 4][:nq, (h % 4) * 65:(h % 4 + 1) * 65]
                    Okey = ("OA", h // 4)
                    hb = h
                else:
                    hc = hh - 8
                    h = hc // 2
                    qz, qkey = qb[:, hc, :nq], ("qb", hc)
                    kt, kname, vt, vname = kb_t, "kb", vb_t, "vb"
                    kc_ = h // 2
                    Oap = OB[hc // 4][:nq, (hc % 4) * 65:(hc % 4 + 1) * 65]
                    Okey = ("OB", hc // 4)
                    hb = 8 + h
                si = scount[0] % 2
                scount[0] += 1
                ps = psS[si]

                def mop(blk):
                    if kind == "t":
                        if near is not None:
                            if isA:
                                return MBN[:nq, h, near * 512 + blk * 128: near * 512 + (blk + 1) * 128], ("MBN", h)
                            return NB[:nq, hb, near * 512 + blk * 128: near * 512 + (blk + 1) * 128], ("NB",)
                        if isA:
                            return MB[:nq, g * 512 + blk * 128: g * 512 + (blk + 1) * 128], ("MB",)
                        return None, None
                    if meta_mode == "mm":
                        return NBmm[:nq, hb, :], ("NBmm",)
                    if meta_mode == "j0":
                        if isA:
                            return MBNm[:nq, h, :], ("MBNm",)
                        return NBm0[:nq, hb, :], ("NBm0",)
                    if isA:
                        return MB[:nq, 512 * (j + 1): 512 * (j + 1) + 16], ("MB",)
                    return None, None
                first = True
                for blk in range(nkb):
                    P.op("pe", "matmul", dict(out=ps[:nk, blk * 128:blk * 128 + nq],
                                              lhsT=kt[:, kc_, blk * 128:blk * 128 + nk], rhs=qz,
                                              start=first, stop=False, skip_group_check=True),
                         r=[(kname, tb, kc_ + 10), qkey], w=[("psS", si)])
                    first = False
                for blk in range(nkb):
                    m_ap, m_key = mop(blk)
                    if m_ap is not None:
                        P.op("pe", "matmul", dict(out=ps[:nk, blk * 128:blk * 128 + nq], lhsT=m_ap, rhs=idn[:nq, :nq],
                                                  start=False, stop=False, skip_group_check=True),
                             r=[m_key, ("idn",)], w=[("psS", si)])
                usebias = (kind == "t" and near is None) or (kind == "m" and meta_mode == "far")
                bias_ap = b31s[:nk, hb:hb + 1] if usebias else zcol[:nk, 0:1]
                pi = pcount[0] % 3
                pcount[0] += 1
                pT = pTs[pi]
                ncol = (nkb - 1) * 128 + nq
                P.op("act", "activation", dict(out=pT[:nk, :ncol], in_=ps[:nk, :ncol], func=AF.Exp, bias=bias_ap),
                     r=[("psS", si), ("b31",), ("zcol",)], w=[("pT", pi)])
                if pend[0] is not None:
                    pend[0]()

                def pv(Oap=Oap, pT=pT, nk=nk, nq=nq, vt=vt, h=h, pi=pi, vname=vname, tb=tb, Okey=Okey, nkb=nkb):
                    for blk in range(nkb):
                        P.op("pe", "matmul", dict(out=Oap, lhsT=pT[:nk, blk * 128:blk * 128 + nq],
                                                  rhs=vt[:nk, blk, h * 65:(h + 1) * 65],
                                                  start=False, stop=False, skip_group_check=True),
                             r=[("pT", pi), (vname, tb, blk)], w=[Okey])
                pend[0] = pv
        if pend[0] is not None:
            pend[0]()
            pend[0] = None

    def finalize(nq, tok0):
        for i in range(2):
            ov = OA[i][:nq, 0:260].rearrange("p (h f) -> p h f", f=65)
            P.op("dve", "reciprocal", dict(out=sm[:nq, 12 + 4 * i:16 + 4 * i], in_=ov[:, :, 64]),
                 r=[("OA", i)], w=[("sm", "ra", i)])
            for hl in range(4):
                h = 4 * i + hl
                P.op("dve", "tensor_scalar", dict(out=abo[:nq, h * 64:(h + 1) * 64], in0=ov[:, hl, 0:64],
                                                  scalar1=sm[:nq, 12 + h:13 + h], scalar2=None, op0=ALU.mult),
                     r=[("OA", i), ("sm", "ra", i)], w=[("abo", h)])
        for i in range(2):
            ov = OB[i][:nq, 0:260].rearrange("p (h f) -> p h f", f=65)
            P.op("dve", "reciprocal", dict(out=sm[:nq, 20 + 4 * i:24 + 4 * i], in_=ov[:, :, 64]),
                 r=[("OB", i)], w=[("sm", "rb", i)])
        rv = sm[:nq, 20:28].rearrange("p (h c) -> p h c", c=2)[:, :, 1]
        P.op("dve", "tensor_scalar", dict(out=rv, in0=rv, scalar1=sm[:nq, NLAM:NLAM + 1], scalar2=None, op0=ALU.mult),
             r=[("sm", "rb", 0), ("sm", "rb", 1), ("sm", "nlam")], w=[("sm", "rb", 0), ("sm", "rb", 1)])
        for h in range(4):
            i = h // 2
            ov = OB[i][:nq, 0:260].rearrange("p (h f) -> p h f", f=65)
            c0, c1 = (2 * h) % 4, (2 * h + 1) % 4
            t0 = fin[:nq, h * 64:(h + 1) * 64]
            bh = fin[:nq, 256 + h * 64:256 + (h + 1) * 64]
            P.op("dve", "tensor_scalar", dict(out=t0, in0=ov[:, c0, 0:64], scalar1=sm[:nq, 20 + 2 * h:21 + 2 * h],
                                              scalar2=None, op0=ALU.mult),
                 r=[("OB", i), ("sm", "rb", i)], w=[("fin", h)])
            P.op("dve", "scalar_tensor_tensor", dict(out=bh, in0=ov[:, c1, 0:64], scalar=sm[:nq, 21 + 2 * h:22 + 2 * h],
                                                     in1=t0, op0=ALU.mult, op1=ALU.add),
                 r=[("OB", i), ("sm", "rb", i), ("fin", h)], w=[("finb", h)])
            P.op("dve", "tensor_tensor", dict(out=t0, in0=bh, in1=bh, op=ALU.mult), r=[("finb", h)], w=[("fin", h)])
            P.op("dve", "tensor_reduce", dict(out=sm[:nq, 28 + h:29 + h], in_=t0, axis=AX.X, op=ALU.add),
                 r=[("fin", h)], w=[("sm", "ss", h)])
            P.op("act", "activation", dict(out=sm[:nq, 28 + h:29 + h], in_=sm[:nq, 28 + h:29 + h], func=AF.Sqrt,
                                           bias=epsc[:nq, 0:1], scale=1.0 / 64.0),
                 r=[("sm", "ss", h), ("epsc",)], w=[("sm", "ss", h)])
            P.op("dve", "reciprocal", dict(out=sm[:nq, 28 + h:29 + h], in_=sm[:nq, 28 + h:29 + h]),
                 r=[("sm", "ss", h)], w=[("sm", "ss", h)])
            P.op("dve", "scalar_tensor_tensor", dict(out=abo[:nq, 512 + h * 64:512 + (h + 1) * 64], in0=bh,
                                                     scalar=sm[:nq, 28 + h:29 + h], in1=dn[:nq, :],
                                                     op0=ALU.mult, op1=ALU.mult),
                 r=[("finb", h), ("sm", "ss", h), ("dn",)], w=[("abo", 8 + h)])
        P.dma(mix[tok0:tok0 + nq, 0:768], abo[:nq, :], r=[("abo", k) for k in range(12)])

    def load_q(tok0, nq, need_idx):
        for h in range(8):
            P.dma(qa[(h % 2) * 64:(h % 2) * 64 + 64, h, :nq],
                  fm[FM_OFF["qa"] + h * 64:FM_OFF["qa"] + (h + 1) * 64, tok0:tok0 + nq], w=[("qa", h)])
        for hc in range(8):
            h, c = hc // 2, hc % 2
            p0 = (h % 2) * 64 + c * 32
            r0 = FM_OFF["qb"] + h * 64 + c * 32
            P.dma(qb[p0:p0 + 32, hc, :nq], fm[r0:r0 + 32, tok0:tok0 + nq], w=[("qb", hc)])
        if need_idx:
            for h in range(8):
                P.dma(qi[(h % 2) * 64:(h % 2) * 64 + 64, h, :nq],
                      fm[FM_OFF["qi"] + h * 64:FM_OFF["qi"] + (h + 1) * 64, tok0:tok0 + nq], w=[("qi", h)])
            P.dma(wis[:nq, :], tmq[tok0:tok0 + nq, 512:520], w=[("wis",)])

    if do_meta:
        load_q(0, 16, False)
        attend(16, [], "mm", None)
        finalize(16, 0)

    for j in range(njobs):
        tok0 = 16 + 128 * j
        nq = 128
        n = 512 * (j + 1) + 16
        load_q(tok0, nq, True)
        P.op("act", "activation", dict(out=absw[:, :], in_=wis[:, :], func=AF.Abs, scale=float(C_IDX)),
             r=[("wis",)], w=[("absw",)])
        P.op("act", "activation", dict(out=sgn[:, :], in_=wis[:, :], func=AF.Sign), r=[("wis",)], w=[("sgn",)])
        acckeys = []
        for g in list(range(j + 1)) + ["m"]:
            kb_ = icount[0] % 2
            icount[0] += 1
            ki_t = kis[kb_]
            if g == "m":
                nk, c0 = 16, 512 * (j + 1)
                for e in range(2):
                    P.dma(ki_t[e * 64:(e + 1) * 64, 0:16], gKv[0][0, KI0:KI0 + 64, 0:16], w=[("ki", kb_, e)])
            else:
                nk, c0 = 512, 512 * g
                tix, t0 = tile_of_tok(16 + 128 * g)
                for e in range(2):
                    P.dma(ki_t[e * 64:(e + 1) * 64, :].rearrange("p (r t) -> p r t", r=4),
                          gKv[tix][:, KI0:KI0 + 64, t0:t0 + 128].rearrange("r p t -> p r t"),
                          w=[("ki", kb_, e, r_) for r_ in range(4)])
            kikeys = [("ki", kb_, e) for e in range(2)] + [("ki", kb_, e, r_) for e in range(2) for r_ in range(4)]
            akey = ("acc", g)
            acckeys.append(akey)
            for h in range(8):
                pi = (icount[0] * 8 + h) % 2
                ps = psI[pi]
                P.op("pe", "matmul", dict(out=ps[:, :nk], lhsT=qi[:, h, :], rhs=ki_t[:, :nk], start=True, stop=True),
                     r=[("qi", h)] + kikeys, w=[("psI", pi)])
                ri = rcount[0] % 3
                rcount[0] += 1
                rb = rbuf[ri]
                P.op("act", "activation", dict(out=rb[:, :nk], in_=ps[:, :nk], func=AF.Relu, scale=absw[:, h:h + 1]),
                     r=[("psI", pi), ("absw",)], w=[("r", ri)])
                if h == 0:
                    P.op("dve", "tensor_scalar", dict(out=acc[:, c0:c0 + nk], in0=rb[:, :nk], scalar1=sgn[:, 0:1],
                                                      scalar2=None, op0=ALU.mult),
                         r=[("r", ri), ("sgn",)], w=[akey])
                else:
                    P.op("dve", "scalar_tensor_tensor", dict(out=acc[:, c0:c0 + nk], in0=rb[:, :nk],
                                                             scalar=sgn[:, h:h + 1], in1=acc[:, c0:c0 + nk],
                                                             op0=ALU.mult, op1=ALU.add),
                         r=[("r", ri), ("sgn",), akey], w=[akey])
        dkey = ("acc", j)
        d0 = 512 * j
        P.op("dve", "scalar_tensor_tensor", dict(out=dtmp[:, :], in0=dm[:, :], scalar=-1.0, in1=acc[:, d0:d0 + 512],
                                                 op0=ALU.mult, op1=ALU.add), r=[("dm",), dkey], w=[("dtmp",)])
        P.op("dve", "tensor_reduce", dict(out=smc(RMIN), in_=dtmp[:, :], axis=AX.X, op=ALU.min),
             r=[("dtmp",)], w=[("sm", "rmin")])
        P.op("dve", "tensor_tensor", dict(out=acc[:, d0:d0 + 512], in0=acc[:, d0:d0 + 512], in1=dm[:, :], op=ALU.add),
             r=[("dm",), dkey], w=[dkey])
        P.op("dve", "tensor_reduce", dict(out=smc(RTMP), in_=acc[:, d0 + 512:n], axis=AX.X, op=ALU.min),
             r=acckeys, w=[("sm", "rtmp")])
        P.op("dve", "tensor_tensor", dict(out=smc(RMIN), in0=smc(RMIN), in1=smc(RTMP), op=ALU.min),
             r=[("sm", "rmin"), ("sm", "rtmp")], w=[("sm", "rmin")])
        if j > 0:
            P.op("dve", "tensor_reduce", dict(out=smc(RTMP), in_=acc[:, 0:d0], axis=AX.X, op=ALU.min),
                 r=acckeys, w=[("sm", "rtmp")])
            P.op("dve", "tensor_tensor", dict(out=smc(RMIN), in0=smc(RMIN), in1=smc(RTMP), op=ALU.min),
                 r=[("sm", "rmin"), ("sm", "rtmp")], w=[("sm", "rmin")])
        P.op("dve", "tensor_reduce", dict(out=smc(HI), in_=acc[:, 0:n], axis=AX.X, op=ALU.max),
             r=acckeys, w=[("sm", "hi")])
        P.op("dve", "tensor_copy", dict(out=smc(LO), in_=smc(RMIN)), r=[("sm", "rmin")], w=[("sm", "lo")])
        for it in range(nbis):
            P.op("dve", "scalar_tensor_tensor", dict(out=smc(MID), in0=smc(LO), scalar=smc(HI), in1=half[:, :],
                                                     op0=ALU.add, op1=ALU.mult),
                 r=[("sm", "lo"), ("sm", "hi"), ("half",)], w=[("sm", "mid")])
            P.op("dve", "tensor_scalar", dict(out=MB[:, 0:n], in0=acc[:, 0:n], scalar1=smc(MID), scalar2=0.0,
                                              op0=ALU.is_ge, op1=ALU.add, accum_out=smc(CNT)),
                 r=acckeys + [("sm", "mid")], w=[("MB",), ("sm", "cnt")])
            P.op("dve", "tensor_single_scalar", dict(out=smu[:, 0:1], in_=smc(CNT), scalar=255.5, op=ALU.is_ge),
                 r=[("sm", "cnt")], w=[("smu", 0)])
            P.op("dve", "tensor_single_scalar", dict(out=smu[:, 1:2], in_=smc(CNT), scalar=255.5, op=ALU.is_lt),
                 r=[("sm", "cnt")], w=[("smu", 1)])
            P.op("dve", "copy_predicated", dict(out=smc(LO), mask=smu[:, 0:1], data=smc(MID)),
                 r=[("smu", 0), ("sm", "mid")], w=[("sm", "lo")])
            P.op("dve", "copy_predicated", dict(out=smc(HI), mask=smu[:, 1:2], data=smc(MID)),
                 r=[("smu", 1), ("sm", "mid")], w=[("sm", "hi")])
        P.op("dve", "tensor_scalar", dict(out=MB[:, 0:n], in0=acc[:, 0:n], scalar1=smc(LO), scalar2=-BIG,
                                          op0=ALU.is_lt, op1=ALU.mult),
             r=acckeys + [("sm", "lo")], w=[("MB",)])
        if j == 0:
            tiles = [(0, 1)]
            for h in range(8):
                P.op("pool", "tensor_tensor", dict(out=MBN[:, h, 512:1024], in0=MB[:, 0:512], in1=NB[:, h, 512:1024],
                                                   op=ALU.add), r=[("MB",), ("NB",)], w=[("MBN", h)])
            for h in range(8):
                P.op("pool", "tensor_tensor", dict(out=MBNm[:, h, :], in0=MB[:, 512:528], in1=NBm0[:, h, :], op=ALU.add),
                     r=[("MB",), ("NBm0",), ("MBNm",)], w=[("MBNm",)])
            meta_mode = "j0"
        else:
            tiles = [(g, None) for g in range(j - 1)] + [(j - 1, 0), (j, 1)]
            for h in range(8):
                P.op("pool", "tensor_tensor", dict(out=MBN[:, h, :], in0=MB[:, 512 * (j - 1):512 * (j + 1)],
                                                   in1=NB[:, h, :], op=ALU.add),
                     r=[("MB",), ("NB",)], w=[("MBN", h)])
            meta_mode = "far"
        attend(nq, tiles, meta_mode, j)
        finalize(nq, tok0)


def emit_p2c(P, gKC, tmq, tmk, mix, cst, nblocks=128, do_meta=True):
    S = P.sb
    DT = S("DT", [128, 4, 128], F32)
    QD = S("QD", [64, 4, 128], F32)
    kd = S("kd", [128, 4], F32)
    kd16 = S("kd16", [16, 4], F32)
    oh = S("oh", [128, 4], F32)
    rn = S("rn", [128, 256], F32)
    idf = S("idf", [128, 128], F32)
    epsc = S("epsc", [128, 1], F32)
    kvb = [S("kvb%d" % i, [128, 512], F32) for i in range(2)]
    kdec = [S("kdec%d" % i, [128, 256], F32) for i in range(2)]
    ring = S("ring", [64, 4, 256], F32)
    ssel = S("ssel", [64, 256], F32)
    qtk = S("qtk", [128, 520], F32)
    ktk = S("ktk", [128, 512], F32)
    qT = S("qT", [64, 4, 128], F32)
    kT = S("kT", [64, 4, 128], F32)
    qd = S("qd", [64, 4, 128], F32)
    PT = [S("PT%d" % i, [128, 128], F32) for i in range(2)]
    ret = S("ret", [128, 256], F32)
    ss = S("ss", [128, 4], F32)
    yb = S("yb", [128, 256], F32)
    sg = S("sg", [128, 256], F32)
    cob = S("cob", [128, 256], BF16)
    psU = [P.ps("psU%d" % i) for i in range(2)]
    psA = [P.ps("psA%d" % i) for i in range(2)]
    psT = [P.ps("psT%d" % i) for i in range(2)]
    psO = P.ps("psO")
    gv = [a.rearrange("(r t) f -> r t f", r=4) for a in gKC]
    P.dma(DT[:, :, :], cst["DT"].rearrange("h j i -> j h i"), w=[("DT",)])
    P.dma(QD[:, :, :], cst["QD"][:, :, :], w=[("QD",)])
    P.dma(kd[:, :], cst["kd"][:, :], w=[("kd",)])
    P.dma(kd16[:, :], cst["kd16"][:, :], w=[("kd16",)])
    P.dma(oh[:, :], cst["oh"][:, :], w=[("oh",)])
    P.dma(rn[:, :], cst["rn"][:, :], w=[("rn",)])
    P.dma(idf[:, :], cst["identf"][:, :], w=[("idf",)])
    P.op("dve", "memset", dict(ap=epsc[:, :], constant=EPS), w=[("epsc",)])
    acount = [0]
    tcount = [0]

    def ret_block(tok0, L, use_state):
        P.dma(qtk[:L, :], tmq[tok0:tok0 + L, :], w=[("qtk",)])
        tix_, o_ = tile_of_tok(tok0)
        P.dma(ktk[:L, :], tmk[tix_][o_:o_ + L, :], w=[("ktk",)])
        for (src, skey, dst, dkey) in ((qtk, ("qtk",), qT, "qT"), (ktk, ("ktk",), kT, "kT")):
            for h in range(4):
                ti = tcount[0] % 2
                tcount[0] += 1
                P.op("pe", "transpose", dict(out=psT[ti][:64, :L], in_=src[:L, h * 64:(h + 1) * 64], identity=idf[:L, :L]),
                     r=[skey, ("idf",)], w=[("psT", ti)])
                P.op("act", "activation", dict(out=dst[:, h, :L], in_=psT[ti][:64, :L], func=AF.Copy),
                     r=[("psT", ti)], w=[(dkey, h)])
        qTk = [("qT", h) for h in range(4)]
        kTk = [("kT", h) for h in range(4)]
        if use_state:
            P.op("dve", "tensor_scalar", dict(out=ssel[:, :], in0=ring[:, 0, :], scalar1=oh[:64, 0:1], scalar2=None,
                                              op0=ALU.mult), r=[("ring", 0), ("oh",)], w=[("ssel",)])
            for c in range(1, 4):
                P.op("dve", "scalar_tensor_tensor", dict(out=ssel[:, :], in0=ring[:, c, :], scalar=oh[:64, c:c + 1],
                                                         in1=ssel[:, :], op0=ALU.mult, op1=ALU.add),
                     r=[("ring", c), ("oh",), ("ssel",)], w=[("ssel",)])
            P.op("dve", "tensor_tensor", dict(out=qd[:, :, :L], in0=qT[:, :, :L], in1=QD[:, :, :L], op=ALU.mult),
                 r=qTk + [("QD",)], w=[("qd",)])
        for h in range(4):
            ai = acount[0] % 2
            acount[0] += 1
            P.op("pe", "matmul", dict(out=psA[ai][:L, :L], lhsT=kT[:, h, :L], rhs=qT[:, h, :L], start=True, stop=True),
                 r=[("kT", h), ("qT", h)], w=[("psA", ai)])
            P.op("dve", "tensor_tensor", dict(out=PT[ai][:L, :L], in0=psA[ai][:L, :L], in1=DT[:L, h, :L], op=ALU.mult),
                 r=[("psA", ai), ("DT",)], w=[("PT", ai)])
            P.op("pe", "matmul", dict(out=psO[:L, h * 64:(h + 1) * 64], lhsT=PT[ai][:L, :L],
                                      rhs=ktk[:L, 256 + h * 64:256 + (h + 1) * 64],
                                      start=(h == 0), stop=(not use_state), skip_group_check=True),
                 r=[("PT", ai), ("ktk",)], w=[("psO",)])
            if use_state:
                P.op("pe", "matmul", dict(out=psO[:L, h * 64:(h + 1) * 64], lhsT=qd[:, h, :L],
                                          rhs=ssel[:, h * 64:(h + 1) * 64], start=False, stop=True,
                                          skip_group_check=True),
                     r=[("qd",), ("ssel",)], w=[("psO",)])
        P.op("act", "activation", dict(out=ret[:L, :], in_=psO[:L, 0:256], func=AF.Copy), r=[("psO",)], w=[("ret",)])
        ybk = [("yb", h) for h in range(4)]
        ssk = [("ss", h) for h in range(4)]
        P.op("dve", "tensor_tensor", dict(out=yb[:L, :], in0=ret[:L, :], in1=ret[:L, :], op=ALU.mult),
             r=[("ret",)], w=ybk)
        P.op("dve", "tensor_reduce", dict(out=ss[:L, :], in_=yb[:L, :].rearrange("p (h d) -> p h d", d=64),
                                          axis=AX.X, op=ALU.add), r=ybk, w=ssk)
        P.op("act", "activation", dict(out=ss[:L, :], in_=ss[:L, :], func=AF.Sqrt, bias=epsc[:L, 0:1], scale=1.0 / 64.0),
             r=ssk + [("epsc",)], w=ssk)
        P.op("dve", "reciprocal", dict(out=ss[:L, :], in_=ss[:L, :]), r=ssk, w=ssk)
        for h in range(4):
            P.op("dve", "scalar_tensor_tensor", dict(out=yb[:L, h * 64:(h + 1) * 64], in0=ret[:L, h * 64:(h + 1) * 64],
                                                     scalar=ss[:L, h:h + 1], in1=rn[:L, h * 64:(h + 1) * 64],
                                                     op0=ALU.mult, op1=ALU.mult),
                 r=[("ret",), ("ss", h), ("rn",)], w=[("yb", h)])
        P.op("act", "activation", dict(out=sg[:L, :], in_=qtk[:L, 256:512], func=AF.Silu), r=[("qtk",)], w=[("sg",)])
        P.op("pool", "tensor_tensor", dict(out=cob[:L, :], in0=yb[:L, :], in1=sg[:L, :], op=ALU.mult),
             r=ybk + [("sg",)], w=[("cob",)])
        P.dma(mix[tok0:tok0 + L, 768:1024], cob[:L, :], r=[("cob",)])

    if do_meta:
        ret_block(0, 16, False)
    for B in range(nblocks):
        bi = B % 2
        if B == 0:
            rk, t0, L, kdt = 0, 0, 16, kd16
        else:
            rk, t0, L, kdt = (B - 1) % 4, 16 + 128 * ((B - 1) // 4), 128, kd
        tix_, o_ = tile_of_tok(t0)
        P.dma(kvb[bi][:L, :], gv[tix_][rk, o_:o_ + L, :], w=[("kvb", bi)])
        for h in range(4):
            P.op("pool", "tensor_scalar", dict(out=kdec[bi][:L, h * 64:(h + 1) * 64], in0=kvb[bi][:L, h * 64:(h + 1) * 64],
                                               scalar1=kdt[:L, h:h + 1], scalar2=None, op0=ALU.mult),
                 r=[("kvb", bi), ("kd",), ("kd16",)], w=[("kdec", bi, h)])
        for h in range(4):
            P.op("pe", "matmul", dict(out=psU[bi][:64, h * 64:(h + 1) * 64], lhsT=kdec[bi][:L, h * 64:(h + 1) * 64],
                                      rhs=kvb[bi][:L, 256 + h * 64:256 + (h + 1) * 64], start=True, stop=True),
                 r=[("kdec", bi, h), ("kvb", bi)], w=[("psU", bi)])
        slot, pslot = B % 4, (B - 1) % 4
        if B == 0:
            P.op("dve", "tensor_copy", dict(out=ring[:, slot, :], in_=psU[bi][:64, 0:256]),
                 r=[("psU", bi)], w=[("ring", slot)])
        else:
            for h in range(4):
                P.op("dve", "scalar_tensor_tensor", dict(out=ring[:, slot, h * 64:(h + 1) * 64],
                                                         in0=ring[:, pslot, h * 64:(h + 1) * 64],
                                                         scalar=float(GAM[h] ** L), in1=psU[bi][:64, h * 64:(h + 1) * 64],
                                                         op0=ALU.mult, op1=ALU.add),
                     r=[("ring", pslot), ("psU", bi), ("ring", slot)], w=[("ring", slot)])
        if B % 4 == 3:
            ret_block(16 + 128 * (B // 4), 128, True)


def build_fused(cfg=None):
    cfg = cfg or {}
    P = Prog()
    D = P.dram
    x_in = D("hT0", [1024, NT], F32, "ExternalInput") if 0 in cfg.get("layers", (0, 1)) else None
    cs = D("cs", [NT, 512], F32, "ExternalInput")
    out = D("outT", [1024, NT], F32, "ExternalOutput")
    cst = dict(
        dmask=D("dmask", [128, 512], F32, "ExternalInput"),
        nb=D("nb", [12, 128, 1024], BF16, "ExternalInput"),
        nbm0=D("nbm0", [12, 128, 16], BF16, "ExternalInput"),
        nbmm=D("nbmm", [12, 16, 16], BF16, "ExternalInput"),
        b31=D("b31", [128, 12], F32, "ExternalInput"),
        identb=D("identb", [128, 128], BF16, "ExternalInput"),
        identf=D("identf", [128, 128], F32, "ExternalInput"),
        DT=D("DT", [4, 128, 128], F32, "ExternalInput"),
        QD=D("QD", [64, 4, 128], F32, "ExternalInput"),
        kd=D("kd", [128, 4], F32, "ExternalInput"),
        kd16=D("kd16", [16, 4], F32, "ExternalInput"),
        oh=D("oh", [128, 4], F32, "ExternalInput"),
    )
    gf = D("gf", [128, 8], F32, "ExternalInput")
    layers = cfg.get("layers", (0, 1))
    hmid = None
    if len(layers) == 2:
        hmid = D("hmid", [1024, NT], F32, "Internal")
    elif layers[0] == 1:
        hmid = D("hmid", [1024, NT], F32, "ExternalInput")
    hT = x_in if layers[0] == 0 else hmid
    for l in layers:
        w_in = D("w_in%d" % l, [1024, 3912], F32, "ExternalInput")
        gm = D("gm%d" % l, [128, 8], F32, "ExternalInput")
        wo = D("wo%d" % l, [1024, 1024], F32, "ExternalInput")
        w1 = D("w1_%d" % l, [1024, 4096], F32, "ExternalInput")
        w2 = D("w2_%d" % l, [4096, 1024], F32, "ExternalInput")
        gff = D("gff%d" % l, [128, 8], F32, "ExternalInput")
        cl = dict(cst)
        cl["lamb"] = D("lamb%d" % l, [128, 128], F32, "ExternalInput")
        cl["dnb"] = D("dnb%d" % l, [128, 64], F32, "ExternalInput")
        cl["rn"] = D("rn%d" % l, [128, 256], F32, "ExternalInput")
        fmo = D("fmo%d" % l, [NFM, NT], BF16, "Internal")
        tls = tiles_of(9)
        ksl = [D("ksl%d_%d" % (l, i), [NKROW, n_], BF16, "Internal") for i, (_, n_) in enumerate(tls)]
        tmb = [D("tmb%d_%d" % (l, i), [n_, NVB], BF16, "Internal") for i, (_, n_) in enumerate(tls)]
        tmk = [D("tmk%d_%d" % (l, i), [n_, NKC], F32, "Internal") for i, (_, n_) in enumerate(tls)]
        tmq = D("tmq%d" % l, [NT, NQC], F32, "Internal")
        gK = [D("gK%d_%d" % (l, i), [4 * NKROW, n_], BF16, "Internal") for i, (_, n_) in enumerate(tls)]
        gV = [D("gV%d_%d" % (l, i), [4 * n_, NVB], BF16, "Internal") for i, (_, n_) in enumerate(tls)]
        gKC = [D("gKC%d_%d" % (l, i), [4 * n_, NKC], F32, "Internal") for i, (_, n_) in enumerate(tls)]
        mix = D("mix%d" % l, [NT, 1024], BF16, "Internal")
        lambda_init = 0.8 - 0.6 * _math.exp(-0.3 * l)
        with P.phase():
            emit_p1(P, hT, w_in, gm, cs, fmo, ksl, tmb, tmk, tmq, gK, gV, gKC, cfg.get("p1_tiles", 9))
        with P.phase():
            emit_p2ab(P, fmo, gK, gV, tmq, mix, cl, cfg.get("njobs", NJ), True, lambda_init, cfg.get("nbis", NBIS))
        with P.phase():
            emit_p2c(P, gKC, tmq, tmk, mix, cl, cfg.get("nblocks", 128), True)
        with P.phase():
            emit_p3(P, hT, mix, wo, w1, w2, gff, gf, out if (l == 1 or len(layers) == 1) else hmid, cst["identb"],
                    cfg.get("p3_tiles", 17), final=(l == 1))
        hT = hmid
    return P.finish()


def rel_bucket_np(dist):
    n = np.maximum(dist, 0)
    nf = np.maximum(n, 1).astype(np.float32)
    large = 16 + (np.log(nf / np.float32(16)) / np.float32(_math.log(128 / 16)) * np.float32(16)).astype(np.int32)
    large = np.minimum(large, 31)
    return np.where(n < 16, n, large)


def local_positions(cc):
    pos = [np.arange(16)]
    for j in range(NJ):
        pos.append(16 + 128 * (4 * j + cc) + np.arange(128))
    return np.concatenate(pos)


def rope_table(cc):
    pos = local_positions(cc).astype(np.float32)
    inv = (np.float32(10000.0) ** (-np.arange(0, 64, 2, dtype=np.float32) / np.float32(64))).astype(np.float32)
    ang = pos[:, None] * inv[None, :]
    cos = np.cos(ang).astype(np.float32)
    sin = np.sin(ang).astype(np.float32)
    return np.ascontiguousarray(np.concatenate([np.tile(cos, (1, 8)), np.tile(sin, (1, 8))], axis=1))


def attn_consts(rel_bias, cc):
    i = np.arange(128)
    nb = np.empty((12, 128, 1024), np.float32)
    dmask = np.empty((128, 512), np.float32)
    for near in range(2):
        for cp in range(4):
            dR = (4 + cc - cp) if near == 0 else (cc - cp)
            dist = 128 * dR + i[:, None] - i[None, :]
            vis = dist >= 0
            bk = rel_bucket_np(dist)
            for hb in range(12):
                vals = rel_bias[bk, hb]
                nb[hb, :, near * 512 + cp * 128: near * 512 + (cp + 1) * 128] = np.where(vis, vals, np.float32(-BIG))
            if near == 1:
                dmask[:, cp * 128:(cp + 1) * 128] = np.where(vis, np.float32(0.0), np.float32(-BIG))
    s = np.arange(16)
    dist = 16 + 128 * cc + i[:, None] - s[None, :]
    bk = rel_bucket_np(dist)
    nbm0 = np.stack([rel_bias[bk, hb] for hb in range(12)]).astype(np.float32)
    dist = s[:, None] - s[None, :]
    bk = rel_bucket_np(dist)
    nbmm = np.stack([np.where(dist >= 0, rel_bias[bk, hb], np.float32(-BIG)) for hb in range(12)]).astype(np.float32)
    b31 = np.ascontiguousarray(np.broadcast_to(rel_bias[31][None, :], (128, 12))).astype(np.float32)
    return dict(dmask=dmask, nb=nb.astype(NP_BF16), nbm0=nbm0.astype(NP_BF16), nbmm=nbmm.astype(NP_BF16), b31=b31)


def ret_consts():
    i = np.arange(128)
    DT = np.zeros((4, 128, 128), np.float64)
    QD = np.zeros((64, 4, 128), np.float64)
    kd = np.zeros((128, 4), np.float64)
    kd16 = np.zeros((16, 4), np.float64)
    for h in range(4):
        g = GAM[h]
        d = i[None, :] - i[:, None]
        DT[h] = np.where(d >= 0, g ** np.maximum(d, 0), 0.0) / 8.0
        QD[:, h, :] = (g ** (i + 1.0))[None, :]
        kd[:, h] = g ** (127.0 - i) / 8.0
        kd16[:, h] = g ** (15.0 - np.arange(16)) / 8.0
    return dict(DT=DT.astype(np.float32), QD=QD.astype(np.float32), kd=kd.astype(np.float32), kd16=kd16.astype(np.float32))


def garr(v):
    return np.ascontiguousarray(np.asarray(v, np.float32).reshape(8, 128).T)


def shard_x(x, meta):
    hTs = []
    for c in range(8):
        b, cc = c // 4, c % 4
        xb = np.asarray(x[b], np.float32).reshape(NJ, 4, 128, 1024)[:, cc].reshape(NJ * 128, 1024)
        hTs.append(np.ascontiguousarray(np.concatenate([np.asarray(meta, np.float32), xb], axis=0).T))
    return hTs


def make_in_maps(inp):
    rel_bias = np.asarray(inp["rel_bias"], np.float32)
    hTs = shard_x(inp["x"], inp["meta"])
    rc = ret_consts()
    common = dict(identb=np.eye(128, dtype=np.float32).astype(NP_BF16), identf=np.eye(128, dtype=np.float32),
                  gf=garr(inp["final_norm"]), **rc)
    for l in range(2):
        common["w_in%d" % l] = np.ascontiguousarray(np.asarray(inp["w_in"][l], np.float32)[:, W_PERM])
        common["gm%d" % l] = garr(inp["norm_mix"][l])
        common["wo%d" % l] = np.ascontiguousarray(np.asarray(inp["w_out"][l], np.float32))
        common["w1_%d" % l] = np.ascontiguousarray(np.asarray(inp["w_ff1"][l], np.float32))
        common["w2_%d" % l] = np.ascontiguousarray(np.asarray(inp["w_ff2"][l], np.float32))
        common["gff%d" % l] = garr(inp["norm_ff"][l])
        common["lamb%d" % l] = np.ascontiguousarray(np.broadcast_to(
            np.asarray(inp["diff_lambda"][l], np.float32).reshape(1, 128), (128, 128)))
        common["dnb%d" % l] = np.ascontiguousarray(np.broadcast_to(
            np.asarray(inp["diff_norm"][l], np.float32).reshape(1, 64), (128, 64)))
        common["rn%d" % l] = np.ascontiguousarray(np.broadcast_to(
            np.asarray(inp["ret_norm"][l], np.float32).reshape(1, 256), (128, 256)))
    ropes = [rope_table(cc) for cc in range(4)]
    acs = [attn_consts(rel_bias, cc) for cc in range(4)]
    maps = []
    for c in range(8):
        cc = c % 4
        oh = np.zeros((128, 4), np.float32)
        oh[:, cc] = 1.0
        m = dict(common)
        m.update(acs[cc])
        m.update(hT0=hTs[c], cs=ropes[cc], oh=oh)
        maps.append(m)
    return maps


_NC_CACHE = {}


N_LAUNCH = 2


def run_fused(inp, cfg=None):
    cfg = dict(cfg or {})
    maps = make_in_maps(inp)
    if N_LAUNCH == 1:
        plans = [(0, 1)]
    else:
        plans = [(0,), (1,)]
    outs = None
    for layers in plans:
        c2 = dict(cfg)
        c2["layers"] = layers
        key = tuple(sorted(c2.items()))
        if key not in _NC_CACHE:
            _NC_CACHE[key] = build_fused(c2)
        if outs is not None:
            for c in range(8):
                maps[c]["hmid"] = outs[c]
        per_layer = ["w_in%d", "gm%d", "wo%d", "w1_%d", "w2_%d", "gff%d", "lamb%d", "dnb%d", "rn%d"]
        drop = set()
        for l in (0, 1):
            if l not in layers:
                drop |= {n % l for n in per_layer}
        if 0 not in layers:
            drop.add("hT0")
        if len(layers) == 2:
            drop.add("hmid")
        use = [{k: v for k, v in m.items() if k not in drop} for m in maps]
        res = run(_NC_CACHE[key], use)
        outs = [r["outT"] for r in res]
    return outs


def kernel(x, meta, rel_bias, w_in, norm_mix, diff_lambda, diff_norm, ret_norm, w_out, norm_ff, w_ff1, w_ff2, final_norm):
    inp = dict(x=x, meta=meta, rel_bias=rel_bias, w_in=w_in, norm_mix=norm_mix, diff_lambda=diff_lambda,
               diff_norm=diff_norm, ret_norm=ret_norm, w_out=w_out, norm_ff=norm_ff, w_ff1=w_ff1, w_ff2=w_ff2,
               final_norm=final_norm)
    inp = {k: np.asarray(v) for k, v in inp.items()}
    outs = run_fused(inp)
    out = np.empty((2, 16384, 1024), np.float32)
    for c in range(8):
        b, cc = c // 4, c % 4
        out[b].reshape(NJ, 4, 128, 1024)[:, cc] = outs[c][:, 16:].T.reshape(NJ, 128, 1024)
    return out
```

```python
import contextlib
import math as _math
import numpy as np
import concourse.bass as bass
import concourse.mybir as mybir
from concourse.bass_utils import run_bass_kernel_spmd

try:
    import ml_dtypes as _mld
    NP_BF16 = _mld.bfloat16
except Exception:
    NP_BF16 = None

F32 = mybir.dt.float32
BF16 = mybir.dt.bfloat16
U32 = mybir.dt.uint32
AF = mybir.ActivationFunctionType
ALU = mybir.AluOpType
AX = mybir.AxisListType

NSLOT = 24


class Prog:
    def __init__(self):
        self.nc = nc = bass.Bass("TRN2", target_bir_lowering=False)
        self.top = contextlib.ExitStack()
        self.cur = self.top
        self.engs = {"pe": nc.tensor, "act": nc.scalar, "dve": nc.vector, "pool": nc.gpsimd, "sp": nc.sync}
        self.sems = {e: self.top.enter_context(nc.semaphore("s_" + e)) for e in ("pe", "act", "dve", "pool")}
        self.cnt = {e: 0 for e in self.sems}
        self.slots = {q: [self.top.enter_context(nc.semaphore("d_%s_%d" % (q, s))) for s in range(NSLOT)]
                      for q in ("sp", "pool")}
        self.slotval = {q: [0] * NSLOT for q in self.slots}
        self.slotnext = {q: 0 for q in self.slots}
        self.ccs = []
        self.waited = {e: {} for e in self.engs}
        self.lastw = {}
        self.readers = {}
        self.nph = 0
        self.nuniq = 0

    def dram(self, name, shape, dt, kind):
        return self.nc.dram_tensor(name, list(shape), dt, kind=kind).ap()

    def sb(self, name, shape, dt):
        self.nuniq += 1
        return self.cur.enter_context(self.nc.sbuf_tensor("s%d_%s" % (self.nuniq, name), list(shape), dt))

    def ps(self, name, shape=(128, 512), dt=F32):
        self.nuniq += 1
        return self.cur.enter_context(self.nc.psum_tensor("p%d_%s" % (self.nuniq, name), list(shape), dt))

    @contextlib.contextmanager
    def phase(self):
        old = self.cur
        self.cur = contextlib.ExitStack()
        try:
            yield
        finally:
            self.barrier()
            self.cur.close()
            self.cur = old

    @staticmethod
    def _excl(k):
        return isinstance(k, tuple) and isinstance(k[0], str) and (k[0].startswith("ps") or k[0] in ("OA", "OB"))

    def _deps(self, eng, r, w, is_compute):
        w = tuple(w) + tuple(k for k in r if self._excl(k) and k not in w)
        toks = []
        for k in r:
            if k in self.lastw:
                toks.append(self.lastw[k])
        for k in w:
            if k in self.lastw:
                t = self.lastw[k]
                toks.append(t)
            for t in self.readers.get(k, ()):
                toks.append(t)
        need = {}
        for (sem, val, src) in toks:
            if is_compute and eng == "pe" and src == "pe":
                continue
            if is_compute and src == eng and val <= self.cnt[eng] - 4:
                continue
            if need.get(id(sem), (None, 0))[1] < val:
                need[id(sem)] = (sem, val)
        return w, need

    def _emit_waits(self, eng, need):
        E = self.engs[eng]
        wd = self.waited[eng]
        for sid, (sem, val) in need.items():
            if wd.get(sid, 0) >= val:
                continue
            E.wait_ge(sem, val)
            wd[sid] = val

    def _record(self, r, w, tok):
        for k in w:
            self.lastw[k] = tok
            self.readers[k] = []
        for k in r:
            if k not in w:
                self.readers.setdefault(k, []).append(tok)

    def op(self, eng, name, kw, r=(), w=()):
        w, need = self._deps(eng, r, w, True)
        self._emit_waits(eng, need)
        ins = getattr(self.engs[eng], name)(**kw)
        self.cnt[eng] += 1
        ins.then_inc(self.sems[eng], 1)
        self._record(r, w, (self.sems[eng], self.cnt[eng], eng))

    def dma(self, out, in_, r=(), w=(), q="sp", **kw):
        w, need = self._deps(q, r, w, False)
        si = self.slotnext[q]
        self.slotnext[q] = (si + 1) % NSLOT
        sem = self.slots[q][si]
        pv = self.slotval[q][si]
        if pv > 0 and need.get(id(sem), (None, 0))[1] < pv:
            need[id(sem)] = (sem, pv)
        self._emit_waits(q, need)
        ins = self.engs[q].dma_start(out=out, in_=in_, **kw)
        self.slotval[q][si] += 16
        ins.then_inc(sem, 16)
        self._record(r, w, (sem, self.slotval[q][si], "dma"))

    def coll(self, in_ap, out_ap, groups, r=()):
        _, need = self._deps("pool", r, (), False)
        self._emit_waits("pool", need)
        if not self.ccs:
            self.ccs = [self.top.enter_context(self.nc.semaphore("cc%d" % i)) for i in range(8)]
            self.ccval = [0] * 8
            self.ccn = 0
        i = self.ccn % 8
        self.ccn += 1
        sem = self.ccs[i]
        if self.ccval[i] > 0 and self.waited["pool"].get(id(sem), 0) < self.ccval[i]:
            self.nc.gpsimd.wait_ge(sem, self.ccval[i])
            self.waited["pool"][id(sem)] = self.ccval[i]
        ins = self.nc.gpsimd.collective_compute("AllGather", ALU.bypass, replica_groups=groups,
                                                ins=[in_ap], outs=[out_ap])
        ins.then_inc(sem)
        self.ccval[i] += 1

    def barrier(self):
        for eng, E in self.engs.items():
            wd = self.waited[eng]
            for e2, sem in self.sems.items():
                if self.cnt[e2] > 0 and wd.get(id(sem), 0) < self.cnt[e2]:
                    E.wait_ge(sem, self.cnt[e2])
                    wd[id(sem)] = self.cnt[e2]
            for q in self.slots:
                for si in range(NSLOT):
                    v = self.slotval[q][si]
                    sem = self.slots[q][si]
                    if v > 0 and wd.get(id(sem), 0) < v:
                        E.wait_ge(sem, v)
                        wd[id(sem)] = v
            for i, sem in enumerate(self.ccs):
                v = self.ccval[i]
                if v > 0 and wd.get(id(sem), 0) < v:
                    E.wait_ge(sem, v)
                    wd[id(sem)] = v
        self.lastw.clear()
        self.readers.clear()

    def finish(self):
        self.barrier()
        self.top.close()
        return self.nc


def run(prog_nc, in_maps):
    res = run_bass_kernel_spmd(prog_nc, in_maps, core_ids=list(range(len(in_maps))))
    return res.results


NT = 4112
NJ = 32
TALL = 16400
EPS = 1e-6
NBIS = 24
BIG = 30000.0
C_IDX = (8 ** -0.5) * (64 ** -0.5)
GAM = [1.0 - 2.0 ** (-5.0 - h) for h in range(4)]
GROUPS = [[0, 1, 2, 3], [4, 5, 6, 7]]
_OFF = dict(qa=0, ka=512, va=1024, qi=1536, ki=2048, wi=2112, qb=2120, kb=2376, vb=2632,
            qc=2888, kc=3144, vc=3400, gc=3656)
_SZ = dict(qa=512, ka=512, va=512, qi=512, ki=64, wi=8, qb=256, kb=256, vb=256, qc=256,
           kc=256, vc=256, gc=256)
FM_ORDER = ["qa", "qi", "qb", "ka", "kb", "ki"]
TM_ORDER = ["va", "vb", "kc", "vc", "qc", "gc", "wi"]
FM_OFF, TM_OFF = {}, {}
_o = 0
for _n in FM_ORDER:
    FM_OFF[_n] = _o
    _o += _SZ[_n]
NFM = _o
KROW0 = FM_OFF["ka"]
NKROW = NFM - KROW0
_o = 0
for _n in TM_ORDER:
    TM_OFF[_n] = _o
    _o += _SZ[_n]
NTM = _o
W_PERM = np.concatenate([np.arange(_OFF[n], _OFF[n] + _SZ[n]) for n in FM_ORDER + TM_ORDER])
NVB = 780
NKC = 512
NQC = 520


def tiles_of(ntiles):
    t = [(0, 16)]
    for i in range(8):
        t.append((16 + 512 * i, 512))
    return t[:ntiles]


def tiles256(ntiles):
    t = [(0, 16)]
    for i in range(16):
        t.append((16 + 256 * i, 256))
    return t[:ntiles]


def emit_rmsnorm_T(P, ht, out, ntok, g_sb, sq, ps_ss, rstd, sd, ones_bf, eps_t, htkey, outkey):
    P.op("act", "activation", dict(out=sq[:, :, :ntok], in_=ht[:, :, :ntok], func=AF.Square),
         r=[htkey], w=[("sq",)])
    for k in range(8):
        P.op("pe", "matmul", dict(out=ps_ss[:, :ntok], lhsT=ones_bf[:, :], rhs=sq[:, k, :ntok],
                                  start=(k == 0), stop=(k == 7)),
             r=[("sq",), ("ones",)], w=[("ps_ss",)])
    P.op("act", "activation", dict(out=sd[:, :ntok], in_=ps_ss[:, :ntok], func=AF.Sqrt,
                                   bias=eps_t[:, 0:1], scale=1.0 / 1024.0),
         r=[("ps_ss",), ("eps",)], w=[("sd",)])
    P.op("dve", "reciprocal", dict(out=rstd[:, :ntok], in_=sd[:, :ntok]), r=[("sd",)], w=[("rstd",)])
    for k in range(8):
        P.op("dve", "scalar_tensor_tensor", dict(out=out[:, k, :ntok], in0=ht[:, k, :ntok],
                                                 scalar=g_sb[:, k:k + 1], in1=rstd[:, :ntok],
                                                 op0=ALU.mult, op1=ALU.mult),
             r=[htkey, ("rstd",), ("g",)], w=[outkey])


def tile_of_tok(t0):
    if t0 < 16:
        return 0, t0
    return 1 + (t0 - 16) // 512, (t0 - 16) % 512


def emit_p1(P, hT, w, g, cs, fmo, ksl, tmb, tmk, tmq, gK, gV, gKC, ntiles=9):
    w_sb = P.sb("w_sb", [128, 8, 3912], BF16)
    g_sb = P.sb("g_sb", [128, 8], F32)
    ones_bf = P.sb("ones_bf", [128, 128], BF16)
    eps_t = P.sb("eps_t", [128, 1], F32)
    hts = [P.sb("ht%d" % i, [128, 8, 512], F32) for i in range(2)]
    uTs = [P.sb("uT%d" % i, [128, 8, 512], BF16) for i in range(2)]
    sq = P.sb("sq", [128, 8, 512], BF16)
    sd = P.sb("sd", [128, 512], F32)
    rstd = P.sb("rstd", [128, 512], F32)
    fms = [P.sb("fm%d" % i, [128, 512], BF16) for i in range(4)]
    tks = [P.sb("tk%d" % i, [128, NKC], F32) for i in range(2)]
    tqs = [P.sb("tq%d" % i, [128, NQC], F32) for i in range(2)]
    tbs = [P.sb("tb%d" % i, [128, 12, 65], BF16) for i in range(2)]
    css = [P.sb("cs%d" % i, [128, 512], F32) for i in range(2)]
    rt = P.sb("rt", [128, 4, 256], F32)
    ps_ss = P.ps("ps_ss")
    pss = [P.ps("ps%d" % i) for i in range(6)]
    hTv = hT.rearrange("(k p) t -> p k t", p=128)
    wv = w.rearrange("(k p) c -> p k c", p=128)
    P.op("dve", "memset", dict(ap=ones_bf[:, :], constant=1.0), w=[("ones",)])
    P.op("dve", "memset", dict(ap=eps_t[:, :], constant=EPS), w=[("eps",)])
    P.dma(g_sb[:, :], g[:, :], w=[("g",)])
    for i in range(2):
        P.op("dve", "memset", dict(ap=tbs[i][:, :, :], constant=1.0), w=[("tb", i, 0), ("tb", i, 1)])
    for k in range(8):
        P.dma(w_sb[:, k, :], wv[:, k, :], w=[("w", k)], q="pool")
    scale_of = {"qa": 0.125, "qb": 32.0 ** -0.5}
    fm_groups = []
    for n in FM_ORDER:
        for r0 in range(0, _SZ[n], 128):
            fm_groups.append((FM_OFF[n] + r0, min(128, _SZ[n] - r0), scale_of.get(n, 1.0)))
    tm_groups = [(0, 512), (512, 512), (1024, 512), (1536, 264)]
    pc = [0]
    fc = [0]
    sc_ = [0]

    def nextps():
        i = pc[0] % 6
        pc[0] += 1
        return i

    for ti, (tok0, ntok) in enumerate(tiles_of(ntiles)):
        b = ti % 2
        ht, uT = hts[b], uTs[b]
        P.dma(ht[:, :, :ntok], hTv[:, :, tok0:tok0 + ntok], w=[("ht", b)])
        emit_rmsnorm_T(P, ht, uT, ntok, g_sb, sq, ps_ss, rstd, sd, ones_bf, eps_t, ("ht", b), ("uT", b))
        for (r0, M, sc) in fm_groups:
            pi = nextps()
            ps = pss[pi]
            for k in range(8):
                P.op("pe", "matmul", dict(out=ps[:M, :ntok], lhsT=w_sb[:, k, r0:r0 + M], rhs=uT[:, k, :ntok],
                                          start=(k == 0), stop=(k == 7)),
                     r=[("uT", b), ("w", k)], w=[("ps", pi)])
            fi = fc[0] % 4
            fc[0] += 1
            fm = fms[fi]
            P.op("act", "activation", dict(out=fm[:M, :ntok], in_=ps[:M, :ntok], func=AF.Copy, scale=sc),
                 r=[("ps", pi)], w=[("fm", fi)])
            if r0 >= KROW0:
                P.dma(ksl[ti][r0 - KROW0:r0 - KROW0 + M, 0:ntok], fm[:M, :ntok], r=[("fm", fi)], w=[("dK", ti, r0)])
            else:
                P.dma(fmo[r0:r0 + M, tok0:tok0 + ntok], fm[:M, :ntok], r=[("fm", fi)])
        nsb = max(1, ntok // 128)
        for s in range(nsb):
            nt = min(128, ntok)
            t0 = tok0 + s * 128
            si = sc_[0] % 2
            sc_[0] += 1
            tk, tq, tb, cst = tks[si], tqs[si], tbs[si], css[si]
            P.dma(cst[:nt, :], cs[t0:t0 + nt, :], w=[("cs", si)])
            for gi, (c0, ncol) in enumerate(tm_groups):
                pi = nextps()
                ps = pss[pi]
                for k in range(8):
                    P.op("pe", "matmul", dict(out=ps[:nt, :ncol], lhsT=uT[:, k, s * 128:s * 128 + nt],
                                              rhs=w_sb[:, k, NFM + c0:NFM + c0 + ncol],
                                              start=(k == 0), stop=(k == 7)),
                         r=[("uT", b), ("w", k)], w=[("ps", pi)])
                if gi == 0:
                    P.op("dve", "tensor_copy", dict(out=tb[:nt, 0:8, 0:64],
                                                    in_=ps[:nt, 0:512].rearrange("p (h d) -> p h d", d=64)),
                         r=[("ps", pi)], w=[("tb", si, 0)])
                elif gi == 1:
                    P.op("act", "activation", dict(out=tb[:nt, 8:12, 0:64],
                                                   in_=ps[:nt, 0:256].rearrange("p (h d) -> p h d", d=64), func=AF.Copy),
                         r=[("ps", pi)], w=[("tb", si, 1)])
                    emit_rotary(P, ps[:nt, 256:512], tk[:nt, 0:256], cst, nt, rt, ("ps", pi), ("cs", si), ("tk", si, 0))
                elif gi == 2:
                    P.op("act", "activation", dict(out=tk[:nt, 256:512], in_=ps[:nt, 0:256], func=AF.Copy),
                         r=[("ps", pi)], w=[("tk", si, 1)])
                    emit_rotary(P, ps[:nt, 256:512], tq[:nt, 0:256], cst, nt, rt, ("ps", pi), ("cs", si), ("tq", si, 0))
                else:
                    P.op("act", "activation", dict(out=tq[:nt, 256:520], in_=ps[:nt, 0:264], func=AF.Copy),
                         r=[("ps", pi)], w=[("tq", si, 1)])
            P.dma(tmb[ti][s * 128:s * 128 + nt, :], tb[:nt, :, :].rearrange("p h f -> p (h f)"),
                  r=[("tb", si, 0), ("tb", si, 1)], w=[("dV", ti, s)])
            P.dma(tmk[ti][s * 128:s * 128 + nt, :], tk[:nt, :], r=[("tk", si, 0), ("tk", si, 1)], w=[("dC", ti, s)])
            P.dma(tmq[t0:t0 + nt, :], tq[:nt, :], r=[("tq", si, 0), ("tq", si, 1)])
        P.coll(ksl[ti][:, :], gK[ti][:, :], GROUPS, r=[("dK", ti, r0) for (r0, M, sc) in fm_groups if r0 >= KROW0])
        P.coll(tmb[ti][:, :], gV[ti][:, :], GROUPS, r=[("dV", ti, s) for s in range(nsb)])
        P.coll(tmk[ti][:, :], gKC[ti][:, :], GROUPS, r=[("dC", ti, s) for s in range(nsb)])


def emit_rotary(P, ps256, out256, cst, nt, rt, pskey, cskey, outkey):
    psv = ps256.rearrange("p (h d) -> p h d", d=64)
    x1, x2 = psv[:, :, 0:32], psv[:, :, 32:64]
    cosv = cst[:nt, 0:128].rearrange("p (h d) -> p h d", d=32)
    sinv = cst[:nt, 256:384].rearrange("p (h d) -> p h d", d=32)
    tv = [rt[:nt, i, 0:128].rearrange("p (h d) -> p h d", d=32) for i in range(4)]
    ov = out256.rearrange("p (h d) -> p h d", d=64)
    for i, (a_, b_) in enumerate([(x1, cosv), (x2, sinv), (x1, sinv), (x2, cosv)]):
        P.op("dve", "tensor_tensor", dict(out=tv[i], in0=a_, in1=b_, op=ALU.mult),
             r=[pskey, cskey], w=[("rt", i)])
    P.op("pool", "tensor_tensor", dict(out=ov[:, :, 0:32], in0=tv[0], in1=tv[1], op=ALU.subtract),
         r=[("rt", 0), ("rt", 1)], w=[outkey])
    P.op("pool", "tensor_tensor", dict(out=ov[:, :, 32:64], in0=tv[2], in1=tv[3], op=ALU.add),
         r=[("rt", 2), ("rt", 3), outkey], w=[outkey])


def emit_p3(P, hT, mix, wo, w1, w2, g, gf, hoT, identb, ntiles=17, final=False):
    wo_sb = P.sb("wo_sb", [128, 8, 1024], BF16)
    w1_sb = P.sb("w1_sb", [128, 8, 4096], BF16)
    w2_sb = P.sb("w2_sb", [128, 32, 1024], BF16)
    g_sb = P.sb("g_sb", [128, 8], F32)
    gf_sb = P.sb("gf_sb", [128, 8], F32)
    ones_bf = P.sb("ones_bf", [128, 128], BF16)
    idb = P.sb("idb", [128, 128], BF16)
    eps_t = P.sb("eps_t", [128, 1], F32)
    ht = P.sb("ht", [128, 8, 256], F32)
    mts = [P.sb("mt%d" % i, [128, 1024], BF16) for i in range(2)]
    mx = P.sb("mx", [128, 8, 256], BF16)
    uT = P.sb("uT", [128, 8, 256], BF16)
    sq = P.sb("sq", [128, 8, 256], BF16)
    hid = P.sb("hid", [128, 32, 256], BF16)
    sd = P.sb("sd", [128, 256], F32)
    rstd = P.sb("rstd", [128, 256], F32)
    rl = [P.sb("rl%d" % i, [128, 256], F32) for i in range(2)]
    ps_ss = P.ps("ps_ss")
    pss = [P.ps("ps%d" % i) for i in range(5)]
    ptr = [P.ps("ptr%d" % i, [128, 512], BF16) for i in range(2)]
    hTv = hT.rearrange("(k p) t -> p k t", p=128)
    hoTv = hoT.rearrange("(k p) t -> p k t", p=128)
    P.op("dve", "memset", dict(ap=ones_bf[:, :], constant=1.0), w=[("ones",)])
    P.op("dve", "memset", dict(ap=eps_t[:, :], constant=EPS), w=[("eps",)])
    P.dma(g_sb[:, :], g[:, :], w=[("g",)])
    P.dma(gf_sb[:, :], gf[:, :], w=[("gf",)])
    P.dma(idb[:, :], identb[:, :], w=[("idb",)])
    wov = wo.rearrange("(k p) c -> p k c", p=128)
    w1v = w1.rearrange("(k p) c -> p k c", p=128)
    w2v = w2.rearrange("(f p) c -> p f c", p=128)
    for k in range(8):
        P.dma(wo_sb[:, k, :], wov[:, k, :], w=[("wo", k)], q="pool")
    for k in range(8):
        P.dma(w1_sb[:, k, :], w1v[:, k, :], w=[("w1", k)], q="pool")
    for f in range(0, 32, 4):
        P.dma(w2_sb[:, f:f + 4, :], w2v[:, f:f + 4, :], w=[("w2", f // 4)], q="pool")
    pc = [0]

    def nextps():
        i = pc[0] % 5
        pc[0] += 1
        return i

    rc = 0
    mc = 0
    tc = 0
    for ti, (tok0, ntok) in enumerate(tiles256(ntiles)):
        P.dma(ht[:, :, :ntok], hTv[:, :, tok0:tok0 + ntok], w=[("ht",)])
        nsb = max(1, ntok // 128)
        for s in range(nsb):
            nt = min(128, ntok)
            mi = mc % 2
            mc += 1
            mt = mts[mi]
            P.dma(mt[:nt, :], mix[tok0 + s * 128: tok0 + s * 128 + nt, :], w=[("mt", mi)])
            for kq in range(2):
                ti2 = tc % 2
                tc += 1
                pt = ptr[ti2]
                for i in range(4):
                    kk = 4 * kq + i
                    P.op("pe", "transpose", dict(out=pt[:, i * 128:i * 128 + nt], in_=mt[:nt, kk * 128:(kk + 1) * 128],
                                                 identity=idb[:nt, :nt]),
                         r=[("mt", mi), ("idb",)], w=[("ptr", ti2)])
                P.op("act", "activation", dict(out=mx[:, 4 * kq:4 * kq + 4, s * 128:s * 128 + nt],
                                               in_=pt[:, :].rearrange("p (c t) -> p c t", t=128)[:, :, :nt], func=AF.Copy),
                     r=[("ptr", ti2)], w=[("mx", s, kq)])
        mxkeys = [("mx", s, kq) for s in range(nsb) for kq in range(2)]
        for m in range(8):
            pi = nextps()
            ps = pss[pi]
            for k in range(8):
                P.op("pe", "matmul", dict(out=ps[:, :ntok], lhsT=wo_sb[:, k, m * 128:(m + 1) * 128], rhs=mx[:, k, :ntok],
                                          start=(k == 0), stop=(k == 7)),
                     r=mxkeys + [("wo", k)], w=[("ps", pi)])
            P.op("dve", "tensor_tensor", dict(out=ht[:, m, :ntok], in0=ht[:, m, :ntok], in1=ps[:, :ntok], op=ALU.add),
                 r=[("ps", pi), ("ht",)], w=[("ht",)])
        emit_rmsnorm_T(P, ht, uT, ntok, g_sb, sq, ps_ss, rstd, sd, ones_bf, eps_t, ("ht",), ("uT",))
        for f in range(32):
            pi = nextps()
            ps = pss[pi]
            for k in range(8):
                P.op("pe", "matmul", dict(out=ps[:, :ntok], lhsT=w1_sb[:, k, f * 128:(f + 1) * 128], rhs=uT[:, k, :ntok],
                                          start=(k == 0), stop=(k == 7)),
                     r=[("uT",), ("w1", k)], w=[("ps", pi)])
            ri = rc % 2
            rc += 1
            P.op("act", "activation", dict(out=rl[ri][:, :ntok], in_=ps[:, :ntok], func=AF.Relu),
                 r=[("ps", pi)], w=[("rl", ri)])
            P.op("pool", "tensor_tensor", dict(out=hid[:, f, :ntok], in0=rl[ri][:, :ntok], in1=rl[ri][:, :ntok], op=ALU.mult),
                 r=[("rl", ri)], w=[("hid", f)])
        for m in range(8):
            pi = nextps()
            ps = pss[pi]
            for f in range(32):
                P.op("pe", "matmul", dict(out=ps[:, :ntok], lhsT=w2_sb[:, f, m * 128:(m + 1) * 128], rhs=hid[:, f, :ntok],
                                          start=(f == 0), stop=(f == 31)),
                     r=[("hid", f), ("w2", f // 4)], w=[("ps", pi)])
            P.op("dve", "tensor_tensor", dict(out=ht[:, m, :ntok], in0=ht[:, m, :ntok], in1=ps[:, :ntok], op=ALU.add),
                 r=[("ps", pi), ("ht",)], w=[("ht",)])
        if final:
            emit_rmsnorm_T(P, ht, ht, ntok, gf_sb, sq, ps_ss, rstd, sd, ones_bf, eps_t, ("ht",), ("ht",))
        P.dma(hoTv[:, :, tok0:tok0 + ntok], ht[:, :, :ntok], r=[("ht",)])


def emit_p2ab(P, fm, gK, gV, tmq, mix, cst, njobs=NJ, do_meta=True, lambda_init=0.2, nbis=NBIS):
    S = P.sb
    acc = S("acc", [128, TALL], F32)
    MB = S("MB", [128, TALL], BF16)
    MBN = S("MBN", [128, 8, 1024], BF16)
    MBNm = S("MBNm", [128, 8, 16], BF16)
    NB = S("NB", [128, 12, 1024], BF16)
    NBm0 = S("NBm0", [128, 12, 16], BF16)
    NBmm = S("NBmm", [16, 12, 16], BF16)
    dm = S("dm", [128, 512], F32)
    dtmp = S("dtmp", [128, 512], F32)
    b31s = S("b31s", [128, 12], F32)
    zcol = S("zcol", [128, 1], F32)
    half = S("half", [128, 1], F32)
    epsc = S("epsc", [128, 1], F32)
    idn = S("idn", [128, 128], BF16)
    lam_in = S("lam_in", [128, 128], F32)
    dn = S("dn", [128, 64], F32)
    kis = [S("ki%d" % i, [128, 512], BF16) for i in range(2)]
    kas = [S("ka%d" % i, [128, 4, 512], BF16) for i in range(2)]
    kbs = [S("kb%d" % i, [128, 2, 512], BF16) for i in range(2)]
    vas = [S("va%d" % i, [128, 4, 520], BF16) for i in range(2)]
    vbs = [S("vb%d" % i, [128, 4, 260], BF16) for i in range(2)]
    qa = S("qa", [128, 8, 128], BF16)
    qb = S("qb", [128, 8, 128], BF16)
    qi = S("qi", [128, 8, 128], BF16)
    wis = S("wis", [128, 8], F32)
    absw = S("absw", [128, 8], F32)
    sgn = S("sgn", [128, 8], F32)
    rbuf = [S("r%d" % i, [128, 512], F32) for i in range(3)]
    pTs = [S("pT%d" % i, [128, 512], BF16) for i in range(3)]
    sm = S("sm", [128, 32], F32)
    smu = S("smu", [128, 4], U32)
    abo = S("abo", [128, 768], BF16)
    fin = S("fin", [128, 512], F32)
    psI = [P.ps("psI%d" % i) for i in range(2)]
    psS = [P.ps("psS%d" % i) for i in range(2)]
    OA = [P.ps("OA%d" % i) for i in range(2)]
    OB = [P.ps("OB%d" % i) for i in range(2)]
    LO, HI, MID, CNT, RMIN, RTMP, LAM, NLAM = 0, 1, 2, 3, 4, 5, 6, 7
    gKv = [a.rearrange("(r f) t -> r f t", r=4) for a in gK]
    gVv = [a.rearrange("(r t) f -> r t f", r=4) for a in gV]
    KA0, KB0, KI0 = 0, FM_OFF["kb"] - KROW0, FM_OFF["ki"] - KROW0

    def smc(i):
        return sm[:, i:i + 1]

    P.dma(dm[:, :], cst["dmask"][:, :], w=[("dm",)])
    P.dma(NB[:, :, :], cst["nb"].rearrange("h p k -> p h k"), w=[("NB",)])
    P.dma(NBm0[:, :, :], cst["nbm0"].rearrange("h p k -> p h k"), w=[("NBm0",)])
    P.dma(NBmm[:, :, :], cst["nbmm"].rearrange("h p k -> p h k"), w=[("NBmm",)])
    P.dma(b31s[:, :], cst["b31"][:, :], w=[("b31",)])
    P.dma(idn[:, :], cst["identb"][:, :], w=[("idn",)])
    P.dma(lam_in[:, :], cst["lamb"][:, :], w=[("lam_in",)])
    P.dma(dn[:, :], cst["dnb"][:, :], w=[("dn",)])
    P.op("dve", "memset", dict(ap=zcol[:, :], constant=0.0), w=[("zcol",)])
    P.op("dve", "memset", dict(ap=half[:, :], constant=0.5), w=[("half",)])
    P.op("dve", "memset", dict(ap=epsc[:, :], constant=EPS), w=[("epsc",)])
    P.op("dve", "memset", dict(ap=qa[:, :, :], constant=0.0), w=[("qa", h) for h in range(8)])
    P.op("dve", "memset", dict(ap=qb[:, :, :], constant=0.0), w=[("qb", h) for h in range(8)])
    P.op("dve", "memset", dict(ap=qi[:, :, :], constant=0.0), w=[("qi", h) for h in range(8)])
    P.op("dve", "tensor_tensor", dict(out=fin[:, 0:32], in0=lam_in[:, 0:32], in1=lam_in[:, 32:64], op=ALU.mult),
         r=[("lam_in",)], w=[("fin",)])
    P.op("dve", "tensor_tensor", dict(out=fin[:, 32:64], in0=lam_in[:, 64:96], in1=lam_in[:, 96:128], op=ALU.mult),
         r=[("lam_in",), ("fin",)], w=[("fin",)])
    P.op("dve", "tensor_reduce", dict(out=sm[:, 8:10], in_=fin[:, 0:64].rearrange("p (a b) -> p a b", b=32),
                                      axis=AX.X, op=ALU.add), r=[("fin",)], w=[("sm", "l")])
    P.op("act", "activation", dict(out=sm[:, 10:12], in_=sm[:, 8:10], func=AF.Exp), r=[("sm", "l")], w=[("sm", "l2")])
    P.op("dve", "tensor_tensor", dict(out=smc(LAM), in0=sm[:, 11:12], in1=sm[:, 10:11], op=ALU.subtract),
         r=[("sm", "l2")], w=[("sm", "lam")])
    P.op("dve", "tensor_scalar", dict(out=smc(NLAM), in0=smc(LAM), scalar1=-float(lambda_init), scalar2=None,
                                      op0=ALU.add), r=[("sm", "lam")], w=[("sm", "nlam")])
    P.op("dve", "tensor_scalar", dict(out=dn[:, :], in0=dn[:, :], scalar1=float(1.0 - lambda_init), scalar2=None,
                                      op0=ALU.mult), r=[("dn",)], w=[("dn",)])

    tcount = [0]
    rcount = [0]
    pcount = [0]
    scount = [0]
    icount = [0]

    pend = [None]

    def attend(nq, tiles, meta_mode, j):
        for i in range(2):
            P.op("dve", "memset", dict(ap=OA[i][:nq, 0:260], constant=0.0), w=[("OA", i)])
            P.op("dve", "memset", dict(ap=OB[i][:nq, 0:260], constant=0.0), w=[("OB", i)])
        steps = [("t", g, near) for (g, near) in tiles] + [("m", None, None)]
        for (kind, g, near) in steps:
            tb = tcount[0] % 2
            tcount[0] += 1
            ka_t, kb_t, va_t, vb_t = kas[tb], kbs[tb], vas[tb], vbs[tb]
            if kind == "t":
                tix, t0 = tile_of_tok(16 + 128 * g)
                nkb, nk = 4, 128
                allr = lambda nm: [(nm, tb, r_) for r_ in range(4)]
                for c_ in range(4):
                    P.dma(ka_t[:, c_, :].rearrange("p (r t) -> p r t", r=4),
                          gKv[tix][:, KA0 + c_ * 128:KA0 + (c_ + 1) * 128, t0:t0 + 128].rearrange("r p t -> p r t"),
                          w=[("ka", tb, c_ + 10)])
                for c_ in range(2):
                    P.dma(kb_t[:, c_, :].rearrange("p (r t) -> p r t", r=4),
                          gKv[tix][:, KB0 + c_ * 128:KB0 + (c_ + 1) * 128, t0:t0 + 128].rearrange("r p t -> p r t"),
                          w=[("kb", tb, c_ + 10)])
                P.dma(va_t[:, :, :], gVv[tix][:, t0:t0 + 128, 0:520].rearrange("r p f -> p r f"), w=allr("va"))
                P.dma(vb_t[:, :, :], gVv[tix][:, t0:t0 + 128, 520:780].rearrange("r p f -> p r f"), w=allr("vb"))
            else:
                nkb, nk = 1, 16
                P.dma(ka_t[:, :, 0:16], gKv[0][0, KA0:KA0 + 512, 0:16].rearrange("(c p) t -> p c t", p=128),
                      w=[("ka", tb, c_ + 10) for c_ in range(4)])
                P.dma(kb_t[:, :, 0:16], gKv[0][0, KB0:KB0 + 256, 0:16].rearrange("(c p) t -> p c t", p=128),
                      w=[("kb", tb, c_ + 10) for c_ in range(2)])
                P.dma(va_t[0:16, 0, :], gVv[0][0, 0:16, 0:520], w=[("va", tb, r_) for r_ in range(4)])
                P.dma(vb_t[0:16, 0, :], gVv[0][0, 0:16, 520:780], w=[("vb", tb, r_) for r_ in range(4)])
            for hh in range(16):
                isA = hh < 8
                if isA:
                    h = hh
                    qz, qkey = qa[:, h, :nq], ("qa", h)
                    kt, kname, vt, vname = ka_t, "ka", va_t, "va"
                    kc_ = h // 2
                    Oap = OA[h // 4][:nq, (h % 4) * 65:(h % 4 + 1) * 65]
                    Okey = ("OA", h // 4)
                    hb = h
                else:
                    hc = hh - 8
                    h = hc // 2
                    qz, qkey = qb[:, hc, :nq], ("qb", hc)
                    kt, kname, vt, vname = kb_t, "kb", vb_t, "vb"
                    kc_ = h // 2
                    Oap = OB[hc // 4][:nq, (hc % 4) * 65:(hc % 4 + 1) * 65]
                    Okey = ("OB", hc // 4)
                    hb = 8 + h
                si = scount[0] % 2
                scount[0] += 1
                ps = psS[si]

                def mop(blk):
                    if kind == "t":
                        if near is not None:
                            if isA:
                                return MBN[:nq, h, near * 512 + blk * 128: near * 512 + (blk + 1) * 128], ("MBN", h)
                            return NB[:nq, hb, near * 512 + blk * 128: near * 512 + (blk + 1) * 128], ("NB",)
                        if isA:
                            return MB[:nq, g * 512 + blk * 128: g * 512 + (blk + 1) * 128], ("MB",)
                        return None, None
                    if meta_mode == "mm":
                        return NBmm[:nq, hb, :], ("NBmm",)
                    if meta_mode == "j0":
                        if isA:
                            return MBNm[:nq, h, :], ("MBNm",)
                        return NBm0[:nq, hb, :], ("NBm0",)
                    if isA:
                        return MB[:nq, 512 * (j + 1): 512 * (j + 1) + 16], ("MB",)
                    return None, None
                first = True
                for blk in range(nkb):
                    P.op("pe", "matmul", dict(out=ps[:nk, blk * 128:blk * 128 + nq],
                                              lhsT=kt[:, kc_, blk * 128:blk * 128 + nk], rhs=qz,
                                              start=first, stop=False, skip_group_check=True),
                         r=[(kname, tb, kc_ + 10), qkey], w=[("psS", si)])
                    first = False
                for blk in range(nkb):
                    m_ap, m_key = mop(blk)
                    if m_ap is not None:
                        P.op("pe", "matmul", dict(out=ps[:nk, blk * 128:blk * 128 + nq], lhsT=m_ap, rhs=idn[:nq, :nq],
                                                  start=False, stop=False, skip_group_check=True),
                             r=[m_key, ("idn",)], w=[("psS", si)])
                usebias = (kind == "t" and near is None) or (kind == "m" and meta_mode == "far")
                bias_ap = b31s[:nk, hb:hb + 1] if usebias else zcol[:nk, 0:1]
                pi = pcount[0] % 3
                pcount[0] += 1
                pT = pTs[pi]
                ncol = (nkb - 1) * 128 + nq
                P.op("act", "activation", dict(out=pT[:nk, :ncol], in_=ps[:nk, :ncol], func=AF.Exp, bias=bias_ap),
                     r=[("psS", si), ("b31",), ("zcol",)], w=[("pT", pi)])
                if pend[0] is not None:
                    pend[0]()

                def pv(Oap=Oap, pT=pT, nk=nk, nq=nq, vt=vt, h=h, pi=pi, vname=vname, tb=tb, Okey=Okey, nkb=nkb):
                    for blk in range(nkb):
                        P.op("pe", "matmul", dict(out=Oap, lhsT=pT[:nk, blk * 128:blk * 128 + nq],
                                                  rhs=vt[:nk, blk, h * 65:(h + 1) * 65],
                                                  start=False, stop=False, skip_group_check=True),
                             r=[("pT", pi), (vname, tb, blk)], w=[Okey])
                pend[0] = pv
        if pend[0] is not None:
            pend[0]()
            pend[0] = None

    def finalize(nq, tok0):
        for i in range(2):
            ov = OA[i][:nq, 0:260].rearrange("p (h f) -> p h f", f=65)
            P.op("dve", "reciprocal", dict(out=sm[:nq, 12 + 4 * i:16 + 4 * i], in_=ov[:, :, 64]),
                 r=[("OA", i)], w=[("sm", "ra", i)])
            for hl in range(4):
                h = 4 * i + hl
                P.op("dve", "tensor_scalar", dict(out=abo[:nq, h * 64:(h + 1) * 64], in0=ov[:, hl, 0:64],
                                                  scalar1=sm[:nq, 12 + h:13 + h], scalar2=None, op0=ALU.mult),
                     r=[("OA", i), ("sm", "ra", i)], w=[("abo", h)])
        for i in range(2):
            ov = OB[i][:nq, 0:260].rearrange("p (h f) -> p h f", f=65)
            P.op("dve", "reciprocal", dict(out=sm[:nq, 20 + 4 * i:24 + 4 * i], in_=ov[:, :, 64]),
                 r=[("OB", i)], w=[("sm", "rb", i)])
        rv = sm[:nq, 20:28].rearrange("p (h c) -> p h c", c=2)[:, :, 1]
        P.op("dve", "tensor_scalar", dict(out=rv, in0=rv, scalar1=sm[:nq, NLAM:NLAM + 1], scalar2=None, op0=ALU.mult),
             r=[("sm", "rb", 0), ("sm", "rb", 1), ("sm", "nlam")], w=[("sm", "rb", 0), ("sm", "rb", 1)])
        for h in range(4):
            i = h // 2
            ov = OB[i][:nq, 0:260].rearrange("p (h f) -> p h f", f=65)
            c0, c1 = (2 * h) % 4, (2 * h + 1) % 4
            t0 = fin[:nq, h * 64:(h + 1) * 64]
            bh = fin[:nq, 256 + h * 64:256 + (h + 1) * 64]
            P.op("dve", "tensor_scalar", dict(out=t0, in0=ov[:, c0, 0:64], scalar1=sm[:nq, 20 + 2 * h:21 + 2 * h],
                                              scalar2=None, op0=ALU.mult),
                 r=[("OB", i), ("sm", "rb", i)], w=[("fin", h)])
            P.op("dve", "scalar_tensor_tensor", dict(out=bh, in0=ov[:, c1, 0:64], scalar=sm[:nq, 21 + 2 * h:22 + 2 * h],
                                                     in1=t0, op0=ALU.mult, op1=ALU.add),
                 r=[("OB", i), ("sm", "rb", i), ("fin", h)], w=[("finb", h)])
            P.op("dve", "tensor_tensor", dict(out=t0, in0=bh, in1=bh, op=ALU.mult), r=[("finb", h)], w=[("fin", h)])
            P.op("dve", "tensor_reduce", dict(out=sm[:nq, 28 + h:29 + h], in_=t0, axis=AX.X, op=ALU.add),
                 r=[("fin", h)], w=[("sm", "ss", h)])
            P.op("act", "activation", dict(out=sm[:nq, 28 + h:29 + h], in_=sm[:nq, 28 + h:29 + h], func=AF.Sqrt,
                                           bias=epsc[:nq, 0:1], scale=1.0 / 64.0),
                 r=[("sm", "ss", h), ("epsc",)], w=[("sm", "ss", h)])
            P.op("dve", "reciprocal", dict(out=sm[:nq, 28 + h:29 + h], in_=sm[:nq, 28 + h:29 + h]),
                 r=[("sm", "ss", h)], w=[("sm", "ss", h)])
            P.op("dve", "scalar_tensor_tensor", dict(out=abo[:nq, 512 + h * 64:512 + (h + 1) * 64], in0=bh,
                                                     scalar=sm[:nq, 28 + h:29 + h], in1=dn[:nq, :],
                                                     op0=ALU.mult, op1=ALU.mult),
                 r=[("finb", h), ("sm", "ss", h), ("dn",)], w=[("abo", 8 + h)])
        P.dma(mix[tok0:tok0 + nq, 0:768], abo[:nq, :], r=[("abo", k) for k in range(12)])

    def load_q(tok0, nq, need_idx):
        for h in range(8):
            P.dma(qa[(h % 2) * 64:(h % 2) * 64 + 64, h, :nq],
                  fm[FM_OFF["qa"] + h * 64:FM_OFF["qa"] + (h + 1) * 64, tok0:tok0 + nq], w=[("qa", h)])
        for hc in range(8):
            h, c = hc // 2, hc % 2
            p0 = (h % 2) * 64 + c * 32
            r0 = FM_OFF["qb"] + h * 64 + c * 32
            P.dma(qb[p0:p0 + 32, hc, :nq], fm[r0:r0 + 32, tok0:tok0 + nq], w=[("qb", hc)])
        if need_idx:
            for h in range(8):
                P.dma(qi[(h % 2) * 64:(h % 2) * 64 + 64, h, :nq],
                      fm[FM_OFF["qi"] + h * 64:FM_OFF["qi"] + (h + 1) * 64, tok0:tok0 + nq], w=[("qi", h)])
            P.dma(wis[:nq, :], tmq[tok0:tok0 + nq, 512:520], w=[("wis",)])

    if do_meta:
        load_q(0, 16, False)
        attend(16, [], "mm", None)
        finalize(16, 0)

    for j in range(njobs):
        tok0 = 16 + 128 * j
        nq = 128
        n = 512 * (j + 1) + 16
        load_q(tok0, nq, True)
        P.op("act", "activation", dict(out=absw[:, :], in_=wis[:, :], func=AF.Abs, scale=float(C_IDX)),
             r=[("wis",)], w=[("absw",)])
        P.op("act", "activation", dict(out=sgn[:, :], in_=wis[:, :], func=AF.Sign), r=[("wis",)], w=[("sgn",)])
        acckeys = []
        for g in list(range(j + 1)) + ["m"]:
            kb_ = icount[0] % 2
            icount[0] += 1
            ki_t = kis[kb_]
            if g == "m":
                nk, c0 = 16, 512 * (j + 1)
                for e in range(2):
                    P.dma(ki_t[e * 64:(e + 1) * 64, 0:16], gKv[0][0, KI0:KI0 + 64, 0:16], w=[("ki", kb_, e)])
            else:
                nk, c0 = 512, 512 * g
                tix, t0 = tile_of_tok(16 + 128 * g)
                for e in range(2):
                    P.dma(ki_t[e * 64:(e + 1) * 64, :].rearrange("p (r t) -> p r t", r=4),
                          gKv[tix][:, KI0:KI0 + 64, t0:t0 + 128].rearrange("r p t -> p r t"),
                          w=[("ki", kb_, e, r_) for r_ in range(4)])
            kikeys = [("ki", kb_, e) for e in range(2)] + [("ki", kb_, e, r_) for e in range(2) for r_ in range(4)]
            akey = ("acc", g)
            acckeys.append(akey)
            for h in range(8):
                pi = (icount[0] * 8 + h) % 2
                ps = psI[pi]
                P.op("pe", "matmul", dict(out=ps[:, :nk], lhsT=qi[:, h, :], rhs=ki_t[:, :nk], start=True, stop=True),
                     r=[("qi", h)] + kikeys, w=[("psI", pi)])
                ri = rcount[0] % 3
                rcount[0] += 1
                rb = rbuf[ri]
                P.op("act", "activation", dict(out=rb[:, :nk], in_=ps[:, :nk], func=AF.Relu, scale=absw[:, h:h + 1]),
                     r=[("psI", pi), ("absw",)], w=[("r", ri)])
                if h == 0:
                    P.op("dve", "tensor_scalar", dict(out=acc[:, c0:c0 + nk], in0=rb[:, :nk], scalar1=sgn[:, 0:1],
                                                      scalar2=None, op0=ALU.mult),
                         r=[("r", ri), ("sgn",)], w=[akey])
                else:
                    P.op("dve", "scalar_tensor_tensor", dict(out=acc[:, c0:c0 + nk], in0=rb[:, :nk],
                                                             scalar=sgn[:, h:h + 1], in1=acc[:, c0:c0 + nk],
                                                             op0=ALU.mult, op1=ALU.add),
                         r=[("r", ri), ("sgn",), akey], w=[akey])
        dkey = ("acc", j)
        d0 = 512 * j
        P.op("dve", "scalar_tensor_tensor", dict(out=dtmp[:, :], in0=dm[:, :], scalar=-1.0, in1=acc[:, d0:d0 + 512],
                                                 op0=ALU.mult, op1=ALU.add), r=[("dm",), dkey], w=[("dtmp",)])
        P.op("dve", "tensor_reduce", dict(out=smc(RMIN), in_=dtmp[:, :], axis=AX.X, op=ALU.min),
             r=[("dtmp",)], w=[("sm", "rmin")])
        P.op("dve", "tensor_tensor", dict(out=acc[:, d0:d0 + 512], in0=acc[:, d0:d0 + 512], in1=dm[:, :], op=ALU.add),
             r=[("dm",), dkey], w=[dkey])
        P.op("dve", "tensor_reduce", dict(out=smc(RTMP), in_=acc[:, d0 + 512:n], axis=AX.X, op=ALU.min),
             r=acckeys, w=[("sm", "rtmp")])
        P.op("dve", "tensor_tensor", dict(out=smc(RMIN), in0=smc(RMIN), in1=smc(RTMP), op=ALU.min),
             r=[("sm", "rmin"), ("sm", "rtmp")], w=[("sm", "rmin")])
        if j > 0:
            P.op("dve", "tensor_reduce", dict(out=smc(RTMP), in_=acc[:, 0:d0], axis=AX.X, op=ALU.min),
                 r=acckeys, w=[("sm", "rtmp")])
            P.op("dve", "tensor_tensor", dict(out=smc(RMIN), in0=smc(RMIN), in1=smc(RTMP), op=ALU.min),
                 r=[("sm", "rmin"), ("sm", "rtmp")], w=[("sm", "rmin")])
        P.op("dve", "tensor_reduce", dict(out=smc(HI), in_=acc[:, 0:n], axis=AX.X, op=ALU.max),
             r=acckeys, w=[("sm", "hi")])
        P.op("dve", "tensor_copy", dict(out=smc(LO), in_=smc(RMIN)), r=[("sm", "rmin")], w=[("sm", "lo")])
        for it in range(nbis):
            P.op("dve", "scalar_tensor_tensor", dict(out=smc(MID), in0=smc(LO), scalar=smc(HI), in1=half[:, :],
                                                     op0=ALU.add, op1=ALU.mult),
                 r=[("sm", "lo"), ("sm", "hi"), ("half",)], w=[("sm", "mid")])
            P.op("dve", "tensor_scalar", dict(out=MB[:, 0:n], in0=acc[:, 0:n], scalar1=smc(MID), scalar2=0.0,
                                              op0=ALU.is_ge, op1=ALU.add, accum_out=smc(CNT)),
                 r=acckeys + [("sm", "mid")], w=[("MB",), ("sm", "cnt")])
            P.op("dve", "tensor_single_scalar", dict(out=smu[:, 0:1], in_=smc(CNT), scalar=255.5, op=ALU.is_ge),
                 r=[("sm", "cnt")], w=[("smu", 0)])
            P.op("dve", "tensor_single_scalar", dict(out=smu[:, 1:2], in_=smc(CNT), scalar=255.5, op=ALU.is_lt),
                 r=[("sm", "cnt")], w=[("smu", 1)])
            P.op("dve", "copy_predicated", dict(out=smc(LO), mask=smu[:, 0:1], data=smc(MID)),
                 r=[("smu", 0), ("sm", "mid")], w=[("sm", "lo")])
            P.op("dve", "copy_predicated", dict(out=smc(HI), mask=smu[:, 1:2], data=smc(MID)),
                 r=[("smu", 1), ("sm", "mid")], w=[("sm", "hi")])
        P.op("dve", "tensor_scalar", dict(out=MB[:, 0:n], in0=acc[:, 0:n], scalar1=smc(LO), scalar2=-BIG,
                                          op0=ALU.is_lt, op1=ALU.mult),
             r=acckeys + [("sm", "lo")], w=[("MB",)])
        if j == 0:
            tiles = [(0, 1)]
            for h in range(8):
                P.op("pool", "tensor_tensor", dict(out=MBN[:, h, 512:1024], in0=MB[:, 0:512], in1=NB[:, h, 512:1024],
                                                   op=ALU.add), r=[("MB",), ("NB",)], w=[("MBN", h)])
            for h in range(8):
                P.op("pool", "tensor_tensor", dict(out=MBNm[:, h, :], in0=MB[:, 512:528], in1=NBm0[:, h, :], op=ALU.add),
                     r=[("MB",), ("NBm0",), ("MBNm",)], w=[("MBNm",)])
            meta_mode = "j0"
        else:
            tiles = [(g, None) for g in range(j - 1)] + [(j - 1, 0), (j, 1)]
            for h in range(8):
                P.op("pool", "tensor_tensor", dict(out=MBN[:, h, :], in0=MB[:, 512 * (j - 1):512 * (j + 1)],
                                                   in1=NB[:, h, :], op=ALU.add),
                     r=[("MB",), ("NB",)], w=[("MBN", h)])
            meta_mode = "far"
        attend(nq, tiles, meta_mode, j)
        finalize(nq, tok0)


def emit_p2c(P, gKC, tmq, tmk, mix, cst, nblocks=128, do_meta=True):
    S = P.sb
    DT = S("DT", [128, 4, 128], F32)
    QD = S("QD", [64, 4, 128], F32)
    kd = S("kd", [128, 4], F32)
    kd16 = S("kd16", [16, 4], F32)
    oh = S("oh", [128, 4], F32)
    rn = S("rn", [128, 256], F32)
    idf = S("idf", [128, 128], F32)
    epsc = S("epsc", [128, 1], F32)
    kvb = [S("kvb%d" % i, [128, 512], F32) for i in range(2)]
    kdec = [S("kdec%d" % i, [128, 256], F32) for i in range(2)]
    ring = S("ring", [64, 4, 256], F32)
    ssel = S("ssel", [64, 256], F32)
    qtk = S("qtk", [128, 520], F32)
    ktk = S("ktk", [128, 512], F32)
    qT = S("qT", [64, 4, 128], F32)
    kT = S("kT", [64, 4, 128], F32)
    qd = S("qd", [64, 4, 128], F32)
    PT = [S("PT%d" % i, [128, 128], F32) for i in range(2)]
    ret = S("ret", [128, 256], F32)
    ss = S("ss", [128, 4], F32)
    yb = S("yb", [128, 256], F32)
    sg = S("sg", [128, 256], F32)
    cob = S("cob", [128, 256], BF16)
    psU = [P.ps("psU%d" % i) for i in range(2)]
    psA = [P.ps("psA%d" % i) for i in range(2)]
    psT = [P.ps("psT%d" % i) for i in range(2)]
    psO = P.ps("psO")
    gv = [a.rearrange("(r t) f -> r t f", r=4) for a in gKC]
    P.dma(DT[:, :, :], cst["DT"].rearrange("h j i -> j h i"), w=[("DT",)])
    P.dma(QD[:, :, :], cst["QD"][:, :, :], w=[("QD",)])
    P.dma(kd[:, :], cst["kd"][:, :], w=[("kd",)])
    P.dma(kd16[:, :], cst["kd16"][:, :], w=[("kd16",)])
    P.dma(oh[:, :], cst["oh"][:, :], w=[("oh",)])
    P.dma(rn[:, :], cst["rn"][:, :], w=[("rn",)])
    P.dma(idf[:, :], cst["identf"][:, :], w=[("idf",)])
    P.op("dve", "memset", dict(ap=epsc[:, :], constant=EPS), w=[("epsc",)])
    acount = [0]
    tcount = [0]

    def ret_block(tok0, L, use_state):
        P.dma(qtk[:L, :], tmq[tok0:tok0 + L, :], w=[("qtk",)])
        tix_, o_ = tile_of_tok(tok0)
        P.dma(ktk[:L, :], tmk[tix_][o_:o_ + L, :], w=[("ktk",)])
        for (src, skey, dst, dkey) in ((qtk, ("qtk",), qT, "qT"), (ktk, ("ktk",), kT, "kT")):
            for h in range(4):
                ti = tcount[0] % 2
                tcount[0] += 1
                P.op("pe", "transpose", dict(out=psT[ti][:64, :L], in_=src[:L, h * 64:(h + 1) * 64], identity=idf[:L, :L]),
                     r=[skey, ("idf",)], w=[("psT", ti)])
                P.op("act", "activation", dict(out=dst[:, h, :L], in_=psT[ti][:64, :L], func=AF.Copy),
                     r=[("psT", ti)], w=[(dkey, h)])
        qTk = [("qT", h) for h in range(4)]
        kTk = [("kT", h) for h in range(4)]
        if use_state:
            P.op("dve", "tensor_scalar", dict(out=ssel[:, :], in0=ring[:, 0, :], scalar1=oh[:64, 0:1], scalar2=None,
                                              op0=ALU.mult), r=[("ring", 0), ("oh",)], w=[("ssel",)])
            for c in range(1, 4):
                P.op("dve", "scalar_tensor_tensor", dict(out=ssel[:, :], in0=ring[:, c, :], scalar=oh[:64, c:c + 1],
                                                         in1=ssel[:, :], op0=ALU.mult, op1=ALU.add),
                     r=[("ring", c), ("oh",), ("ssel",)], w=[("ssel",)])
            P.op("dve", "tensor_tensor", dict(out=qd[:, :, :L], in0=qT[:, :, :L], in1=QD[:, :, :L], op=ALU.mult),
                 r=qTk + [("QD",)], w=[("qd",)])
        for h in range(4):
            ai = acount[0] % 2
            acount[0] += 1
            P.op("pe", "matmul", dict(out=psA[ai][:L, :L], lhsT=kT[:, h, :L], rhs=qT[:, h, :L], start=True, stop=True),
                 r=[("kT", h), ("qT", h)], w=[("psA", ai)])
            P.op("dve", "tensor_tensor", dict(out=PT[ai][:L, :L], in0=psA[ai][:L, :L], in1=DT[:L, h, :L], op=ALU.mult),
                 r=[("psA", ai), ("DT",)], w=[("PT", ai)])
            P.op("pe", "matmul", dict(out=psO[:L, h * 64:(h + 1) * 64], lhsT=PT[ai][:L, :L],
                                      rhs=ktk[:L, 256 + h * 64:256 + (h + 1) * 64],
                                      start=(h == 0), stop=(not use_state), skip_group_check=True),
                 r=[("PT", ai), ("ktk",)], w=[("psO",)])
            if use_state:
                P.op("pe", "matmul", dict(out=psO[:L, h * 64:(h + 1) * 64], lhsT=qd[:, h, :L],
                                          rhs=ssel[:, h * 64:(h + 1) * 64], start=False, stop=True,
                                          skip_group_check=True),
                     r=[("qd",), ("ssel",)], w=[("psO",)])
        P.op("act", "activation", dict(out=ret[:L, :], in_=psO[:L, 0:256], func=AF.Copy), r=[("psO",)], w=[("ret",)])
        ybk = [("yb", h) for h in range(4)]
        ssk = [("ss", h) for h in range(4)]
        P.op("dve", "tensor_tensor", dict(out=yb[:L, :], in0=ret[:L, :], in1=ret[:L, :], op=ALU.mult),
             r=[("ret",)], w=ybk)
        P.op("dve", "tensor_reduce", dict(out=ss[:L, :], in_=yb[:L, :].rearrange("p (h d) -> p h d", d=64),
                                          axis=AX.X, op=ALU.add), r=ybk, w=ssk)
        P.op("act", "activation", dict(out=ss[:L, :], in_=ss[:L, :], func=AF.Sqrt, bias=epsc[:L, 0:1], scale=1.0 / 64.0),
             r=ssk + [("epsc",)], w=ssk)
        P.op("dve", "reciprocal", dict(out=ss[:L, :], in_=ss[:L, :]), r=ssk, w=ssk)
        for h in range(4):
            P.op("dve", "scalar_tensor_tensor", dict(out=yb[:L, h * 64:(h + 1) * 64], in0=ret[:L, h * 64:(h + 1) * 64],
                                                     scalar=ss[:L, h:h + 1], in1=rn[:L, h * 64:(h + 1) * 64],
                                                     op0=ALU.mult, op1=ALU.mult),
                 r=[("ret",), ("ss", h), ("rn",)], w=[("yb", h)])
        P.op("act", "activation", dict(out=sg[:L, :], in_=qtk[:L, 256:512], func=AF.Silu), r=[("qtk",)], w=[("sg",)])
        P.op("pool", "tensor_tensor", dict(out=cob[:L, :], in0=yb[:L, :], in1=sg[:L, :], op=ALU.mult),
             r=ybk + [("sg",)], w=[("cob",)])
        P.dma(mix[tok0:tok0 + L, 768:1024], cob[:L, :], r=[("cob",)])

    if do_meta:
        ret_block(0, 16, False)
    for B in range(nblocks):
        bi = B % 2
        if B == 0:
            rk, t0, L, kdt = 0, 0, 16, kd16
        else:
            rk, t0, L, kdt = (B - 1) % 4, 16 + 128 * ((B - 1) // 4), 128, kd
        tix_, o_ = tile_of_tok(t0)
        P.dma(kvb[bi][:L, :], gv[tix_][rk, o_:o_ + L, :], w=[("kvb", bi)])
        for h in range(4):
            P.op("pool", "tensor_scalar", dict(out=kdec[bi][:L, h * 64:(h + 1) * 64], in0=kvb[bi][:L, h * 64:(h + 1) * 64],
                                               scalar1=kdt[:L, h:h + 1], scalar2=None, op0=ALU.mult),
                 r=[("kvb", bi), ("kd",), ("kd16",)], w=[("kdec", bi, h)])
        for h in range(4):
            P.op("pe", "matmul", dict(out=psU[bi][:64, h * 64:(h + 1) * 64], lhsT=kdec[bi][:L, h * 64:(h + 1) * 64],
                                      rhs=kvb[bi][:L, 256 + h * 64:256 + (h + 1) * 64], start=True, stop=True),
                 r=[("kdec", bi, h), ("kvb", bi)], w=[("psU", bi)])
        slot, pslot = B % 4, (B - 1) % 4
        if B == 0:
            P.op("dve", "tensor_copy", dict(out=ring[:, slot, :], in_=psU[bi][:64, 0:256]),
                 r=[("psU", bi)], w=[("ring", slot)])
        else:
            for h in range(4):
                P.op("dve", "scalar_tensor_tensor", dict(out=ring[:, slot, h * 64:(h + 1) * 64],
                                                         in0=ring[:, pslot, h * 64:(h + 1) * 64],
                                                         scalar=float(GAM[h] ** L), in1=psU[bi][:64, h * 64:(h + 1) * 64],
                                                         op0=ALU.mult, op1=ALU.add),
                     r=[("ring", pslot), ("psU", bi), ("ring", slot)], w=[("ring", slot)])
        if B % 4 == 3:
            ret_block(16 + 128 * (B // 4), 128, True)


def build_fused(cfg=None):
    cfg = cfg or {}
    P = Prog()
    D = P.dram
    x_in = D("hT0", [1024, NT], F32, "ExternalInput") if 0 in cfg.get("layers", (0, 1)) else None
    cs = D("cs", [NT, 512], F32, "ExternalInput")
    out = D("outT", [1024, NT], F32, "ExternalOutput")
    cst = dict(
        dmask=D("dmask", [128, 512], F32, "ExternalInput"),
        nb=D("nb", [12, 128, 1024], BF16, "ExternalInput"),
        nbm0=D("nbm0", [12, 128, 16], BF16, "ExternalInput"),
        nbmm=D("nbmm", [12, 16, 16], BF16, "ExternalInput"),
        b31=D("b31", [128, 12], F32, "ExternalInput"),
        identb=D("identb", [128, 128], BF16, "ExternalInput"),
        identf=D("identf", [128, 128], F32, "ExternalInput"),
        DT=D("DT", [4, 128, 128], F32, "ExternalInput"),
        QD=D("QD", [64, 4, 128], F32, "ExternalInput"),
        kd=D("kd", [128, 4], F32, "ExternalInput"),
        kd16=D("kd16", [16, 4], F32, "ExternalInput"),
        oh=D("oh", [128, 4], F32, "ExternalInput"),
    )
    gf = D("gf", [128, 8], F32, "ExternalInput")
    layers = cfg.get("layers", (0, 1))
    hmid = None
    if len(layers) == 2:
        hmid = D("hmid", [1024, NT], F32, "Internal")
    elif layers[0] == 1:
        hmid = D("hmid", [1024, NT], F32, "ExternalInput")
    hT = x_in if layers[0] == 0 else hmid
    for l in layers:
        w_in = D("w_in%d" % l, [1024, 3912], F32, "ExternalInput")
        gm = D("gm%d" % l, [128, 8], F32, "ExternalInput")
        wo = D("wo%d" % l, [1024, 1024], F32, "ExternalInput")
        w1 = D("w1_%d" % l, [1024, 4096], F32, "ExternalInput")
        w2 = D("w2_%d" % l, [4096, 1024], F32, "ExternalInput")
        gff = D("gff%d" % l, [128, 8], F32, "ExternalInput")
        cl = dict(cst)
        cl["lamb"] = D("lamb%d" % l, [128, 128], F32, "ExternalInput")
        cl["dnb"] = D("dnb%d" % l, [128, 64], F32, "ExternalInput")
        cl["rn"] = D("rn%d" % l, [128, 256], F32, "ExternalInput")
        fmo = D("fmo%d" % l, [NFM, NT], BF16, "Internal")
        tls = tiles_of(9)
        ksl = [D("ksl%d_%d" % (l, i), [NKROW, n_], BF16, "Internal") for i, (_, n_) in enumerate(tls)]
        tmb = [D("tmb%d_%d" % (l, i), [n_, NVB], BF16, "Internal") for i, (_, n_) in enumerate(tls)]
        tmk = [D("tmk%d_%d" % (l, i), [n_, NKC], F32, "Internal") for i, (_, n_) in enumerate(tls)]
        tmq = D("tmq%d" % l, [NT, NQC], F32, "Internal")
        gK = [D("gK%d_%d" % (l, i), [4 * NKROW, n_], BF16, "Internal") for i, (_, n_) in enumerate(tls)]
        gV = [D("gV%d_%d" % (l, i), [4 * n_, NVB], BF16, "Internal") for i, (_, n_) in enumerate(tls)]
        gKC = [D("gKC%d_%d" % (l, i), [4 * n_, NKC], F32, "Internal") for i, (_, n_) in enumerate(tls)]
        mix = D("mix%d" % l, [NT, 1024], BF16, "Internal")
        lambda_init = 0.8 - 0.6 * _math.exp(-0.3 * l)
        with P.phase():
            emit_p1(P, hT, w_in, gm, cs, fmo, ksl, tmb, tmk, tmq, gK, gV, gKC, cfg.get("p1_tiles", 9))
        with P.phase():
            emit_p2ab(P, fmo, gK, gV, tmq, mix, cl, cfg.get("njobs", NJ), True, lambda_init, cfg.get("nbis", NBIS))
        with P.phase():
            emit_p2c(P, gKC, tmq, tmk, mix, cl, cfg.get("nblocks", 128), True)
        with P.phase():
            emit_p3(P, hT, mix, wo, w1, w2, gff, gf, out if (l == 1 or len(layers) == 1) else hmid, cst["identb"],
                    cfg.get("p3_tiles", 17), final=(l == 1))
        hT = hmid
    return P.finish()


def rel_bucket_np(dist):
    n = np.maximum(dist, 0)
    nf = np.maximum(n, 1).astype(np.float32)
    large = 16 + (np.log(nf / np.float32(16)) / np.float32(_math.log(128 / 16)) * np.float32(16)).astype(np.int32)
    large = np.minimum(large, 31)
    return np.where(n < 16, n, large)


def local_positions(cc):
    pos = [np.arange(16)]
    for j in range(NJ):
        pos.append(16 + 128 * (4 * j + cc) + np.arange(128))
    return np.concatenate(pos)


def rope_table(cc):
    pos = local_positions(cc).astype(np.float32)
    inv = (np.float32(10000.0) ** (-np.arange(0, 64, 2, dtype=np.float32) / np.float32(64))).astype(np.float32)
    ang = pos[:, None] * inv[None, :]
    cos = np.cos(ang).astype(np.float32)
    sin = np.sin(ang).astype(np.float32)
    return np.ascontiguousarray(np.concatenate([np.tile(cos, (1, 8)), np.tile(sin, (1, 8))], axis=1))


def attn_consts(rel_bias, cc):
    i = np.arange(128)
    nb = np.empty((12, 128, 1024), np.float32)
    dmask = np.empty((128, 512), np.float32)
    for near in range(2):
        for cp in range(4):
            dR = (4 + cc - cp) if near == 0 else (cc - cp)
            dist = 128 * dR + i[:, None] - i[None, :]
            vis = dist >= 0
            bk = rel_bucket_np(dist)
            for hb in range(12):
                vals = rel_bias[bk, hb]
                nb[hb, :, near * 512 + cp * 128: near * 512 + (cp + 1) * 128] = np.where(vis, vals, np.float32(-BIG))
            if near == 1:
                dmask[:, cp * 128:(cp + 1) * 128] = np.where(vis, np.float32(0.0), np.float32(-BIG))
    s = np.arange(16)
    dist = 16 + 128 * cc + i[:, None] - s[None, :]
    bk = rel_bucket_np(dist)
    nbm0 = np.stack([rel_bias[bk, hb] for hb in range(12)]).astype(np.float32)
    dist = s[:, None] - s[None, :]
    bk = rel_bucket_np(dist)
    nbmm = np.stack([np.where(dist >= 0, rel_bias[bk, hb], np.float32(-BIG)) for hb in range(12)]).astype(np.float32)
    b31 = np.ascontiguousarray(np.broadcast_to(rel_bias[31][None, :], (128, 12))).astype(np.float32)
    return dict(dmask=dmask, nb=nb.astype(NP_BF16), nbm0=nbm0.astype(NP_BF16), nbmm=nbmm.astype(NP_BF16), b31=b31)


def ret_consts():
    i = np.arange(128)
    DT = np.zeros((4, 128, 128), np.float64)
    QD = np.zeros((64, 4, 128), np.float64)
    kd = np.zeros((128, 4), np.float64)
    kd16 = np.zeros((16, 4), np.float64)
    for h in range(4):
        g = GAM[h]
        d = i[None, :] - i[:, None]
        DT[h] = np.where(d >= 0, g ** np.maximum(d, 0), 0.0) / 8.0
        QD[:, h, :] = (g ** (i + 1.0))[None, :]
        kd[:, h] = g ** (127.0 - i) / 8.0
        kd16[:, h] = g ** (15.0 - np.arange(16)) / 8.0
    return dict(DT=DT.astype(np.float32), QD=QD.astype(np.float32), kd=kd.astype(np.float32), kd16=kd16.astype(np.float32))


def garr(v):
    return np.ascontiguousarray(np.asarray(v, np.float32).reshape(8, 128).T)


def shard_x(x, meta):
    hTs = []
    for c in range(8):
        b, cc = c // 4, c % 4
        xb = np.asarray(x[b], np.float32).reshape(NJ, 4, 128, 1024)[:, cc].reshape(NJ * 128, 1024)
        hTs.append(np.ascontiguousarray(np.concatenate([np.asarray(meta, np.float32), xb], axis=0).T))
    return hTs


def make_in_maps(inp):
    rel_bias = np.asarray(inp["rel_bias"], np.float32)
    hTs = shard_x(inp["x"], inp["meta"])
    rc = ret_consts()
    common = dict(identb=np.eye(128, dtype=np.float32).astype(NP_BF16), identf=np.eye(128, dtype=np.float32),
                  gf=garr(inp["final_norm"]), **rc)
    for l in range(2):
        common["w_in%d" % l] = np.ascontiguousarray(np.asarray(inp["w_in"][l], np.float32)[:, W_PERM])
        common["gm%d" % l] = garr(inp["norm_mix"][l])
        common["wo%d" % l] = np.ascontiguousarray(np.asarray(inp["w_out"][l], np.float32))
        common["w1_%d" % l] = np.ascontiguousarray(np.asarray(inp["w_ff1"][l], np.float32))
        common["w2_%d" % l] = np.ascontiguousarray(np.asarray(inp["w_ff2"][l], np.float32))
        common["gff%d" % l] = garr(inp["norm_ff"][l])
        common["lamb%d" % l] = np.ascontiguousarray(np.broadcast_to(
            np.asarray(inp["diff_lambda"][l], np.float32).reshape(1, 128), (128, 128)))
        common["dnb%d" % l] = np.ascontiguousarray(np.broadcast_to(
            np.asarray(inp["diff_norm"][l], np.float32).reshape(1, 64), (128, 64)))
        common["rn%d" % l] = np.ascontiguousarray(np.broadcast_to(
            np.asarray(inp["ret_norm"][l], np.float32).reshape(1, 256), (128, 256)))
    ropes = [rope_table(cc) for cc in range(4)]
    acs = [attn_consts(rel_bias, cc) for cc in range(4)]
    maps = []
    for c in range(8):
        cc = c % 4
        oh = np.zeros((128, 4), np.float32)
        oh[:, cc] = 1.0
        m = dict(common)
        m.update(acs[cc])
        m.update(hT0=hTs[c], cs=ropes[cc], oh=oh)
        maps.append(m)
    return maps


_NC_CACHE = {}


N_LAUNCH = 1


def run_fused(inp, cfg=None):
    cfg = dict(cfg or {})
    maps = make_in_maps(inp)
    if N_LAUNCH == 1:
        plans = [(0, 1)]
    else:
        plans = [(0,), (1,)]
    outs = None
    for layers in plans:
        c2 = dict(cfg)
        c2["layers"] = layers
        key = tuple(sorted(c2.items()))
        if key not in _NC_CACHE:
            _NC_CACHE[key] = build_fused(c2)
        if outs is not None:
            for c in range(8):
                maps[c]["hmid"] = outs[c]
        per_layer = ["w_in%d", "gm%d", "wo%d", "w1_%d", "w2_%d", "gff%d", "lamb%d", "dnb%d", "rn%d"]
        drop = set()
        for l in (0, 1):
            if l not in layers:
                drop |= {n % l for n in per_layer}
        if 0 not in layers:
            drop.add("hT0")
        if len(layers) == 2:
            drop.add("hmid")
        use = [{k: v for k, v in m.items() if k not in drop} for m in maps]
        res = run(_NC_CACHE[key], use)
        outs = [r["outT"] for r in res]
    return outs


def kernel(x, meta, rel_bias, w_in, norm_mix, diff_lambda, diff_norm, ret_norm, w_out, norm_ff, w_ff1, w_ff2, final_norm):
    inp = dict(x=x, meta=meta, rel_bias=rel_bias, w_in=w_in, norm_mix=norm_mix, diff_lambda=diff_lambda,
               diff_norm=diff_norm, ret_norm=ret_norm, w_out=w_out, norm_ff=norm_ff, w_ff1=w_ff1, w_ff2=w_ff2,
               final_norm=final_norm)
    inp = {k: np.asarray(v) for k, v in inp.items()}
    outs = run_fused(inp)
    out = np.empty((2, 16384, 1024), np.float32)
    for c in range(8):
        b, cc = c // 4, c % 4
        out[b].reshape(NJ, 4, 128, 1024)[:, cc] = outs[c][:, 16:].T.reshape(NJ, 128, 1024)
    return out
```
